# Optimizing a Trainium2 kernel written in Bass

```python
import jax, jax.numpy as jnp
from jax import lax
import numpy as np

D_MODEL = 1024
BATCH = 4
SEQ = 4096
DEPTH = 1

GRID_W = 64
CTX_LEN = 256
N_HEADS = 16
Q_RANK = 256
KV_RANK = 128
NOPE_DIM = 64
ROPE_DIM = 32
V_DIM = 64
ROPE_THETA = 10000.0
Q_BLOCK = 128
ATTN_SCALE = (NOPE_DIM + ROPE_DIM) ** -0.5
CONV_CH = D_MODEL
CONV_WIDTH = 31
CONV_HALF = CONV_WIDTH // 2
P_HEADS = 8
N_KEYS = 128
N_EXPERTS = N_KEYS * N_KEYS
P_DK = 256
P_TOPK = 16
P_CHUNK = 128
EPS = 1e-6

OFF_Q = 0
OFF_KV = Q_RANK
OFF_CONV = Q_RANK + KV_RANK + ROPE_DIM
OFF_GATE = OFF_CONV + 2 * CONV_CH
N_IN = OFF_GATE + 2 * D_MODEL

kernel_name = "hybrid_mla_conformer_peer_dit_layer"


def rms_norm(x, g):
    xf = x.astype(jnp.float32)
    y = xf * lax.rsqrt(jnp.mean(xf * xf, axis=-1, keepdims=True) + EPS)
    return (y * g.astype(jnp.float32)).astype(x.dtype)


def layer_norm(x, g, b):
    xf = x.astype(jnp.float32)
    mu = jnp.mean(xf, axis=-1, keepdims=True)
    var = jnp.mean(jnp.square(xf - mu), axis=-1, keepdims=True)
    y = (xf - mu) * lax.rsqrt(var + EPS) * g.astype(jnp.float32) + b.astype(jnp.float32)
    return y.astype(x.dtype)


def modulate(h, shift, scale):
    return h * (1 + scale) + shift


def axial_rope_tables(n_tok):
    rows = n_tok // GRID_W
    r = jnp.repeat(jnp.arange(rows, dtype=jnp.int32), GRID_W)
    col = jnp.tile(jnp.arange(GRID_W, dtype=jnp.int32), rows)
    n_freq = ROPE_DIM // 4
    freq = ROPE_THETA ** (-jnp.arange(n_freq, dtype=jnp.float32) / n_freq)
    ang = jnp.stack([r[:, None] * freq, col[:, None] * freq], axis=1)
    return jnp.cos(ang), jnp.sin(ang)


def apply_rope(x, cos, sin):
    shp = x.shape
    xr = x.reshape(shp[:-1] + (2, 2, ROPE_DIM // 4)).astype(jnp.float32)
    extra = x.ndim - 3
    c = cos.reshape(cos.shape[:1] + (1,) * extra + cos.shape[1:])
    s = sin.reshape(sin.shape[:1] + (1,) * extra + sin.shape[1:])
    x1, x2 = xr[..., 0, :], xr[..., 1, :]
    out = jnp.stack([x1 * c - x2 * s, x2 * c + x1 * s], axis=-2)
    return out.reshape(shp).astype(x.dtype)


def mla_queries(cq, q_norm_g, w_uq):
    b, n, _ = cq.shape
    q = (rms_norm(cq, q_norm_g) @ w_uq).reshape(b, n, N_HEADS, NOPE_DIM + ROPE_DIM)
    return q[..., :NOPE_DIM], q[..., NOPE_DIM:]


def mla_keys(kvr, kv_norm_g, w_ukv):
    b, n, _ = kvr.shape
    c_kv = rms_norm(kvr[..., :KV_RANK], kv_norm_g)
    kv = (c_kv @ w_ukv).reshape(b, n, N_HEADS, NOPE_DIM + V_DIM)
    return kv[..., :NOPE_DIM], kvr[..., KV_RANK:], kv[..., NOPE_DIM:]


def attend(qn, qr, kn, kr, v):
    s = jnp.einsum('bqhd,bkhd->bhqk', qn, kn) + jnp.einsum('bqhd,bkd->bhqk', qr, kr)
    p = jax.nn.softmax(s.astype(jnp.float32) * ATTN_SCALE, axis=-1).astype(v.dtype)
    return jnp.einsum('bhqk,bkhd->bqhd', p, v)


def attend_blocked(qn, qr, kn, kr, v):
    b, n = qn.shape[:2]
    nb = n // Q_BLOCK
    blk = lambda t: jnp.moveaxis(t.reshape((b, nb, Q_BLOCK) + t.shape[2:]), 1, 0)
    out = lax.map(lambda qs: attend(qs[0], qs[1], kn, kr, v), (blk(qn), blk(qr)))
    return jnp.moveaxis(out, 0, 1).reshape(b, n, N_HEADS, V_DIM)


def conformer_conv(u, conv_w, conv_b, ln_g, ln_b, w_pw):
    a, g = jnp.split(u, 2, axis=-1)
    y = a * jax.nn.sigmoid(g)
    y = lax.conv_general_dilated(y, conv_w[:, None, :], window_strides=(1,),
                                 padding=[(CONV_HALF, CONV_HALF)],
                                 dimension_numbers=('NWC', 'WIO', 'NWC'),
                                 feature_group_count=CONV_CH) + conv_b
    y = jax.nn.silu(layer_norm(y, ln_g, ln_b))
    return y @ w_pw


def merge_branches(u, attn_heads, conv_w, conv_b, ln_g, ln_b, w_pw, w_o_mla, w_out):
    b, n = attn_heads.shape[:2]
    y_a = attn_heads.reshape(b, n, N_HEADS * V_DIM) @ w_o_mla
    y_c = conformer_conv(u[..., OFF_CONV:OFF_GATE], conv_w, conv_b, ln_g, ln_b, w_pw)
    g = jax.nn.sigmoid(u[..., OFF_GATE:])
    return (g[..., :D_MODEL] * y_a + g[..., D_MODEL:] * y_c) @ w_out


def peer_ffn(h, w_pq, sub_keys, u_tab, v_tab):
    shp = h.shape
    toks = h.reshape(-1, P_CHUNK, D_MODEL)

    def chunk(xc):
        q = (xc @ w_pq).reshape(P_CHUNK, P_HEADS, 2, P_DK // 2)
        s = jnp.einsum('thpd,hpkd->thpk', q, sub_keys)
        s_top, i_top = lax.top_k(s, P_TOPK)
        cand = s_top[..., 0, :, None] + s_top[..., 1, None, :]
        best, ci = lax.top_k(cand.reshape(P_CHUNK, P_HEADS, P_TOPK * P_TOPK), P_TOPK)
        e1 = jnp.take_along_axis(i_top[..., 0, :], ci // P_TOPK, axis=-1)
        e2 = jnp.take_along_axis(i_top[..., 1, :], ci % P_TOPK, axis=-1)
        expert = e1 * N_KEYS + e2
        gate = jax.nn.softmax(best.astype(jnp.float32), axis=-1).astype(xc.dtype)
        act = jax.nn.gelu(jnp.einsum('thkd,td->thk', u_tab[expert], xc), approximate=False)
        return jnp.einsum('thk,thkd->td', gate * act, v_tab[expert])

    return lax.map(chunk, toks).reshape(shp)


def setup_inputs(seed: int = 0) -> dict:
    key = jax.random.key(seed)
    ks = jax.random.split(key, 30)
    nrm = lambda k, s, sc: jax.random.normal(k, s, jnp.float32) * sc
    D = D_MODEL
    return {
        "x": nrm(ks[0], (BATCH, SEQ, D), 1.0),
        "c": nrm(ks[1], (BATCH, D), 1.0),
        "ctx": nrm(ks[2], (BATCH, CTX_LEN, D), 1.0),
        "c_ctx": nrm(ks[3], (D,), 1.0),
        "w_mod": nrm(ks[4], (DEPTH, D, 6 * D), 0.5 * D ** -0.5),
        "b_mod": nrm(ks[5], (DEPTH, 6 * D), 0.01),
        "norm1_g": 1.0 + nrm(ks[6], (DEPTH, D), 0.02),
        "norm2_g": 1.0 + nrm(ks[7], (DEPTH, D), 0.02),
        "w_in": nrm(ks[8], (DEPTH, D, N_IN), D ** -0.5),
        "q_norm_g": 1.0 + nrm(ks[9], (DEPTH, Q_RANK), 0.02),
        "w_uq": nrm(ks[10], (DEPTH, Q_RANK, N_HEADS * (NOPE_DIM + ROPE_DIM)), Q_RANK ** -0.5),
        "kv_norm_g": 1.0 + nrm(ks[11], (DEPTH, KV_RANK), 0.02),
        "w_ukv": nrm(ks[12], (DEPTH, KV_RANK, N_HEADS * (NOPE_DIM + V_DIM)), KV_RANK ** -0.5),
        "w_o_mla": nrm(ks[13], (DEPTH, N_HEADS * V_DIM, D), (N_HEADS * V_DIM) ** -0.5),
        "conv_w": nrm(ks[14], (DEPTH, CONV_WIDTH, CONV_CH), CONV_WIDTH ** -0.5),
        "conv_b": nrm(ks[15], (DEPTH, CONV_CH), 0.01),
        "conv_ln_g": 1.0 + nrm(ks[16], (DEPTH, CONV_CH), 0.02),
        "conv_ln_b": nrm(ks[17], (DEPTH, CONV_CH), 0.01),
        "w_pw": nrm(ks[18], (DEPTH, CONV_CH, D), CONV_CH ** -0.5),
        "w_out": nrm(ks[19], (DEPTH, D, D), D ** -0.5),
        "w_pq": nrm(ks[20], (DEPTH, D, P_HEADS * P_DK), D ** -0.5),
        "sub_keys": nrm(ks[21], (DEPTH, P_HEADS, 2, N_KEYS, P_DK // 2), (P_DK // 2) ** -0.5),
        "u_experts": nrm(ks[22], (DEPTH, N_EXPERTS, D), D ** -0.5),
        "v_experts": nrm(ks[23], (DEPTH, N_EXPERTS, D), 1.0),
        "final_g": 1.0 + nrm(ks[24], (D,), 0.02),
    }


def reference(x, c, ctx, c_ctx, w_mod, b_mod, norm1_g, norm2_g, w_in, q_norm_g, w_uq,
              kv_norm_g, w_ukv, w_o_mla, conv_w, conv_b, conv_ln_g, conv_ln_b, w_pw,
              w_out, w_pq, sub_keys, u_experts, v_experts, final_g):
    n_lat = x.shape[1]
    cos, sin = axial_rope_tables(n_lat)
    for l in range(DEPTH):
        last = l == DEPTH - 1
        mod_lat = (jax.nn.silu(c) @ w_mod[l] + b_mod[l])[:, None, :]
        mod_ctx = jax.nn.silu(c_ctx) @ w_mod[l] + b_mod[l]
        sh1, sc1, g1, sh2, sc2, g2 = jnp.split(mod_lat, 6, axis=-1)
        csh1, csc1, cg1, csh2, csc2, cg2 = jnp.split(mod_ctx, 6, axis=-1)

        h = modulate(rms_norm(x, norm1_g[l]), sh1, sc1)
        u = h @ w_in[l]
        qn, qr = mla_queries(u[..., OFF_Q:OFF_KV], q_norm_g[l], w_uq[l])
        qr = apply_rope(qr, cos, sin)
        kn_l, kr_l, v_l = mla_keys(u[..., OFF_KV:OFF_CONV], kv_norm_g[l], w_ukv[l])
        kr_l = apply_rope(kr_l, cos, sin)

        hc = modulate(rms_norm(ctx, norm1_g[l]), csh1, csc1)
        if last:
            uc = hc @ w_in[l][:, OFF_KV:OFF_CONV]
            kvr_c = uc
        else:
            uc = hc @ w_in[l]
            kvr_c = uc[..., OFF_KV:OFF_CONV]
        kn_c, kr_c, v_c = mla_keys(kvr_c, kv_norm_g[l], w_ukv[l])

        kn = jnp.concatenate([kn_l, kn_c], axis=1)
        kr = jnp.concatenate([kr_l, kr_c], axis=1)
        vv = jnp.concatenate([v_l, v_c], axis=1)
        attn_lat = attend_blocked(qn, qr, kn, kr, vv)
        x = x + g1 * merge_branches(u, attn_lat, conv_w[l], conv_b[l], conv_ln_g[l],
                                    conv_ln_b[l], w_pw[l], w_o_mla[l], w_out[l])
        if not last:
            qn_c, qr_c = mla_queries(uc[..., OFF_Q:OFF_KV], q_norm_g[l], w_uq[l])
            attn_c = attend(qn_c, qr_c, kn_c, kr_c, v_c)
            ctx = ctx + cg1 * merge_branches(uc, attn_c, conv_w[l], conv_b[l], conv_ln_g[l],
                                             conv_ln_b[l], w_pw[l], w_o_mla[l], w_out[l])

        h2 = modulate(rms_norm(x, norm2_g[l]), sh2, sc2)
        x = x + g2 * peer_ffn(h2, w_pq[l], sub_keys[l], u_experts[l], v_experts[l])
        if not last:
            hc2 = modulate(rms_norm(ctx, norm2_g[l]), csh2, csc2)
            ctx = ctx + cg2 * peer_ffn(hc2, w_pq[l], sub_keys[l], u_experts[l], v_experts[l])
    return rms_norm(x, final_g)
```

```python
import numpy as np
from contextlib import ExitStack
import concourse.bass as bass
import concourse.mybir as mybir
from concourse.bass_utils import run_bass_kernel_spmd

F32 = mybir.dt.float32
BF16 = mybir.dt.bfloat16
U32 = mybir.dt.uint32
ALU = mybir.AluOpType
AF = mybir.ActivationFunctionType
AX = mybir.AxisListType
ENGS = ("tensor", "vector", "scalar", "gpsimd", "sync")

D = 1024
SEQ = 4096
CTX = 256
NKEY = SEQ + CTX
NOWN = 2048
NH = 16
EPS = 1e-6
ATTN_SCALE = 96 ** -0.5
OFF_KV = 256
OFF_KR = 384
OFF_CONV = 416
OFF_GATE = 2464
NV = 360
CUT = 0


class _Op:
    __slots__ = ("eng", "fn", "waits", "signal", "sem", "val", "is_dma", "inc")

    def __init__(self, eng, fn):
        self.eng = eng
        self.fn = fn
        self.waits = []
        self.signal = False
        self.sem = None
        self.val = 0
        self.is_dma = False
        self.inc = 1


class Prog:
    def __init__(self, nc):
        self.nc = nc
        self.ops = {e: [] for e in ENGS}
        self.last_w = {}
        self.readers = {}
        self.all_ops = []
        self.epoch = None
        self.last_by_sem = {}

    def op(self, eng, fn, reads=(), writes=(), dma=False, sem_key=None):
        o = _Op(eng, fn)
        o.is_dma = dma
        deps = []
        for r in reads:
            w = self.last_w.get(r, self.epoch)
            if w is not None:
                deps.append((w, True))
        for r in writes:
            w = self.last_w.get(r, self.epoch)
            if w is not None:
                deps.append((w, False))
            for rd in self.readers.get(r, {}).values():
                deps.append((rd, False))
        seen = set()
        for d, raw in deps:
            if id(d) in seen:
                continue
            same = (not d.is_dma) and (not dma) and d.eng == eng
            if same and eng == "tensor":
                continue
            seen.add(id(d))
            o.waits.append(d)
            d.signal = True
        if dma:
            o.sem = ("dma", sem_key if sem_key is not None else (writes[0] if writes else reads[0]))
            o.inc = 16
            o.signal = True
        else:
            o.sem = ("eng", eng)
        for r in reads:
            self.readers.setdefault(r, {})[o.sem] = o
        for r in writes:
            self.last_w[r] = o
            self.readers[r] = {}
        self.ops[eng].append(o)
        self.all_ops.append(o)
        self.last_by_sem[o.sem] = o
        return o

    def barrier(self, fn):
        o = _Op("gpsimd", fn)
        o.sem = ("eng", "gpsimd")
        for d in self.last_by_sem.values():
            if d.eng == "gpsimd" and not d.is_dma:
                continue
            o.waits.append(d)
            d.signal = True
        o.signal = True
        self.ops["gpsimd"].append(o)
        self.all_ops.append(o)
        self.last_by_sem[o.sem] = o
        self.last_w = {}
        self.readers = {}
        self.epoch = o

    def emit(self, final_wait_eng="sync"):
        nc = self.nc
        last_dma = {}
        for o in self.all_ops:
            if o.is_dma:
                last_dma[o.sem] = o
        counters = {}
        for o in self.all_ops:
            if o.signal:
                counters[o.sem] = counters.get(o.sem, 0) + o.inc
                o.val = counters[o.sem]
        sem_keys = list(counters.keys())
        self.n_sems = len(sem_keys)
        with ExitStack() as st:
            sems = {}
            for i, k in enumerate(sem_keys):
                sems[k] = st.enter_context(nc.semaphore("s%d" % i))
            block = st.enter_context(nc.Block())

            def make(engname):
                ops = self.ops[engname]

                def body(e):
                    waited = {}
                    for o in ops:
                        for d in o.waits:
                            if waited.get(d.sem, 0) >= d.val:
                                continue
                            e.wait_ge(sems[d.sem], d.val)
                            waited[d.sem] = d.val
                        inst = o.fn(e)
                        if o.signal:
                            inst.then_inc(sems[o.sem], o.inc)
                    if engname == final_wait_eng:
                        for k, o in last_dma.items():
                            if waited.get(k, 0) < o.val:
                                e.wait_ge(sems[k], o.val)
                return body

            for engname in ENGS:
                if not self.ops[engname] and engname != final_wait_eng:
                    continue
                getattr(block, engname)(make(engname))


class Arena:
    def __init__(self, tensor, nbytes):
        self.t = tensor
        self.n = nbytes
        self.off = 0

    def alloc(self, shape_free, dt):
        es = 4 if dt in (F32, U32) else 2
        n = int(np.prod(shape_free)) * es
        n_al = (n + 63) // 64 * 64
        assert self.off + n_al <= self.n, ("arena overflow", self.off, n_al, self.n)
        v = self.t[:, self.off // 2:(self.off + n) // 2]
        self.off += n_al
        if es == 4:
            v = v.bitcast(dt)
        if len(shape_free) == 2:
            v = v.rearrange("p (a b) -> p a b", b=shape_free[1])
        elif len(shape_free) == 3:
            v = v.rearrange("p (a b c) -> p a b c", b=shape_free[1], c=shape_free[2])
        return v

    def mark(self):
        return self.off

    def release(self, m):
        self.off = m


def build_nc(stop_after=None, debug=False):
    nc = bass.Bass("TRN2", target_bir_lowering=False)

    def din(name, shape, dt=F32):
        return nc.dram_tensor(name, list(shape), dt, kind="ExternalInput").ap()

    xk = din("xk", [34, 128, D])
    xo = din("xo", [17, 128, D])
    vecs_d = din("vecs", [128, NV])
    wmod = din("wmod", [D, 6 * D])
    bmodg = din("bmodg", [128, 2048])
    fgrow_d = din("fgrow", [128, D])
    win = din("win", [D, 4512])
    wkr = din("wkr", [D, 192])
    wuq = din("wuq", [256, 1536])
    wuqp = din("wuqp", [256, 1536])
    wukv = din("wukv", [128, 2048])
    womla = din("womla", [D, D])
    wpw = din("wpw", [D, D])
    wout = din("wout", [D, D])
    wpq = din("wpq", [D, 2048])
    subkT_d = din("subkT", [128, 16 * 128])
    ut = din("ut", [128, 128, 1024])
    vt = din("vt", [128, 128, 1024])
    ropek = din("ropek", [2, 128, SEQ])
    ropeq = din("ropeq", [2, 128, NOWN])
    out_d = nc.dram_tensor("out", [16, 128, D], F32, kind="ExternalOutput").ap()
    utb = nc.dram_tensor("utb", [64, 128, 2048], BF16, kind="Internal").ap()
    vtb = nc.dram_tensor("vtb", [64, 128, 2048], BF16, kind="Internal").ap()
    x1s = nc.dram_tensor("x1s", [16, 128, D], F32, kind="Internal").ap()
    hts = nc.dram_tensor("hts", [128, 8 * NOWN], BF16, kind="Internal").ap()
    zts = nc.dram_tensor("zts", [128, 8 * NOWN], BF16, kind="Internal").ap()
    dbg = {}
    if debug:
        for nm, shp in debug.items():
            dt_ = F32
            if isinstance(shp, tuple) and len(shp) == 2 and shp[1] == "bf16":
                shp, dt_ = shp[0], BF16
            dbg[nm] = nc.dram_tensor("dbg_" + nm, list(shp), dt_, kind="ExternalOutput").ap()

    st = ExitStack()
    with st:
        def sbuf(name, shape, dt):
            return st.enter_context(nc.sbuf_tensor("sb_" + name, list(shape), dt))

        ARENA_BYTES = 172 * 1024
        arena_t = sbuf("arena", [128, ARENA_BYTES // 2], BF16)
        ar = Arena(arena_t, ARENA_BYTES)
        identb = sbuf("identb", [128, 128], BF16)
        identf = sbuf("identf", [128, 128], F32)
        onesb = sbuf("onesb", [128, 128], BF16)
        onesf = sbuf("onesf", [128, 128], F32)
        vecs = sbuf("vecs", [128, NV], F32)
        scT = sbuf("scT", [128, 8, 2], F32)
        screp = sbuf("screp", [128, 8, 128], F32)
        modT = sbuf("modT", [128, 48, 2], F32)
        A1 = sbuf("A1", [128, 8, 2], F32)
        A2 = sbuf("A2", [128, 8], F32)
        g1row = sbuf("g1row", [128, D], F32)
        g2row = sbuf("g2row", [128, D], F32)
        fgrow = sbuf("fgrow", [128, D], F32)
        stage = sbuf("stage", [128, 2, 1024], F32)
        junk = sbuf("junk", [128, 8], F32)
        ps = [st.enter_context(nc.psum_tensor("ps%d" % i, [128, 512], F32)) for i in range(8)]

        P = Prog(nc)

        def MM(out, lhsT, rhs, start=True, stop=True, r=(), w=()):
            P.op("tensor", lambda e: e.matmul(out, lhsT=lhsT, rhs=rhs, start=start, stop=stop), reads=r, writes=w)

        def TR(out, in_, ident, r=(), w=()):
            P.op("tensor", lambda e: e.transpose(out=out, in_=in_, identity=ident), reads=r, writes=w)

        def ACT(out, in_, func, r=(), w=(), scale=None, bias=None, accum=None):
            kw = {}
            if scale is not None:
                kw["scale"] = scale
            if bias is not None:
                kw["bias"] = bias
            if accum is not None:
                kw["accum_out"] = accum
            P.op("scalar", lambda e: e.activation(out=out, in_=in_, func=func, **kw), reads=r, writes=w)

        def TT(eng, out, in0, in1, op, r=(), w=()):
            P.op(eng, lambda e: e.tensor_tensor(out=out, in0=in0, in1=in1, op=op), reads=r, writes=w)

        def TS(eng, out, in0, s1, s2, op0, op1=None, r=(), w=()):
            if op1 is None:
                P.op(eng, lambda e: e.tensor_scalar(out=out, in0=in0, scalar1=s1, scalar2=None, op0=op0), reads=r, writes=w)
            else:
                P.op(eng, lambda e: e.tensor_scalar(out=out, in0=in0, scalar1=s1, scalar2=s2, op0=op0, op1=op1), reads=r, writes=w)

        def STT(out, in0, scalar, in1, op0, op1, r=(), w=()):
            P.op("vector", lambda e: e.scalar_tensor_tensor(out=out, in0=in0, scalar=scalar, in1=in1, op0=op0, op1=op1), reads=r, writes=w)

        def CP(eng, out, in_, r=(), w=()):
            if eng == "scalar":
                P.op(eng, lambda e: e.copy(out=out, in_=in_), reads=r, writes=w)
            else:
                P.op(eng, lambda e: e.tensor_copy(out=out, in_=in_), reads=r, writes=w)

        def RECIP(out, in_, r=(), w=()):
            P.op("vector", lambda e: e.reciprocal(out=out, in_=in_), reads=r, writes=w)

        def MEMSET(eng, ap, val, w=()):
            P.op(eng, lambda e: e.memset(ap, val), writes=w)

        dmaq = ["sync", "scalar"]
        dma_i = [0]

        def DMA(out, in_, r=(), w=(), q=None, sem_key=None):
            if q is None:
                q = "sync"
            P.op(q, lambda e: e.dma_start(out=out, in_=in_), reads=r, writes=w, dma=True, sem_key=sem_key)

        def BARRIER():
            P.barrier(lambda e: e.memset(junk[:, 0:1], 0.0))
            ar.off = RC

        RA, RB, RC = 0, 34 * 1024, 67 * 1024
        stage_i = [0]
        cv_i = [0]

        def load_w(dst, src, ncols, wkey, conv_eng=None):
            assert ncols <= 1024
            s = stage_i[0] % 2
            stage_i[0] += 1
            sv = stage[:, s, 0:ncols]
            if len(src.shape) == 3:
                sv = sv.rearrange("p (a b) -> p a b", b=src.shape[2])
            DMA(sv, src, w=[("stage", s)])
            if conv_eng is None:
                conv_eng = ("gpsimd", "vector")[cv_i[0] % 2]
                cv_i[0] += 1
            CP(conv_eng, dst, sv, r=[("stage", s)], w=[wkey])

        def dump(name, src, r):
            if debug and name in dbg:
                DMA(dbg[name], src, r=r, w=[("dbg", name)])

        MEMSET("gpsimd", identf[:], 0.0, w=["identf"])
        P.op("gpsimd", lambda e: e.affine_select(out=identf[:], in_=identf[:], pattern=[[-1, 128]], base=0,
                                                 channel_multiplier=1, compare_op=ALU.not_equal, fill=1.0),
             reads=["identf"], writes=["identf"])
        CP("vector", identb[:], identf[:], r=["identf"], w=["identb"])
        MEMSET("vector", onesb[:], 1.0, w=["onesb"])
        MEMSET("vector", onesf[:], 1.0, w=["onesf"])
        DMA(vecs[:], vecs_d, w=["vecs"])
        DMA(fgrow[:], fgrow_d, w=["fgrow"])
        epsb = sbuf("epsb", [128, 1], F32)
        MEMSET("vector", epsb[:], EPS, w=["epsb"])
        MEMSET("vector", modT[:], 0.0, w=["modT"])
        V_N1, V_N2, V_QG, V_KVG, V_CB, V_LNG, V_LNB, V_CW, V_BM, V_CT, V_HM = 0, 8, 16, 18, 19, 27, 35, 43, 291, 339, 355

        cT = vecs[:, V_CT:V_CT + 16].rearrange("p (k j) -> p k j", j=2)
        ACT(scT[:], cT, AF.Silu, r=["vecs"], w=["scT"])
        CP("vector", screp[:], scT[:, :, 0:1].to_broadcast([128, 8, 128]), r=["scT"], w=["screp"])
        ar.off = RC
        wmb = [ar.alloc([8, 512], F32) for _ in range(2)]
        bg = ar.alloc([2048], F32)
        DMA(bg, bmodg, w=["bg"])
        wmod_v = wmod.rearrange("(k p) f -> p k f", p=128)
        for blk in range(12):
            s = blk % 2
            DMA(wmb[s], wmod_v[:, :, blk * 512:(blk + 1) * 512], w=[("wmb", s)], q=dmaq[blk % 2])
            if blk in (4, 5, 10, 11):
                row = g1row if blk < 6 else g2row
                half = blk % 2
                for k in range(8):
                    MM(ps[0][:], screp[:, k, :], wmb[s][:, k, :], start=(k == 0), stop=(k == 7),
                       r=["screp", ("wmb", s)], w=[("ps", 0)])
                goff = (0 if blk < 6 else 1024) + half * 512
                TT("vector", row[:, half * 512:(half + 1) * 512], ps[0][:], bg[:, goff:goff + 512], ALU.add,
                   r=[("ps", 0), "bg"], w=[("row", blk)])
            else:
                for fc in range(4):
                    for k in range(8):
                        MM(ps[1][:, fc * 2:fc * 2 + 2], wmb[s][:, k, fc * 128:(fc + 1) * 128], scT[:, k, :],
                           start=(k == 0), stop=(k == 7), r=["scT", ("wmb", s)], w=[("ps", 1)])
                for fc in range(4):
                    f = blk * 4 + fc
                    TS("vector", modT[:, f, :], ps[1][:, fc * 2:fc * 2 + 2], vecs[:, V_BM + f:V_BM + f + 1], None, ALU.add,
                       r=[("ps", 1), "vecs"], w=["modT"])
        for j in range(2):
            STT(A1[:, :, j], modT[:, 8:16, j], 1.0, vecs[:, V_N1:V_N1 + 8], ALU.add, ALU.mult, r=["modT", "vecs"], w=["A1"])
        STT(A2[:], modT[:, 32:40, 0], 1.0, vecs[:, V_N2:V_N2 + 8], ALU.add, ALU.mult, r=["modT", "vecs"], w=["A2"])
        dump("modT", modT[:].rearrange("p a b -> p (a b)"), ["modT"])
        dump("g1row", g1row[:], [("row", 4), ("row", 5)])
        if stop_after == "mod":
            P.emit()
            return nc
        BARRIER()

        ar.off = RA
        ckvnT = ar.alloc([NKEY], BF16)
        KT = [ar.alloc([NKEY], BF16) for _ in range(2)]
        cqnT = ar.alloc([2, NOWN], BF16)
        assert ar.off <= RB
        ar.off = RC

        def norm_tile(src_tile, dst, j, Asc, Bsh, xt, sq, xn, ss, rs, psb, tag, dkey):
            if src_tile is not None:
                DMA(xt, src_tile, w=[(tag, "xt")], q=dmaq[j % 2])
            if CUT == 5:
                return
            ACT(sq, xt, AF.Square, r=[(tag, "xt")], w=[(tag, "sq"), (tag, "ss")], accum=ss)
            if CUT == 6:
                return
            ACT(rs, ss, AF.Sqrt, r=[(tag, "ss")], w=[(tag, "rs")], scale=1.0 / D, bias=epsb[:])
            if CUT == 7:
                return
            RECIP(rs, rs, r=[(tag, "rs")], w=[(tag, "rs")])
            TS("vector", xn, xt, rs, None, ALU.mult, r=[(tag, "xt"), (tag, "rs")], w=[(tag, "xn")])
            if CUT == 8:
                return
            pb = ps[psb][:].bitcast(BF16)
            for k in range(8):
                TR(pb[:, k * 128:(k + 1) * 128], xn[:, k * 128:(k + 1) * 128], identb[:], r=[(tag, "xn"), "identb"], w=[("ps", psb)])
            if CUT == 9:
                return
            pb3 = pb.rearrange("p (k t) -> p k t", k=8)
            tmpn = tmpns[j % 2]
            TT("vector", tmpn[:], pb3, Asc.unsqueeze(2).to_broadcast([128, 8, 128]), ALU.mult,
               r=[("ps", psb), "A1", "A2", "modT"], w=[("tmpn", j % 2)])
            TT("gpsimd", dst, tmpn[:], Bsh.unsqueeze(2).to_broadcast([128, 8, 128]), ALU.add,
               r=[("tmpn", j % 2), "A1", "A2", "modT"], w=[(dkey, 0)])

        tmpns = [sbuf("tmpn%d" % i, [128, 8, 128], F32) for i in range(2)]

        xts = [ar.alloc([D], F32) for _ in range(2)]
        sqj = ar.alloc([D], F32)
        xns = [ar.alloc([D], BF16) for _ in range(2)]
        sss = [ar.alloc([1], F32) for _ in range(2)]
        rss = [ar.alloc([1], F32) for _ in range(2)]
        hTb = [ar.alloc([8, 512], BF16) for _ in range(2)]
        wkvb = ar.alloc([8, 320], BF16)
        sqb = ar.alloc([512], BF16)
        rstd = ar.alloc([512], F32)
        rk = [ar.alloc([2, 512], F32) for _ in range(2)]
        t1 = ar.alloc([512], F32)
        t2 = ar.alloc([512], F32)
        win_v = win.rearrange("(k p) f -> p k f", p=128)
        wkr_v = wkr.rearrange("(k p) f -> p k f", p=128)
        load_w(wkvb[:, :, 0:128], win_v[:, :, OFF_KV:OFF_KV + 128], 8 * 128, ("wkvb", 0))
        load_w(wkvb[:, 0:4, 128:320], wkr_v[:, 0:4, :], 4 * 192, ("wkvb", 1))
        load_w(wkvb[:, 4:8, 128:320], wkr_v[:, 4:8, :], 4 * 192, ("wkvb", 2))
        ti = 0
        if CUT == 1:
            P.emit()
            return nc
        for blk in range(9):
            ntile = 4 if blk < 8 else 2
            n = ntile * 128
            hb = hTb[blk % 2]
            jm = 0 if blk < 8 else 1
            for tl in range(ntile):
                t = blk * 4 + tl
                s = ti % 2
                norm_tile(xk[t], hb[:, :, tl * 128:(tl + 1) * 128], ti, A1[:, :, jm], modT[:, 0:8, jm],
                          xts[s], sqj, xns[s], sss[s], rss[s], 2 + s, ("nk", s), ("hTb", blk % 2, s))
                ti += 1
            hr = [(("hTb", blk % 2, s_), 0) for s_ in range(2)]
            if CUT == 2 or CUT >= 5:
                P.emit()
                return nc
            for k in range(8):
                MM(ps[4][:, 0:n], wkvb[:, k, 0:128], hb[:, k, 0:n], start=(k == 0), stop=(k == 7),
                   r=hr + [("wkvb", 0)], w=[("ps", 4)])
            for k in range(8):
                MM(ps[5][0:96, 0:n], wkvb[:, k, 128:224], hb[:, k, 0:n], start=(k == 0), stop=(k == 7),
                   r=hr + [("wkvb", 1), ("wkvb", 2)], w=[("ps", 5)])
            if blk < 8:
                for k in range(8):
                    MM(ps[6][0:96, 0:n], wkvb[:, k, 224:320], hb[:, k, 0:n], start=(k == 0), stop=(k == 7),
                       r=hr + [("wkvb", 1), ("wkvb", 2)], w=[("ps", 6)])
            if CUT == 3:
                P.emit()
                return nc
            ACT(sqb[:, 0:n], ps[4][:, 0:n], AF.Square, r=[("ps", 4)], w=["sqb"])
            MM(ps[7][:, 0:n], onesb[:], sqb[:, 0:n], r=["onesb", "sqb"], w=[("ps", 7)])
            ACT(rstd[:, 0:n], ps[7][:, 0:n], AF.Sqrt, r=[("ps", 7)], w=["rstd"], scale=1.0 / 128, bias=epsb[:])
            RECIP(rstd[:, 0:n], rstd[:, 0:n], r=["rstd"], w=["rstd"])
            STT(ckvnT[:, blk * 512:blk * 512 + n], ps[4][:, 0:n], vecs[:, V_KVG:V_KVG + 1], rstd[:, 0:n], ALU.mult, ALU.mult,
                r=[("ps", 4), "rstd", "vecs"], w=[("ckvnT", blk)])
            if CUT == 4:
                P.emit()
                return nc
            if blk < 8:
                rkb = rk[blk % 2]
                DMA(rkb[64:96, 0, :], ropek[0, 64:96, blk * 512:(blk + 1) * 512], w=[("rk", blk % 2, 0)], q="scalar")
                DMA(rkb[64:96, 1, :], ropek[1, 64:96, blk * 512:(blk + 1) * 512], w=[("rk", blk % 2, 1)], q="scalar")
                TT("vector", t1[64:96, :], ps[5][64:96, :], rkb[64:96, 0, :], ALU.mult, r=[("ps", 5), ("rk", blk % 2, 0)], w=["t1"])
                TT("vector", t2[64:96, :], ps[6][64:96, :], rkb[64:96, 1, :], ALU.mult, r=[("ps", 6), ("rk", blk % 2, 1)], w=["t2"])
                TT("vector", KT[0][64:96, blk * 512:(blk + 1) * 512], t1[64:96, :], t2[64:96, :], ALU.add, r=["t1", "t2"], w=[("KTr", 0, blk)])
                CP("gpsimd", KT[1][64:96, blk * 512:(blk + 1) * 512], KT[0][64:96, blk * 512:(blk + 1) * 512], r=[("KTr", 0, blk)], w=[("KTr", 1, blk)])
            else:
                CP("vector", KT[0][64:96, blk * 512:blk * 512 + n], ps[5][64:96, 0:n], r=[("ps", 5)], w=[("KTr", 0, blk)])
                CP("gpsimd", KT[1][64:96, blk * 512:blk * 512 + n], KT[0][64:96, blk * 512:blk * 512 + n], r=[("KTr", 0, blk)], w=[("KTr", 1, blk)])
        if debug:
            dk = ar.alloc([NKEY], F32)
            CP("vector", dk[:], ckvnT[:], r=[("ckvnT", b) for b in range(9)], w=["dk"])
            dump("ckvnT", dk[:], ["dk"])
            dk2 = ar.alloc([NKEY], F32)
            CP("vector", dk2[64:96, :], KT[1][64:96, :], r=[("KTr", 1, b) for b in range(9)], w=["dk2"])
            dump("krot", dk2[64:96, :], ["dk2"])
        if stop_after == "K":
            P.emit()
            return nc
        BARRIER()

        ar.off = RB
        ypad = ar.alloc([8, NOWN + 30], BF16)
        assert ar.off <= RC
        ar.off = RC
        xts = [ar.alloc([D], F32) for _ in range(2)]
        sqj = ar.alloc([D], F32)
        xns = [ar.alloc([D], BF16) for _ in range(2)]
        sss = [ar.alloc([1], F32) for _ in range(2)]
        rss = [ar.alloc([1], F32) for _ in range(2)]
        hTo = ar.alloc([8, 17 * 128], BF16)
        wch = [ar.alloc([8, 128], BF16) for _ in range(4)]
        sg = [ar.alloc([512], F32) for _ in range(2)]
        yh = ar.alloc([8, 128], BF16)
        sqb2 = ar.alloc([2, 512], BF16)
        rstd = ar.alloc([512], F32)
        for t in range(17):
            s = t % 2
            norm_tile(xo[t], hTo[:, :, t * 128:(t + 1) * 128], t, A1[:, :, 0], modT[:, 0:8, 0],
                      xts[s], sqj, xns[s], sss[s], rss[s], 2 + s, ("no", s), ("hTo", t))
        DMA(hts.rearrange("p (k t) -> p k t", k=8), hTo[:, :, 0:NOWN],
            r=[(("hTo", t), 0) for t in range(16)], w=["hts"])
        blocks = [(tb * 512, 512, [4 * tb + i for i in range(4)]) for tb in range(4)] + [(2048, 128, [16])]
        load_w(wch[0], win_v[:, :, 0:128], 1024, ("wch", 0))
        load_w(wch[1], win_v[:, :, 128:256], 1024, ("wch", 1))
        for bi in range(4):
            t0, n, tiles = blocks[bi]
            hr = [(("hTo", t), 0) for t in tiles]
            for cc in range(2):
                for k in range(8):
                    MM(ps[4 + cc][:], wch[cc][:, k, :], hTo[:, k, t0:t0 + n], start=(k == 0), stop=(k == 7),
                       r=hr + [("wch", cc)], w=[("ps", 4 + cc)])
                ACT(sqb2[:, cc, :], ps[4 + cc][:], AF.Square, r=[("ps", 4 + cc)], w=[("sqb2", cc)])
            MM(ps[6][:], onesb[:], sqb2[:, 0, :], start=True, stop=False, r=[("sqb2", 0)], w=[("ps", 6)])
            MM(ps[6][:], onesb[:], sqb2[:, 1, :], start=False, stop=True, r=[("sqb2", 1)], w=[("ps", 6)])
            ACT(rstd[:], ps[6][:], AF.Sqrt, r=[("ps", 6)], w=["rstd"], scale=1.0 / 256, bias=epsb[:])
            RECIP(rstd[:], rstd[:], r=["rstd"], w=["rstd"])
            for cc in range(2):
                STT(cqnT[:, cc, t0:t0 + n], ps[4 + cc][:], vecs[:, V_QG + cc:V_QG + cc + 1], rstd[:], ALU.mult, ALU.mult,
                    r=[("ps", 4 + cc), "rstd"], w=[("cqnT", cc, bi)])
        for c in range(8):
            wa, wg = (2 * c) % 4, (2 * c + 1) % 4
            load_w(wch[wa], win_v[:, :, OFF_CONV + c * 128:OFF_CONV + (c + 1) * 128], 1024, ("wch", wa))
            load_w(wch[wg], win_v[:, :, OFF_CONV + 1024 + c * 128:OFF_CONV + 1024 + (c + 1) * 128], 1024, ("wch", wg))
            for bi, (t0, n, tiles) in enumerate(blocks):
                hr = [(("hTo", t), 0) for t in tiles]
                ba, bg_ = (4, 5) if bi % 2 == 0 else (6, 7)
                for k in range(8):
                    MM(ps[ba][:, 0:n], wch[wa][:, k, :], hTo[:, k, t0:t0 + n], start=(k == 0), stop=(k == 7),
                       r=hr + [("wch", wa)], w=[("ps", ba)])
                for k in range(8):
                    MM(ps[bg_][:, 0:n], wch[wg][:, k, :], hTo[:, k, t0:t0 + n], start=(k == 0), stop=(k == 7),
                       r=hr + [("wch", wg)], w=[("ps", bg_)])
                sgb = sg[bi % 2]
                ACT(sgb[:, 0:n], ps[bg_][:, 0:n], AF.Sigmoid, r=[("ps", bg_)], w=[("sg", bi % 2)])
                dst = ypad[:, c, 15 + t0:15 + t0 + n] if bi < 4 else yh[:, c, :]
                TT("vector", dst, ps[ba][:, 0:n], sgb[:, 0:n], ALU.mult, r=[("ps", ba), ("sg", bi % 2)],
                   w=[("ypad", c, bi)])
            TS("vector", ypad[:, c, 0:15], yh[:, c, 0:15], vecs[:, V_HM:V_HM + 1], None, ALU.mult,
               r=[("ypad", c, 4), "vecs"], w=[("ypad", c, 5)])
            TS("vector", ypad[:, c, NOWN + 15:NOWN + 30], yh[:, c, 15:30], vecs[:, V_HM + 1:V_HM + 2], None, ALU.mult,
               r=[("ypad", c, 4), "vecs"], w=[("ypad", c, 6)])
        if debug:
            dump("cqnT", cqnT[:].rearrange("p a b -> p (a b)"), [("cqnT", cc, bi) for cc in range(2) for bi in range(4)])
            dump("ypad", ypad[:].rearrange("p a b -> p (a b)"), [("ypad", c, i) for c in range(8) for i in range(7)])
        if stop_after == "O":
            P.emit()
            return nc
        BARRIER()

        dg = [ar.alloc([31, 128], BF16) for _ in range(2)]
        vT = ar.alloc([8, NOWN], BF16)
        sqv = [ar.alloc([512], BF16) for _ in range(2)]
        mean = ar.alloc([512], F32)
        msq = ar.alloc([512], F32)
        rstdc = ar.alloc([512], F32)
        tmpc = [ar.alloc([512], F32) for _ in range(2)]
        for c in range(8):
            d_ = dg[c % 2]
            TT("vector", d_[:], identb[:].unsqueeze(1).to_broadcast([128, 31, 128]),
               vecs[:, V_CW + c * 31:V_CW + (c + 1) * 31].unsqueeze(2).to_broadcast([128, 31, 128]), ALU.mult,
               r=[], w=[("dg", c % 2)])
            for tb in range(4):
                bank = 4 + (tb % 2)
                for k in range(31):
                    MM(ps[bank][:], d_[:, k, :], ypad[:, c, tb * 512 + k:tb * 512 + k + 512], start=(k == 0), stop=(k == 30),
                       r=[("dg", c % 2)], w=[("ps", bank)])
                ACT(vT[:, c, tb * 512:(tb + 1) * 512], ps[bank][:], AF.Identity, r=[("ps", bank)], w=[("vT", c, tb)],
                    bias=vecs[:, V_CB + c:V_CB + c + 1])
        for tb in range(4):
            tsl = slice(tb * 512, (tb + 1) * 512)
            for c in range(8):
                sv = sqv[c % 2]
                ACT(sv[:], vT[:, c, tsl], AF.Square, r=[("vT", c, tb)], w=[("sqv", c % 2)])
                MM(ps[6][:], onesb[:], vT[:, c, tsl], start=(c == 0), stop=(c == 7), r=[("vT", c, tb)], w=[("ps", 6)])
                MM(ps[7][:], onesb[:], sv[:], start=(c == 0), stop=(c == 7), r=[("sqv", c % 2)], w=[("ps", 7)])
            ACT(mean[:], ps[6][:], AF.Identity, r=[("ps", 6)], w=["mean"], scale=1.0 / D)
            ACT(msq[:], ps[7][:], AF.Identity, r=[("ps", 7)], w=["msq"], scale=1.0 / D)
            TT("vector", rstdc[:], mean[:], mean[:], ALU.mult, r=["mean"], w=["rstdc"])
            TT("vector", rstdc[:], msq[:], rstdc[:], ALU.subtract, r=["msq", "rstdc"], w=["rstdc"])
            ACT(rstdc[:], rstdc[:], AF.Sqrt, r=["rstdc"], w=["rstdc"], bias=epsb[:])
            RECIP(rstdc[:], rstdc[:], r=["rstdc"], w=["rstdc"])
            for c in range(8):
                tm = tmpc[c % 2]
                TT("vector", tm[:], vT[:, c, tsl], mean[:], ALU.subtract, r=[("vT", c, tb), "mean"], w=[("tmpc", c % 2)])
                TT("vector", tm[:], tm[:], rstdc[:], ALU.mult, r=[("tmpc", c % 2), "rstdc"], w=[("tmpc", c % 2)])
                ACT(vT[:, c, tsl], tm[:], AF.Silu, r=[("tmpc", c % 2)], w=[("vT", c, tb)],
                    scale=vecs[:, V_LNG + c:V_LNG + c + 1], bias=vecs[:, V_LNB + c:V_LNB + c + 1])
        zkeys = [("vT", c, tb) for c in range(8) for tb in range(4)]
        DMA(zts.rearrange("p (k t) -> p k t", k=8), vT[:], r=zkeys, w=["zts"])
        if debug:
            dump("zT", vT[:].rearrange("p a b -> p (a b)"), zkeys)
        if stop_after == "C":
            P.emit()
            return nc
        BARRIER()

        ar.off = RB
        OT = ar.alloc([8, NOWN], BF16)
        assert ar.off <= RC
        ar.off = RC
        VH = [ar.alloc([34, 128], BF16) for _ in range(2)]
        QT = [ar.alloc([NOWN], BF16) for _ in range(2)]
        PT = [ar.alloc([512], BF16) for _ in range(4)]
        wuqb = ar.alloc([2, 1536], BF16)
        wuqpb = ar.alloc([2, 1536], BF16)
        wukvb = ar.alloc([2048], BF16)
        rq = ar.alloc([2, NOWN], F32)
        qt1 = ar.alloc([512], F32)
        qt2 = ar.alloc([512], F32)
        rden = ar.alloc([512], F32)
        rdenB = ar.alloc([512], F32)
        wuq_v = wuq.rearrange("(k p) f -> p k f", p=128)
        wuqp_v = wuqp.rearrange("(k p) f -> p k f", p=128)
        for k in range(2):
            for hh in range(2):
                load_w(wuqb[:, k, hh * 768:(hh + 1) * 768], wuq_v[:, k, hh * 768:(hh + 1) * 768], 768, ("wuqb", k, hh))
                load_w(wuqpb[:, k, hh * 768:(hh + 1) * 768], wuqp_v[:, k, hh * 768:(hh + 1) * 768], 768, ("wuqpb", k, hh))
        for hh in range(2):
            load_w(wukvb[:, hh * 1024:(hh + 1) * 1024], wukv[:, hh * 1024:(hh + 1) * 1024], 1024, ("wukvb", hh))
        wq_keys = [("wuqb", k, hh) for k in range(2) for hh in range(2)]
        wqp_keys = [("wuqpb", k, hh) for k in range(2) for hh in range(2)]
        wkv_keys = [("wukvb", 0), ("wukvb", 1)]
        DMA(rq[0:96, 0, :], ropeq[0, 0:96, :], w=[("rq", 0)])
        DMA(rq[0:96, 1, :], ropeq[1, 0:96, :], w=[("rq", 1)], q="scalar")
        for hb in range(2):
            MEMSET("gpsimd", VH[hb][:], 0.0, w=[("VH", hb)])
        MEMSET("gpsimd", VH[0][:, :, 64:65], 1.0, w=[("VH", 0)])
        MEMSET("gpsimd", VH[1][:, :, 0:1], 1.0, w=[("VH", 1)])
        ckeys = [("ckvnT", b_) for b_ in range(9)]
        cvf = [ar.alloc([2, 1024], F32) for _ in range(2)]
        cvb = [ar.alloc([2, 1024], BF16) for _ in range(2)]
        cv_steps = [(tab, tabb, nm, g) for (tab, tabb, nm) in ((ut, utb, "u"), (vt, vtb, "v")) for g in range(64)]
        cv_pos = [0]

        def conv_step():
            if cv_pos[0] >= len(cv_steps):
                return
            tab, tabb, nm, g = cv_steps[cv_pos[0]]
            s = cv_pos[0] % 2
            DMA(cvf[s][:], tab[g * 2:(g + 1) * 2].rearrange("i p f -> p i f"), w=[("cvf", s)], q="sync")
            CP(("gpsimd", "vector")[cv_pos[0] % 2], cvb[s][:], cvf[s][:], r=[("cvf", s)], w=[("cvb", s)])
            DMA(tabb[g], cvb[s][:].rearrange("p a b -> p (a b)"), r=[("cvb", s)], w=[("tabb", nm, g)], q="sync",
                sem_key=("tabb", s))
            cv_pos[0] += 1

        def prep_steps(h):
            hb = h % 2
            voff = 0 if hb == 0 else 64
            steps = []
            for blk in range(9):
                def st_k(blk=blk):
                    n = 512 if blk < 8 else 256
                    bank = 5 + (blk % 2)
                    MM(ps[bank][0:64, 0:n], wukvb[:, h * 128:h * 128 + 64], ckvnT[:, blk * 512:blk * 512 + n],
                       r=wkv_keys + ckeys, w=[("ps", bank)])
                    CP("vector", KT[hb][0:64, blk * 512:blk * 512 + n], ps[bank][0:64, 0:n], r=[("ps", bank)], w=[("KTn", hb)])
                steps.append(st_k)
            for g in range(5):
                def st_v(g=g):
                    cnt = 8 if g < 4 else 2
                    bank = 5 + (g % 2)
                    for j in range(cnt):
                        kt = g * 8 + j
                        MM(ps[bank][:, j * 64:(j + 1) * 64], ckvnT[:, kt * 128:(kt + 1) * 128], wukvb[:, h * 128 + 64:h * 128 + 128],
                           r=wkv_keys + ckeys, w=[("ps", bank)])
                    CP("vector", VH[hb][:, g * 8:g * 8 + cnt, voff:voff + 64],
                       ps[bank][:, 0:cnt * 64].rearrange("p (a b) -> p a b", b=64), r=[("ps", bank)], w=[("VH", hb)])
                steps.append(st_v)
            for qb in range(4):
                def st_q(qb=qb):
                    qs = slice(qb * 512, (qb + 1) * 512)
                    for k in range(2):
                        MM(ps[5][0:96, :], wuqb[:, k, h * 96:(h + 1) * 96], cqnT[:, k, qs], start=(k == 0), stop=(k == 1),
                           r=wq_keys + [("cqnT", k, qb)], w=[("ps", 5)])
                    for k in range(2):
                        MM(ps[6][0:96, :], wuqpb[:, k, h * 96:(h + 1) * 96], cqnT[:, k, qs], start=(k == 0), stop=(k == 1),
                           r=wqp_keys + [("cqnT", k, qb)], w=[("ps", 6)])
                    TT("vector", qt1[0:96, :], ps[5][0:96, :], rq[0:96, 0, qs], ALU.mult, r=[("ps", 5), ("rq", 0)], w=["qt1"])
                    TT("vector", qt2[0:96, :], ps[6][0:96, :], rq[0:96, 1, qs], ALU.mult, r=[("ps", 6), ("rq", 1)], w=["qt2"])
                    TT("vector", QT[hb][0:96, qs], qt1[0:96, :], qt2[0:96, :], ALU.add, r=["qt1", "qt2"], w=[("QT", hb)])
                steps.append(st_q)
            return steps

        def prep(h):
            for st_ in prep_steps(h):
                st_()

        prep(0)
        gi = [0]
        for h in range(NH):
            hb = h % 2
            ktr = [("KTn", hb)] + [("KTr", hb, b_) for b_ in range(9)]
            items = [(qb, kt) for qb in range(4) for kt in range(34)]
            base = gi[0]

            def S(idx):
                qb, kt = items[idx]
                sb_ = (base + idx) % 3
                MM(ps[sb_][:], KT[hb][0:96, kt * 128:(kt + 1) * 128], QT[hb][0:96, qb * 512:(qb + 1) * 512],
                   r=ktr + [("QT", hb)], w=[("ps", sb_)])

            pending = []
            nsteps = prep_steps(h + 1) if h + 1 < NH else []
            S(0)
            S(1)
            for idx, (qb, kt) in enumerate(items):
                if idx + 2 < len(items):
                    S(idx + 2)
                sb_ = (base + idx) % 3
                pb_ = (base + idx) % 4
                ob = 3 + (qb % 2)
                qs = slice(qb * 512, (qb + 1) * 512)
                ACT(PT[pb_][:], ps[sb_][:], AF.Exp, r=[("ps", sb_)], w=[("PT", pb_)], scale=ATTN_SCALE)
                MM(ps[ob][:], VH[hb][:, kt, :], PT[pb_][:], start=(kt == 0), stop=(kt == 33),
                   r=[("VH", hb), ("PT", pb_)], w=[("ps", ob)])
                if pending and pending[0][0] <= idx:
                    _, (r0, prow, ob2, qs2) = pending.pop(0)
                    MM(ps[7][:], onesf[r0:r0 + 1, :], rden[r0:r0 + 1, :], r=["rden"], w=[("ps", 7)])
                    CP("vector", rdenB[:], ps[7][:], r=[("ps", 7)], w=["rdenB"])
                    TT("vector", OT[prow, h // 2, qs2], ps[ob2][prow, :], rdenB[prow, :], ALU.mult, r=[("ps", ob2), "rdenB"],
                       w=[("OT", h, qs2.start // 512)])
                if kt == 33:
                    r0 = 64 if hb == 0 else 0
                    prow = slice(0, 64) if hb == 0 else slice(64, 128)
                    RECIP(rden[r0:r0 + 1, :], ps[ob][r0:r0 + 1, :], r=[("ps", ob)], w=["rden"])
                    pending.append((idx + 6 if qb < 3 else idx, (r0, prow, ob, qs)))
                if idx % 17 == 8:
                    conv_step()
                if idx >= 10 and idx % 5 == 0 and nsteps:
                    nsteps.pop(0)()
            while pending:
                _, (r0, prow, ob2, qs2) = pending.pop(0)
                MM(ps[7][:], onesf[r0:r0 + 1, :], rden[r0:r0 + 1, :], r=["rden"], w=[("ps", 7)])
                CP("vector", rdenB[:], ps[7][:], r=[("ps", 7)], w=["rdenB"])
                TT("vector", OT[prow, h // 2, qs2], ps[ob2][prow, :], rdenB[prow, :], ALU.mult, r=[("ps", ob2), "rdenB"],
                   w=[("OT", h, qs2.start // 512)])
            while nsteps:
                nsteps.pop(0)()
            gi[0] += len(items)
        while cv_pos[0] < len(cv_steps):
            conv_step()
        okeys = [("OT", h, qb) for h in range(NH) for qb in range(4)]
        if debug:
            dump("OT", OT[:].rearrange("p a b -> p (a b)"), okeys)
        if stop_after == "ATT":
            P.emit()
            return nc
        BARRIER()

        ar.off = RA
        mT = ar.alloc([8, NOWN], BF16)
        assert ar.off <= RB
        ar.off = RC
        hTo2 = ar.alloc([8, NOWN], BF16)
        zT2 = ar.alloc([8, NOWN], BF16)
        DMA(hTo2[:], hts.rearrange("p (k t) -> p k t", k=8), w=["hTo2"])
        DMA(zT2[:], zts.rearrange("p (k t) -> p k t", k=8), w=["zT2"], q="scalar")
        wm = [[ar.alloc([8, 128], BF16) for _ in range(4)] for _ in range(2)]
        sga = [ar.alloc([512], F32) for _ in range(2)]
        sgc = [ar.alloc([512], F32) for _ in range(2)]
        m1 = [ar.alloc([512], F32) for _ in range(2)]
        womla_v = womla.rearrange("(k p) f -> p k f", p=128)
        wpw_v = wpw.rearrange("(k p) f -> p k f", p=128)
        for c in range(8):
            ws = c % 2
            cs = slice(c * 128, (c + 1) * 128)
            load_w(wm[ws][0], womla_v[:, :, cs], 1024, ("wm", ws, 0))
            load_w(wm[ws][1], wpw_v[:, :, cs], 1024, ("wm", ws, 1))
            load_w(wm[ws][2], win_v[:, :, OFF_GATE + c * 128:OFF_GATE + (c + 1) * 128], 1024, ("wm", ws, 2))
            load_w(wm[ws][3], win_v[:, :, OFF_GATE + 1024 + c * 128:OFF_GATE + 1024 + (c + 1) * 128], 1024, ("wm", ws, 3))
            for tb in range(4):
                tsl = slice(tb * 512, (tb + 1) * 512)
                banks = (0, 1, 2, 3) if tb % 2 == 0 else (4, 5, 6, 7)
                srcs = [(OT, "OT"), (zT2, "zT2"), (hTo2, "hTo2"), (hTo2, "hTo2")]
                for i in range(4):
                    for k in range(8):
                        MM(ps[banks[i]][:], wm[ws][i][:, k, :], srcs[i][0][:, k, tsl], start=(k == 0), stop=(k == 7),
                           r=[("wm", ws, i), srcs[i][1]], w=[("ps", banks[i])])
                p2 = tb % 2
                ACT(sga[p2][:], ps[banks[2]][:], AF.Sigmoid, r=[("ps", banks[2])], w=[("sga", p2)])
                ACT(sgc[p2][:], ps[banks[3]][:], AF.Sigmoid, r=[("ps", banks[3])], w=[("sgc", p2)])
                TT("vector", m1[p2][:], ps[banks[0]][:], sga[p2][:], ALU.mult, r=[("ps", banks[0]), ("sga", p2)], w=[("m1", p2)])
                TT("vector", sgc[p2][:], ps[banks[1]][:], sgc[p2][:], ALU.mult, r=[("ps", banks[1]), ("sgc", p2)], w=[("sgc", p2)])
                TT("vector", mT[:, c, tsl], m1[p2][:], sgc[p2][:], ALU.add, r=[("m1", p2), ("sgc", p2)], w=[("mT", c, tb)])
        if stop_after == "M":
            P.emit()
            return nc
        BARRIER()

        ar.off = RB
        h2T = ar.alloc([8, NOWN], BF16)
        assert ar.off <= RC
        ar.off = RC
        woutb = ar.alloc([8, 1024], BF16)
        wout_v = wout.rearrange("(k p) f -> p k f", p=128)
        for k in range(8):
            load_w(woutb[:, k, :], wout_v[:, k, :], 1024, ("woutb", k))
        wok = [("woutb", k) for k in range(8)]
        xts = [ar.alloc([D], F32) for _ in range(2)]
        x1t = [ar.alloc([D], F32) for _ in range(2)]
        sqj = ar.alloc([D], F32)
        xns = [ar.alloc([D], BF16) for _ in range(2)]
        sss = [ar.alloc([1], F32) for _ in range(2)]
        rss = [ar.alloc([1], F32) for _ in range(2)]
        for tt in range(16):
            s = tt % 2
            DMA(xts[s][:], xo[tt], w=[("xo2", s)], q=dmaq[tt % 2])
            for half in range(2):
                bank = (4 + half) if s == 0 else (6 + half)
                hs = slice(half * 512, (half + 1) * 512)
                for k in range(8):
                    MM(ps[bank][:], mT[:, k, tt * 128:(tt + 1) * 128], woutb[:, k, hs], start=(k == 0), stop=(k == 7),
                       r=wok + ["mT"], w=[("ps", bank)])
                TT("vector", x1t[s][:, hs], ps[bank][:], g1row[:, hs], ALU.mult, r=[("ps", bank)], w=[("x1t", s, half)])
                TT("gpsimd", x1t[s][:, hs], x1t[s][:, hs], xts[s][:, hs], ALU.add, r=[("x1t", s, half), ("xo2", s)], w=[("x1t", s, half)])
            DMA(x1s[tt], x1t[s][:], r=[("x1t", s, 0), ("x1t", s, 1)], w=[("x1s", tt)])
            P.op("gpsimd", lambda e: e.memset(junk[:, 1:2], 0.0), reads=[("x1t", s, 0), ("x1t", s, 1)], writes=[(("n2", s), "xt")])
            norm_tile(None, h2T[:, :, tt * 128:(tt + 1) * 128], tt, A2[:], modT[:, 24:32, 0],
                      x1t[s][:], sqj[:], xns[s][:], sss[s][:], rss[s][:], 2 + s, ("n2", s), ("h2T", tt))
        if stop_after == "M2":
            P.emit()
            return nc
        BARRIER()

        ar.off = RA
        E1T = ar.alloc([NOWN], BF16)
        E2T = ar.alloc([NOWN], BF16)
        WT = ar.alloc([NOWN], F32)
        ar.off = RC
        subkb = ar.alloc([16, 128], BF16)
        wpqc = [ar.alloc([8, 128], BF16) for _ in range(2)]
        qT = ar.alloc([16, 512], BF16)
        scs = [ar.alloc([16, 128], F32) for _ in range(2)]
        sc2 = ar.alloc([16, 128], F32)
        stop_ = ar.alloc([16, 16], F32)
        itopu = ar.alloc([16, 16], U32)
        itopf = ar.alloc([16, 16], F32)
        cand = ar.alloc([8, 256], F32)
        cand2 = ar.alloc([8, 256], F32)
        best = ar.alloc([8, 16], F32)
        ciu = ar.alloc([8, 16], U32)
        cif = ar.alloc([8, 16], F32)
        c1f = ar.alloc([8, 16], F32)
        c2f = ar.alloc([8, 16], F32)
        eq = ar.alloc([8, 16, 16], F32)
        e1f = ar.alloc([8, 16], F32)
        e2f = ar.alloc([8, 16], F32)
        ex = ar.alloc([8, 16], F32)
        se = ar.alloc([8], F32)
        wg = ar.alloc([8, 16], F32)
        iota16 = ar.alloc([16], F32)
        thr16 = ar.alloc([16], F32)
        P.op("gpsimd", lambda e: e.iota(iota16[:], pattern=[[1, 16]], base=0, channel_multiplier=0,
                                        allow_small_or_imprecise_dtypes=True), writes=["iota16"])
        P.op("gpsimd", lambda e: e.iota(thr16[:], pattern=[[16, 16]], base=16, channel_multiplier=0,
                                        allow_small_or_imprecise_dtypes=True), writes=["thr16"])
        MEMSET("gpsimd", thr16[:, 15:16], 1.0e9, w=["thr16"])
        load_w(subkb[:, 0:8, :], subkT_d[:, 0:1024].rearrange("p (a b) -> p a b", b=128), 1024, ("subkb", 0))
        load_w(subkb[:, 8:16, :], subkT_d[:, 1024:2048].rearrange("p (a b) -> p a b", b=128), 1024, ("subkb", 1))
        wpq_v = wpq.rearrange("(k p) f -> p k f", p=128)
        itop_v = itopf[:].rearrange("p (h two) m -> p h two m", two=2)
        stop_v = stop_[:].rearrange("p (h two) m -> p h two m", two=2)
        B4 = [128, 8, 16, 16]
        for tb in range(4):
            for j in range(16):
                ws = j % 2
                load_w(wpqc[ws], wpq_v[:, :, j * 128:(j + 1) * 128], 1024, ("wpqc", ws))
                for k in range(8):
                    MM(ps[4 + ws][:], wpqc[ws][:, k, :], h2T[:, k, tb * 512:(tb + 1) * 512], start=(k == 0), stop=(k == 7),
                       r=[("wpqc", ws), "h2T"], w=[("ps", 4 + ws)])
                CP("vector" if j % 2 else "scalar", qT[:, j, :], ps[4 + ws][:], r=[("ps", 4 + ws)], w=[("qT", j)])
            def score(tt_):
                tl_ = tt_ % 4
                sc_ = scs[tt_ % 2]
                for j in range(16):
                    MM(ps[j // 4][:, (j % 4) * 128:(j % 4 + 1) * 128], qT[:, j, tl_ * 128:(tl_ + 1) * 128], subkb[:, j, :],
                       r=[("qT", j), ("subkb", 0), ("subkb", 1)], w=[("ps", j // 4)])
                for g in range(4):
                    CP("scalar", sc_[:, g * 4:(g + 1) * 4, :], ps[g][:].rearrange("p (a b) -> p a b", b=128), r=[("ps", g)],
                       w=[("sc", tt_ % 2, g)])

            score(tb * 4)
            for tl in range(4):
                tt = tb * 4 + tl
                if tl < 3:
                    score(tt + 1)
                sc = scs[tt % 2]
                for j in range(16):
                    g = j // 4
                    skey = ("sc", tt % 2, g)
                    P.op("vector", lambda e, j=j, sc=sc: e.max(out=stop_[:, j, 0:8], in_=sc[:, j, :]), reads=[skey], writes=[("stop", j)])
                    P.op("vector", lambda e, j=j, sc=sc: e.max_index(out=itopu[:, j, 0:8], in_max=stop_[:, j, 0:8], in_values=sc[:, j, :]),
                         reads=[skey, ("stop", j)], writes=[("itopu", j)])
                    P.op("vector", lambda e, j=j, sc=sc: e.match_replace(out=sc2[:, j, :], in_to_replace=stop_[:, j, 0:8], in_values=sc[:, j, :], imm_value=-1.0e30),
                         reads=[skey, ("stop", j)], writes=[("sc2", j)])
                    P.op("vector", lambda e, j=j: e.max(out=stop_[:, j, 8:16], in_=sc2[:, j, :]), reads=[("sc2", j)], writes=[("stop", j)])
                    P.op("vector", lambda e, j=j: e.max_index(out=itopu[:, j, 8:16], in_max=stop_[:, j, 8:16], in_values=sc2[:, j, :]),
                         reads=[("sc2", j), ("stop", j)], writes=[("itopu", j)])
                sk = [("stop", j) for j in range(16)]
                ik = [("itopu", j) for j in range(16)]
                CP("vector", itopf[:], itopu[:], r=ik, w=["itopf"])
                TT("vector", cand[:].rearrange("p h (a b) -> p h a b", b=16), stop_v[:, :, 0, :].unsqueeze(3).to_broadcast(B4),
                   stop_v[:, :, 1, :].unsqueeze(2).to_broadcast(B4), ALU.add, r=sk, w=["cand"])
                for h in range(8):
                    P.op("vector", lambda e, h=h: e.max(out=best[:, h, 0:8], in_=cand[:, h, :]), reads=["cand"], writes=[("best", h)])
                    P.op("vector", lambda e, h=h: e.max_index(out=ciu[:, h, 0:8], in_max=best[:, h, 0:8], in_values=cand[:, h, :]),
                         reads=["cand", ("best", h)], writes=[("ciu", h)])
                    P.op("vector", lambda e, h=h: e.match_replace(out=cand2[:, h, :], in_to_replace=best[:, h, 0:8], in_values=cand[:, h, :], imm_value=-1.0e30),
                         reads=["cand", ("best", h)], writes=[("cand2", h)])
                    P.op("vector", lambda e, h=h: e.max(out=best[:, h, 8:16], in_=cand2[:, h, :]), reads=[("cand2", h)], writes=[("best", h)])
                    P.op("vector", lambda e, h=h: e.max_index(out=ciu[:, h, 8:16], in_max=best[:, h, 8:16], in_values=cand2[:, h, :]),
                         reads=[("cand2", h), ("best", h)], writes=[("ciu", h)])
                bk = [("best", h) for h in range(8)]
                ck = [("ciu", h) for h in range(8)]
                CP("vector", cif[:], ciu[:], r=ck, w=["cif"])
                TT("vector", eq[:], cif[:].unsqueeze(3).to_broadcast(B4), thr16[:].unsqueeze(1).unsqueeze(1).to_broadcast(B4), ALU.is_ge,
                   r=["cif", "thr16"], w=["eq"])
                P.op("vector", lambda e: e.tensor_reduce(out=c1f[:], in_=eq[:], axis=AX.X, op=ALU.add), reads=["eq"], writes=["c1f"])
                STT(c2f[:], c1f[:], -16.0, cif[:], ALU.mult, ALU.add, r=["c1f", "cif"], w=["c2f"])
                for (cf_, two, ef_) in ((c1f, 0, e1f), (c2f, 1, e2f)):
                    TT("vector", eq[:], cf_[:].unsqueeze(3).to_broadcast(B4), iota16[:].unsqueeze(1).unsqueeze(1).to_broadcast(B4), ALU.is_equal,
                       r=["c1f", "c2f", "iota16"], w=["eq"])
                    TT("vector", eq[:], eq[:], itop_v[:, :, two, :].unsqueeze(2).to_broadcast(B4), ALU.mult, r=["eq", "itopf"], w=["eq"])
                    P.op("vector", lambda e, ef_=ef_: e.tensor_reduce(out=ef_[:], in_=eq[:], axis=AX.X, op=ALU.add), reads=["eq"], writes=[("ef", two)])
                TT("vector", ex[:], best[:], best[:, :, 0:1].to_broadcast([128, 8, 16]), ALU.subtract, r=bk, w=["ex"])
                ACT(ex[:], ex[:], AF.Exp, r=["ex"], w=["ex"])
                P.op("vector", lambda e: e.tensor_reduce(out=se[:], in_=ex[:], axis=AX.X, op=ALU.add), reads=["ex"], writes=["se"])
                RECIP(se[:], se[:], r=["se"], w=["se"])
                TT("vector", wg[:], ex[:], se[:].unsqueeze(2).to_broadcast([128, 8, 16]), ALU.mult, r=["ex", "se"], w=["wg"])
                tsl = slice(tt * 128, (tt + 1) * 128)
                TR(ps[6][:, 0:128], e1f[:].rearrange("p a b -> p (a b)"), identf[:], r=[("ef", 0)], w=[("ps", 6)])
                TR(ps[6][:, 128:256], e2f[:].rearrange("p a b -> p (a b)"), identf[:], r=[("ef", 1)], w=[("ps", 6)])
                TR(ps[6][:, 256:384], wg[:].rearrange("p a b -> p (a b)"), identf[:], r=["wg"], w=[("ps", 6)])
                CP("scalar", E1T[:, tsl], ps[6][:, 0:128], r=[("ps", 6)], w=[("E1T", tt)])
                CP("scalar", E2T[:, tsl], ps[6][:, 128:256], r=[("ps", 6)], w=[("E2T", tt)])
                CP("scalar", WT[:, tsl], ps[6][:, 256:384], r=[("ps", 6)], w=[("WT", tt)])
        if stop_after == "P0":
            P.emit()
            return nc
        BARRIER()

        ar.off = RA + 16 * 1024
        E1Tm = ar.alloc([NOWN], BF16)
        iota3 = ar.alloc([8, 128], BF16)
        iotaH = ar.alloc([8, 64], BF16)
        xfin = ar.alloc([D], F32)
        x1r = ar.alloc([D], F32)
        assert ar.off <= RB
        ar.off = RC
        Wh = [ar.alloc([256, 64], BF16) for _ in range(2)]
        NSLOT = 3
        UTb = [ar.alloc([2, 1024], BF16) for _ in range(NSLOT)]
        Vb = [ar.alloc([2, 1024], BF16) for _ in range(NSLOT)]
        Gs = [ar.alloc([256], BF16) for _ in range(3)]
        GWs = [ar.alloc([256], BF16) for _ in range(3)]
        ssf = ar.alloc([1], F32)
        E1p = [ar.alloc([8, 64], BF16) for _ in range(2)]
        E2p = [ar.alloc([8, 128], BF16) for _ in range(2)]
        xsq = x1r.bitcast(BF16)[:, 0:D]
        P.op("gpsimd", lambda e: e.iota(iota3[:], pattern=[[0, 8], [1, 128]], base=0, channel_multiplier=0,
                                        allow_small_or_imprecise_dtypes=True), writes=["iota3"])
        P.op("gpsimd", lambda e: e.iota(iotaH[:], pattern=[[0, 8], [1, 64]], base=0, channel_multiplier=0,
                                        allow_small_or_imprecise_dtypes=True), writes=["iotaH"])
        TS("vector", E1Tm[:], E1T[:], -64.0, None, ALU.add, r=[], w=["E1Tm"])

        def b_eq(tb_, hf_, m):
            t0 = tb_ * 256 + m * 8
            bi = m % 2
            e1src = E1T if hf_ == 0 else E1Tm
            TT("vector", E1p[bi][:], iotaH[:], e1src[:, t0:t0 + 8].unsqueeze(2).to_broadcast([128, 8, 64]), ALU.is_equal,
               r=["iotaH", "E1Tm"], w=[("E1p", bi)])
            TT("vector", E2p[bi][:], iota3[:], E2T[:, t0:t0 + 8].unsqueeze(2).to_broadcast([128, 8, 128]), ALU.is_equal,
               r=["iota3"], w=[("E2p", bi)])

        def b_mul(tb_, hf_, m):
            t0 = tb_ * 256 + m * 8
            bi = m % 2
            TT("gpsimd", E2p[bi][:], E2p[bi][:], WT[:, t0:t0 + 8].unsqueeze(2).to_broadcast([128, 8, 128]), ALU.mult,
               r=[("E2p", bi)], w=[("E2p", bi)])

        def b_mm(tb_, hf_, m):
            bi = m % 2
            for tq in range(8):
                MM(ps[6][:, tq * 64:(tq + 1) * 64], E2p[bi][:, tq, :], E1p[bi][:, tq, :], r=[("E1p", bi), ("E2p", bi)], w=[("ps", 6)])
            CP("scalar", Wh[hf_][:, m * 8:m * 8 + 8, :], ps[6][:].rearrange("p (a b) -> p a b", b=64), r=[("ps", 6)], w=[("Wh", hf_)])

        def loadg(cg):
            s = cg % NSLOT
            DMA(UTb[s][:].rearrange("p a b -> p (a b)"), utb[cg], w=[("UTb", s)], q="sync")
            DMA(Vb[s][:].rearrange("p a b -> p (a b)"), vtb[cg], w=[("Vb", s)], q="sync")

        for m in range(32):
            b_eq(0, 0, m)
            b_mul(0, 0, m)
            b_mm(0, 0, m)
        for tb in range(8):
            T0 = tb * 256
            ABANK = (4, 5, 7)

            def Amm(i):
                s = (i // 2) % NSLOT
                ab = i % 3
                for k in range(8):
                    MM(ps[ABANK[ab]][:, 0:256], UTb[s][:, i % 2, k * 128:(k + 1) * 128], h2T[:, k, T0:T0 + 256], start=(k == 0), stop=(k == 7),
                       r=[("UTb", s), "h2T"], w=[("ps", ABANK[ab])])

            for c0 in range(NSLOT):
                loadg(c0)
            Amm(0)
            Amm(1)
            for i in range(128):
                hf2 = i // 64
                if i + 2 < 128:
                    Amm(i + 2)
                cg = i // 2
                s = cg % NSLOT
                ab = i % 3
                nxt = (tb, 1) if hf2 == 0 else ((tb + 1, 0) if tb + 1 < 8 else None)
                m = (i % 64) // 2
                if nxt is not None and i % 2 == 0 and m >= 1:
                    b_mm(nxt[0], nxt[1], m - 1)
                ACT(Gs[ab][:], ps[ABANK[ab]][:, 0:256], AF.Gelu, r=[("ps", ABANK[ab])], w=[("Gs", ab)])
                TT("vector", GWs[ab][:], Gs[ab][:], Wh[hf2][:, :, i - 64 * hf2], ALU.mult, r=[("Gs", ab), ("Wh", hf2)], w=[("GWs", ab)])
                for tl in range(2):
                    for half in range(2):
                        MM(ps[tl * 2 + half][:], GWs[ab][:, tl * 128:(tl + 1) * 128], Vb[s][:, i % 2, half * 512:(half + 1) * 512],
                           start=(i == 0), stop=(i == 127), r=[("GWs", ab), ("Vb", s)], w=[("ps", tl * 2 + half)])
                if i % 2 == 1 and cg + NSLOT < 64:
                    loadg(cg + NSLOT)
                if nxt is not None:
                    if i % 2 == 0:
                        b_eq(nxt[0], nxt[1], m)
                    else:
                        b_mul(nxt[0], nxt[1], m)
                        if m == 31:
                            b_mm(nxt[0], nxt[1], 31)
            for tl in range(2):
                tt = tb * 2 + tl
                DMA(x1r[:], x1s[tt], w=["x1r"])
                for half in range(2):
                    hs = slice(half * 512, (half + 1) * 512)
                    TT("vector", xfin[:, hs], ps[tl * 2 + half][:], g2row[:, hs], ALU.mult, r=[("ps", tl * 2 + half)], w=["xfin"])
                TT("vector", xfin[:], xfin[:], x1r[:], ALU.add, r=["xfin", "x1r"], w=["xfin"])
                ACT(xsq, xfin[:], AF.Square, r=["xfin"], w=["x1r", "ssf"], accum=ssf[:])
                ACT(ssf[:], ssf[:], AF.Sqrt, r=["ssf"], w=["ssf"], scale=1.0 / D, bias=epsb[:])
                RECIP(ssf[:], ssf[:], r=["ssf"], w=["ssf"])
                STT(xfin[:], xfin[:], ssf[:, 0:1], fgrow[:], ALU.mult, ALU.mult, r=["xfin", "ssf"], w=["xfin"])
                DMA(out_d[tt], xfin[:], r=["xfin"], w=[("out", tt)], sem_key="out")

        P.emit()
    return nc


def _rope_tabs(pos):
    n_freq = 8
    freq = (np.float32(10000.0) ** (-np.arange(n_freq, dtype=np.float32) / np.float32(n_freq))).astype(np.float32)
    r = (pos // 64).astype(np.float32)
    c = (pos % 64).astype(np.float32)
    ang = np.stack([r[:, None] * freq[None, :], c[:, None] * freq[None, :]], axis=1).astype(np.float32)
    cos = np.cos(ang).astype(np.float32)
    sin = np.sin(ang).astype(np.float32)
    ct = np.zeros((32, pos.shape[0]), np.float32)
    stb = np.zeros((32, pos.shape[0]), np.float32)
    perm = np.zeros(32, np.int64)
    for a in range(2):
        for half in range(2):
            for f in range(8):
                d = a * 16 + half * 8 + f
                perm[d] = a * 16 + (1 - half) * 8 + f
                ct[d] = cos[:, a, f]
                stb[d] = -sin[:, a, f] if half == 0 else sin[:, a, f]
    return ct, stb, perm


def prep_inputs(inp):
    f = lambda a: np.ascontiguousarray(np.asarray(a, dtype=np.float32))
    x, c, ctx, c_ctx = f(inp["x"]), f(inp["c"]), f(inp["ctx"]), f(inp["c_ctx"])
    w_in = f(inp["w_in"])[0]
    _, _, perm = _rope_tabs(np.arange(4))
    shared = {}
    shared["wmod"] = f(inp["w_mod"])[0]
    b_mod = f(inp["b_mod"])[0]
    shared["bmodg"] = np.ascontiguousarray(np.broadcast_to(np.concatenate([b_mod[2048:3072], b_mod[5120:6144]])[None, :], (128, 2048)))
    shared["fgrow"] = np.ascontiguousarray(np.broadcast_to(f(inp["final_g"])[None, :], (128, D)))
    shared["win"] = w_in
    wkr = np.zeros((D, 192), np.float32)
    wkr[:, 64:96] = w_in[:, OFF_KR:OFF_KR + 32]
    wkr[:, 160:192] = w_in[:, OFF_KR + perm]
    shared["wkr"] = wkr
    w_uq = f(inp["w_uq"])[0]
    shared["wuq"] = w_uq
    colp = np.arange(1536).reshape(16, 96).copy()
    for h in range(16):
        colp[h, 64:96] = h * 96 + 64 + perm
    shared["wuqp"] = np.ascontiguousarray(w_uq[:, colp.reshape(-1)])
    shared["wukv"] = f(inp["w_ukv"])[0]
    shared["womla"] = f(inp["w_o_mla"])[0]
    shared["wpw"] = f(inp["w_pw"])[0]
    shared["wout"] = f(inp["w_out"])[0]
    shared["wpq"] = f(inp["w_pq"])[0]
    sk = f(inp["sub_keys"])[0].reshape(16, 128, 128)
    shared["subkT"] = np.ascontiguousarray(sk.transpose(2, 0, 1).reshape(128, 16 * 128))
    u = f(inp["u_experts"])[0]
    shared["ut"] = np.ascontiguousarray(u.reshape(128, 128, 8, 128).transpose(0, 3, 2, 1).reshape(128, 128, 1024))
    shared["vt"] = f(inp["v_experts"])[0].reshape(128, 128, 1024)
    ck, sk_, _ = _rope_tabs(np.arange(SEQ))
    ropek = np.zeros((2, 128, SEQ), np.float32)
    ropek[0, 64:96] = ck
    ropek[1, 64:96] = sk_
    shared["ropek"] = ropek

    def tomaj(v):
        return v.reshape(-1, 128).T

    maps = []
    for core in range(8):
        b, hf = core // 2, core % 2
        m = dict(shared)
        m["xk"] = np.ascontiguousarray(np.concatenate([x[b], ctx[b]], axis=0).reshape(34, 128, D))
        xo = np.zeros((17 * 128, D), np.float32)
        lo = hf * NOWN
        xo[:NOWN] = x[b, lo:lo + NOWN]
        if hf == 1:
            xo[NOWN:NOWN + 15] = x[b, lo - 15:lo]
        else:
            xo[NOWN + 15:NOWN + 30] = x[b, lo + NOWN:lo + NOWN + 15]
        m["xo"] = xo.reshape(17, 128, D)
        vecs = np.zeros((128, NV), np.float32)
        vecs[:, 0:8] = tomaj(f(inp["norm1_g"])[0])
        vecs[:, 8:16] = tomaj(f(inp["norm2_g"])[0])
        vecs[:, 16:18] = tomaj(f(inp["q_norm_g"])[0])
        vecs[:, 18:19] = tomaj(f(inp["kv_norm_g"])[0])
        vecs[:, 19:27] = tomaj(f(inp["conv_b"])[0])
        vecs[:, 27:35] = tomaj(f(inp["conv_ln_g"])[0])
        vecs[:, 35:43] = tomaj(f(inp["conv_ln_b"])[0])
        cw = f(inp["conv_w"])[0]
        vecs[:, 43:291] = cw.reshape(31, 8, 128).transpose(2, 1, 0).reshape(128, 248)
        vecs[:, 291:339] = tomaj(b_mod)
        ct2 = np.stack([tomaj(c[b]), tomaj(c_ctx)], axis=2)
        vecs[:, 339:355] = ct2.reshape(128, 16)
        vecs[:, 355] = 1.0 if hf == 1 else 0.0
        vecs[:, 356] = 1.0 if hf == 0 else 0.0
        m["vecs"] = vecs
        cq_, sq_, _ = _rope_tabs(np.arange(lo, lo + NOWN))
        ropeq = np.zeros((2, 128, NOWN), np.float32)
        ropeq[0, 0:64] = 1.0
        ropeq[0, 64:96] = cq_
        ropeq[1, 64:96] = sq_
        m["ropeq"] = ropeq
        maps.append(m)
    return maps


_NC_CACHE = {}


def kernel(**inputs):
    maps = prep_inputs(inputs)
    if "nc" not in _NC_CACHE:
        _NC_CACHE["nc"] = build_nc()
    nc = _NC_CACHE["nc"]
    res = run_bass_kernel_spmd(nc, maps, core_ids=list(range(8)))
    out = np.zeros((4, SEQ, D), np.float32)
    for core in range(8):
        b, hf = core // 2, core % 2
        out[b, hf * NOWN:(hf + 1) * NOWN] = np.asarray(res.results[core]["out"]).reshape(NOWN, D)
    return out
```

```python
import numpy as np
from contextlib import ExitStack
import concourse.bass as bass
import concourse.mybir as mybir
from concourse.bass_utils import run_bass_kernel_spmd

F32 = mybir.dt.float32
BF16 = mybir.dt.bfloat16
U32 = mybir.dt.uint32
ALU = mybir.AluOpType
AF = mybir.ActivationFunctionType
AX = mybir.AxisListType
ENGS = ("tensor", "vector", "scalar", "gpsimd", "sync")

D = 1024
SEQ = 4096
CTX = 256
NKEY = SEQ + CTX
NOWN = 2048
NH = 16
EPS = 1e-6
ATTN_SCALE = 96 ** -0.5
OFF_KV = 256
OFF_KR = 384
OFF_CONV = 416
OFF_GATE = 2464
NV = 360
CUT = 0


class _Op:
    __slots__ = ("eng", "fn", "waits", "signal", "sem", "val", "is_dma", "inc")

    def __init__(self, eng, fn):
        self.eng = eng
        self.fn = fn
        self.waits = []
        self.signal = False
        self.sem = None
        self.val = 0
        self.is_dma = False
        self.inc = 1


class Prog:
    def __init__(self, nc):
        self.nc = nc
        self.ops = {e: [] for e in ENGS}
        self.last_w = {}
        self.readers = {}
        self.all_ops = []
        self.epoch = None
        self.last_by_sem = {}

    def op(self, eng, fn, reads=(), writes=(), dma=False, sem_key=None):
        o = _Op(eng, fn)
        o.is_dma = dma
        deps = []
        for r in reads:
            w = self.last_w.get(r, self.epoch)
            if w is not None:
                deps.append((w, True))
        for r in writes:
            w = self.last_w.get(r, self.epoch)
            if w is not None:
                deps.append((w, False))
            for rd in self.readers.get(r, {}).values():
                deps.append((rd, False))
        seen = set()
        for d, raw in deps:
            if id(d) in seen:
                continue
            same = (not d.is_dma) and (not dma) and d.eng == eng
            if same and eng == "tensor":
                continue
            seen.add(id(d))
            o.waits.append(d)
            d.signal = True
        if dma:
            o.sem = ("dma", sem_key if sem_key is not None else (writes[0] if writes else reads[0]))
            o.inc = 16
            o.signal = True
        else:
            o.sem = ("eng", eng)
        for r in reads:
            self.readers.setdefault(r, {})[o.sem] = o
        for r in writes:
            self.last_w[r] = o
            self.readers[r] = {}
        self.ops[eng].append(o)
        self.all_ops.append(o)
        self.last_by_sem[o.sem] = o
        return o

    def barrier(self, fn):
        o = _Op("gpsimd", fn)
        o.sem = ("eng", "gpsimd")
        for d in self.last_by_sem.values():
            if d.eng == "gpsimd" and not d.is_dma:
                continue
            o.waits.append(d)
            d.signal = True
        o.signal = True
        self.ops["gpsimd"].append(o)
        self.all_ops.append(o)
        self.last_by_sem[o.sem] = o
        self.last_w = {}
        self.readers = {}
        self.epoch = o

    def emit(self, final_wait_eng="sync"):
        nc = self.nc
        last_dma = {}
        for o in self.all_ops:
            if o.is_dma:
                last_dma[o.sem] = o
        counters = {}
        for o in self.all_ops:
            if o.signal:
                counters[o.sem] = counters.get(o.sem, 0) + o.inc
                o.val = counters[o.sem]
        sem_keys = list(counters.keys())
        self.n_sems = len(sem_keys)
        with ExitStack() as st:
            sems = {}
            for i, k in enumerate(sem_keys):
                sems[k] = st.enter_context(nc.semaphore("s%d" % i))
            block = st.enter_context(nc.Block())

            def make(engname):
                ops = self.ops[engname]

                def body(e):
                    waited = {}
                    for o in ops:
                        for d in o.waits:
                            if waited.get(d.sem, 0) >= d.val:
                                continue
                            e.wait_ge(sems[d.sem], d.val)
                            waited[d.sem] = d.val
                        inst = o.fn(e)
                        if o.signal:
                            inst.then_inc(sems[o.sem], o.inc)
                    if engname == final_wait_eng:
                        for k, o in last_dma.items():
                            if waited.get(k, 0) < o.val:
                                e.wait_ge(sems[k], o.val)
                return body

            for engname in ENGS:
                if not self.ops[engname] and engname != final_wait_eng:
                    continue
                getattr(block, engname)(make(engname))


class Arena:
    def __init__(self, tensor, nbytes):
        self.t = tensor
        self.n = nbytes
        self.off = 0

    def alloc(self, shape_free, dt):
        es = 4 if dt in (F32, U32) else 2
        n = int(np.prod(shape_free)) * es
        n_al = (n + 63) // 64 * 64
        assert self.off + n_al <= self.n, ("arena overflow", self.off, n_al, self.n)
        v = self.t[:, self.off // 2:(self.off + n) // 2]
        self.off += n_al
        if es == 4:
            v = v.bitcast(dt)
        if len(shape_free) == 2:
            v = v.rearrange("p (a b) -> p a b", b=shape_free[1])
        elif len(shape_free) == 3:
            v = v.rearrange("p (a b c) -> p a b c", b=shape_free[1], c=shape_free[2])
        return v

    def mark(self):
        return self.off

    def release(self, m):
        self.off = m


def build_nc(stop_after=None, debug=False):
    nc = bass.Bass("TRN2", target_bir_lowering=False)

    def din(name, shape, dt=F32):
        return nc.dram_tensor(name, list(shape), dt, kind="ExternalInput").ap()

    xk = din("xk", [34, 128, D])
    xo = din("xo", [17, 128, D])
    vecs_d = din("vecs", [128, NV])
    wmod = din("wmod", [D, 6 * D])
    bmodg = din("bmodg", [128, 2048])
    fgrow_d = din("fgrow", [128, D])
    win = din("win", [D, 4512])
    wkr = din("wkr", [D, 192])
    wuq = din("wuq", [256, 1536])
    wuqp = din("wuqp", [256, 1536])
    wukv = din("wukv", [128, 2048])
    womla = din("womla", [D, D])
    wpw = din("wpw", [D, D])
    wout = din("wout", [D, D])
    wpq = din("wpq", [D, 2048])
    subkT_d = din("subkT", [128, 16 * 128])
    ut = din("ut", [128, 128, 1024])
    vt = din("vt", [128, 128, 1024])
    ropek = din("ropek", [2, 128, SEQ])
    ropeq = din("ropeq", [2, 128, NOWN])
    out_d = nc.dram_tensor("out", [16, 128, D], F32, kind="ExternalOutput").ap()
    utb = nc.dram_tensor("utb", [64, 128, 2048], BF16, kind="Internal").ap()
    vtb = nc.dram_tensor("vtb", [64, 128, 2048], BF16, kind="Internal").ap()
    x1s = nc.dram_tensor("x1s", [16, 128, D], F32, kind="Internal").ap()
    hts = nc.dram_tensor("hts", [128, 8 * NOWN], BF16, kind="Internal").ap()
    zts = nc.dram_tensor("zts", [128, 8 * NOWN], BF16, kind="Internal").ap()
    dbg = {}
    if debug:
        for nm, shp in debug.items():
            dt_ = F32
            if isinstance(shp, tuple) and len(shp) == 2 and shp[1] == "bf16":
                shp, dt_ = shp[0], BF16
            dbg[nm] = nc.dram_tensor("dbg_" + nm, list(shp), dt_, kind="ExternalOutput").ap()

    st = ExitStack()
    with st:
        def sbuf(name, shape, dt):
            return st.enter_context(nc.sbuf_tensor("sb_" + name, list(shape), dt))

        ARENA_BYTES = 172 * 1024
        arena_t = sbuf("arena", [128, ARENA_BYTES // 2], BF16)
        ar = Arena(arena_t, ARENA_BYTES)
        identb = sbuf("identb", [128, 128], BF16)
        identf = sbuf("identf", [128, 128], F32)
        onesb = sbuf("onesb", [128, 128], BF16)
        onesf = sbuf("onesf", [128, 128], F32)
        vecs = sbuf("vecs", [128, NV], F32)
        scT = sbuf("scT", [128, 8, 2], F32)
        screp = sbuf("screp", [128, 8, 128], F32)
        modT = sbuf("modT", [128, 48, 2], F32)
        A1 = sbuf("A1", [128, 8, 2], F32)
        A2 = sbuf("A2", [128, 8], F32)
        g1row = sbuf("g1row", [128, D], F32)
        g2row = sbuf("g2row", [128, D], F32)
        fgrow = sbuf("fgrow", [128, D], F32)
        stage = sbuf("stage", [128, 2, 1024], F32)
        junk = sbuf("junk", [128, 8], F32)
        ps = [st.enter_context(nc.psum_tensor("ps%d" % i, [128, 512], F32)) for i in range(8)]

        P = Prog(nc)

        def MM(out, lhsT, rhs, start=True, stop=True, r=(), w=()):
            P.op("tensor", lambda e: e.matmul(out, lhsT=lhsT, rhs=rhs, start=start, stop=stop), reads=r, writes=w)

        def TR(out, in_, ident, r=(), w=()):
            P.op("tensor", lambda e: e.transpose(out=out, in_=in_, identity=ident), reads=r, writes=w)

        def ACT(out, in_, func, r=(), w=(), scale=None, bias=None, accum=None):
            kw = {}
            if scale is not None:
                kw["scale"] = scale
            if bias is not None:
                kw["bias"] = bias
            if accum is not None:
                kw["accum_out"] = accum
            P.op("scalar", lambda e: e.activation(out=out, in_=in_, func=func, **kw), reads=r, writes=w)

        def TT(eng, out, in0, in1, op, r=(), w=()):
            P.op(eng, lambda e: e.tensor_tensor(out=out, in0=in0, in1=in1, op=op), reads=r, writes=w)

        def TS(eng, out, in0, s1, s2, op0, op1=None, r=(), w=()):
            if op1 is None:
                P.op(eng, lambda e: e.tensor_scalar(out=out, in0=in0, scalar1=s1, scalar2=None, op0=op0), reads=r, writes=w)
            else:
                P.op(eng, lambda e: e.tensor_scalar(out=out, in0=in0, scalar1=s1, scalar2=s2, op0=op0, op1=op1), reads=r, writes=w)

        def STT(out, in0, scalar, in1, op0, op1, r=(), w=()):
            P.op("vector", lambda e: e.scalar_tensor_tensor(out=out, in0=in0, scalar=scalar, in1=in1, op0=op0, op1=op1), reads=r, writes=w)

        def CP(eng, out, in_, r=(), w=()):
            if eng == "scalar":
                P.op(eng, lambda e: e.copy(out=out, in_=in_), reads=r, writes=w)
            else:
                P.op(eng, lambda e: e.tensor_copy(out=out, in_=in_), reads=r, writes=w)

        def RECIP(out, in_, r=(), w=()):
            P.op("vector", lambda e: e.reciprocal(out=out, in_=in_), reads=r, writes=w)

        def MEMSET(eng, ap, val, w=()):
            P.op(eng, lambda e: e.memset(ap, val), writes=w)

        dmaq = ["sync", "scalar"]
        dma_i = [0]

        def DMA(out, in_, r=(), w=(), q=None, sem_key=None):
            if q is None:
                q = "sync"
            P.op(q, lambda e: e.dma_start(out=out, in_=in_), reads=r, writes=w, dma=True, sem_key=sem_key)

        def BARRIER():
            P.barrier(lambda e: e.memset(junk[:, 0:1], 0.0))
            ar.off = RC

        RA, RB, RC = 0, 34 * 1024, 67 * 1024
        stage_i = [0]
        cv_i = [0]

        def load_w(dst, src, ncols, wkey, conv_eng=None):
            assert ncols <= 1024
            s = stage_i[0] % 2
            stage_i[0] += 1
            sv = stage[:, s, 0:ncols]
            if len(src.shape) == 3:
                sv = sv.rearrange("p (a b) -> p a b", b=src.shape[2])
            DMA(sv, src, w=[("stage", s)])
            if conv_eng is None:
                conv_eng = ("gpsimd", "vector")[cv_i[0] % 2]
                cv_i[0] += 1
            CP(conv_eng, dst, sv, r=[("stage", s)], w=[wkey])

        def dump(name, src, r):
            if debug and name in dbg:
                DMA(dbg[name], src, r=r, w=[("dbg", name)])

        MEMSET("gpsimd", identf[:], 0.0, w=["identf"])
        P.op("gpsimd", lambda e: e.affine_select(out=identf[:], in_=identf[:], pattern=[[-1, 128]], base=0,
                                                 channel_multiplier=1, compare_op=ALU.not_equal, fill=1.0),
             reads=["identf"], writes=["identf"])
        CP("vector", identb[:], identf[:], r=["identf"], w=["identb"])
        MEMSET("vector", onesb[:], 1.0, w=["onesb"])
        MEMSET("vector", onesf[:], 1.0, w=["onesf"])
        DMA(vecs[:], vecs_d, w=["vecs"])
        DMA(fgrow[:], fgrow_d, w=["fgrow"])
        epsb = sbuf("epsb", [128, 1], F32)
        MEMSET("vector", epsb[:], EPS, w=["epsb"])
        MEMSET("vector", modT[:], 0.0, w=["modT"])
        V_N1, V_N2, V_QG, V_KVG, V_CB, V_LNG, V_LNB, V_CW, V_BM, V_CT, V_HM = 0, 8, 16, 18, 19, 27, 35, 43, 291, 339, 355

        cT = vecs[:, V_CT:V_CT + 16].rearrange("p (k j) -> p k j", j=2)
        ACT(scT[:], cT, AF.Silu, r=["vecs"], w=["scT"])
        CP("vector", screp[:], scT[:, :, 0:1].to_broadcast([128, 8, 128]), r=["scT"], w=["screp"])
        ar.off = RC
        wmb = [ar.alloc([8, 512], F32) for _ in range(2)]
        bg = ar.alloc([2048], F32)
        DMA(bg, bmodg, w=["bg"])
        wmod_v = wmod.rearrange("(k p) f -> p k f", p=128)
        for blk in range(12):
            s = blk % 2
            DMA(wmb[s], wmod_v[:, :, blk * 512:(blk + 1) * 512], w=[("wmb", s)], q=dmaq[blk % 2])
            if blk in (4, 5, 10, 11):
                row = g1row if blk < 6 else g2row
                half = blk % 2
                for k in range(8):
                    MM(ps[0][:], screp[:, k, :], wmb[s][:, k, :], start=(k == 0), stop=(k == 7),
                       r=["screp", ("wmb", s)], w=[("ps", 0)])
                goff = (0 if blk < 6 else 1024) + half * 512
                TT("vector", row[:, half * 512:(half + 1) * 512], ps[0][:], bg[:, goff:goff + 512], ALU.add,
                   r=[("ps", 0), "bg"], w=[("row", blk)])
            else:
                for fc in range(4):
                    for k in range(8):
                        MM(ps[1][:, fc * 2:fc * 2 + 2], wmb[s][:, k, fc * 128:(fc + 1) * 128], scT[:, k, :],
                           start=(k == 0), stop=(k == 7), r=["scT", ("wmb", s)], w=[("ps", 1)])
                for fc in range(4):
                    f = blk * 4 + fc
                    TS("vector", modT[:, f, :], ps[1][:, fc * 2:fc * 2 + 2], vecs[:, V_BM + f:V_BM + f + 1], None, ALU.add,
                       r=[("ps", 1), "vecs"], w=["modT"])
        for j in range(2):
            STT(A1[:, :, j], modT[:, 8:16, j], 1.0, vecs[:, V_N1:V_N1 + 8], ALU.add, ALU.mult, r=["modT", "vecs"], w=["A1"])
        STT(A2[:], modT[:, 32:40, 0], 1.0, vecs[:, V_N2:V_N2 + 8], ALU.add, ALU.mult, r=["modT", "vecs"], w=["A2"])
        dump("modT", modT[:].rearrange("p a b -> p (a b)"), ["modT"])
        dump("g1row", g1row[:], [("row", 4), ("row", 5)])
        if stop_after == "mod":
            P.emit()
            return nc
        BARRIER()

        ar.off = RA
        ckvnT = ar.alloc([NKEY], BF16)
        KT = [ar.alloc([NKEY], BF16) for _ in range(2)]
        cqnT = ar.alloc([2, NOWN], BF16)
        assert ar.off <= RB
        ar.off = RC

        def norm_tile(src_tile, dst, j, Asc, Bsh, xt, sq, xn, ss, rs, psb, tag, dkey):
            if src_tile is not None:
                DMA(xt, src_tile, w=[(tag, "xt")], q=dmaq[j % 2])
            if CUT == 5:
                return
            ACT(sq, xt, AF.Square, r=[(tag, "xt")], w=[(tag, "sq"), (tag, "ss")], accum=ss)
            if CUT == 6:
                return
            ACT(rs, ss, AF.Sqrt, r=[(tag, "ss")], w=[(tag, "rs")], scale=1.0 / D, bias=epsb[:])
            if CUT == 7:
                return
            RECIP(rs, rs, r=[(tag, "rs")], w=[(tag, "rs")])
            TS("vector", xn, xt, rs, None, ALU.mult, r=[(tag, "xt"), (tag, "rs")], w=[(tag, "xn")])
            if CUT == 8:
                return
            pb = ps[psb][:].bitcast(BF16)
            for k in range(8):
                TR(pb[:, k * 128:(k + 1) * 128], xn[:, k * 128:(k + 1) * 128], identb[:], r=[(tag, "xn"), "identb"], w=[("ps", psb)])
            if CUT == 9:
                return
            pb3 = pb.rearrange("p (k t) -> p k t", k=8)
            tmpn = tmpns[j % 2]
            TT("vector", tmpn[:], pb3, Asc.unsqueeze(2).to_broadcast([128, 8, 128]), ALU.mult,
               r=[("ps", psb), "A1", "A2", "modT"], w=[("tmpn", j % 2)])
            TT("gpsimd", dst, tmpn[:], Bsh.unsqueeze(2).to_broadcast([128, 8, 128]), ALU.add,
               r=[("tmpn", j % 2), "A1", "A2", "modT"], w=[(dkey, 0)])

        tmpns = [sbuf("tmpn%d" % i, [128, 8, 128], F32) for i in range(2)]

        xts = [ar.alloc([D], F32) for _ in range(2)]
        sqj = ar.alloc([D], F32)
        xns = [ar.alloc([D], BF16) for _ in range(2)]
        sss = [ar.alloc([1], F32) for _ in range(2)]
        rss = [ar.alloc([1], F32) for _ in range(2)]
        hTb = [ar.alloc([8, 512], BF16) for _ in range(2)]
        wkvb = ar.alloc([8, 320], BF16)
        sqb = ar.alloc([512], BF16)
        rstd = ar.alloc([512], F32)
        rk = [ar.alloc([2, 512], F32) for _ in range(2)]
        t1 = ar.alloc([512], F32)
        t2 = ar.alloc([512], F32)
        win_v = win.rearrange("(k p) f -> p k f", p=128)
        wkr_v = wkr.rearrange("(k p) f -> p k f", p=128)
        load_w(wkvb[:, :, 0:128], win_v[:, :, OFF_KV:OFF_KV + 128], 8 * 128, ("wkvb", 0))
        load_w(wkvb[:, 0:4, 128:320], wkr_v[:, 0:4, :], 4 * 192, ("wkvb", 1))
        load_w(wkvb[:, 4:8, 128:320], wkr_v[:, 4:8, :], 4 * 192, ("wkvb", 2))
        ti = 0
        if CUT == 1:
            P.emit()
            return nc
        for blk in range(9):
            ntile = 4 if blk < 8 else 2
            n = ntile * 128
            hb = hTb[blk % 2]
            jm = 0 if blk < 8 else 1
            for tl in range(ntile):
                t = blk * 4 + tl
                s = ti % 2
                norm_tile(xk[t], hb[:, :, tl * 128:(tl + 1) * 128], ti, A1[:, :, jm], modT[:, 0:8, jm],
                          xts[s], sqj, xns[s], sss[s], rss[s], 2 + s, ("nk", s), ("hTb", blk % 2, s))
                ti += 1
            hr = [(("hTb", blk % 2, s_), 0) for s_ in range(2)]
            if CUT == 2 or CUT >= 5:
                P.emit()
                return nc
            for k in range(8):
                MM(ps[4][:, 0:n], wkvb[:, k, 0:128], hb[:, k, 0:n], start=(k == 0), stop=(k == 7),
                   r=hr + [("wkvb", 0)], w=[("ps", 4)])
            for k in range(8):
                MM(ps[5][0:96, 0:n], wkvb[:, k, 128:224], hb[:, k, 0:n], start=(k == 0), stop=(k == 7),
                   r=hr + [("wkvb", 1), ("wkvb", 2)], w=[("ps", 5)])
            if blk < 8:
                for k in range(8):
                    MM(ps[6][0:96, 0:n], wkvb[:, k, 224:320], hb[:, k, 0:n], start=(k == 0), stop=(k == 7),
                       r=hr + [("wkvb", 1), ("wkvb", 2)], w=[("ps", 6)])
            if CUT == 3:
                P.emit()
                return nc
            ACT(sqb[:, 0:n], ps[4][:, 0:n], AF.Square, r=[("ps", 4)], w=["sqb"])
            MM(ps[7][:, 0:n], onesb[:], sqb[:, 0:n], r=["onesb", "sqb"], w=[("ps", 7)])
            ACT(rstd[:, 0:n], ps[7][:, 0:n], AF.Sqrt, r=[("ps", 7)], w=["rstd"], scale=1.0 / 128, bias=epsb[:])
            RECIP(rstd[:, 0:n], rstd[:, 0:n], r=["rstd"], w=["rstd"])
            STT(ckvnT[:, blk * 512:blk * 512 + n], ps[4][:, 0:n], vecs[:, V_KVG:V_KVG + 1], rstd[:, 0:n], ALU.mult, ALU.mult,
                r=[("ps", 4), "rstd", "vecs"], w=[("ckvnT", blk)])
            if CUT == 4:
                P.emit()
                return nc
            if blk < 8:
                rkb = rk[blk % 2]
                DMA(rkb[64:96, 0, :], ropek[0, 64:96, blk * 512:(blk + 1) * 512], w=[("rk", blk % 2, 0)], q="scalar")
                DMA(rkb[64:96, 1, :], ropek[1, 64:96, blk * 512:(blk + 1) * 512], w=[("rk", blk % 2, 1)], q="scalar")
                TT("vector", t1[64:96, :], ps[5][64:96, :], rkb[64:96, 0, :], ALU.mult, r=[("ps", 5), ("rk", blk % 2, 0)], w=["t1"])
                TT("vector", t2[64:96, :], ps[6][64:96, :], rkb[64:96, 1, :], ALU.mult, r=[("ps", 6), ("rk", blk % 2, 1)], w=["t2"])
                TT("vector", KT[0][64:96, blk * 512:(blk + 1) * 512], t1[64:96, :], t2[64:96, :], ALU.add, r=["t1", "t2"], w=[("KTr", 0, blk)])
                CP("gpsimd", KT[1][64:96, blk * 512:(blk + 1) * 512], KT[0][64:96, blk * 512:(blk + 1) * 512], r=[("KTr", 0, blk)], w=[("KTr", 1, blk)])
            else:
                CP("vector", KT[0][64:96, blk * 512:blk * 512 + n], ps[5][64:96, 0:n], r=[("ps", 5)], w=[("KTr", 0, blk)])
                CP("gpsimd", KT[1][64:96, blk * 512:blk * 512 + n], KT[0][64:96, blk * 512:blk * 512 + n], r=[("KTr", 0, blk)], w=[("KTr", 1, blk)])
        if debug:
            dk = ar.alloc([NKEY], F32)
            CP("vector", dk[:], ckvnT[:], r=[("ckvnT", b) for b in range(9)], w=["dk"])
            dump("ckvnT", dk[:], ["dk"])
            dk2 = ar.alloc([NKEY], F32)
            CP("vector", dk2[64:96, :], KT[1][64:96, :], r=[("KTr", 1, b) for b in range(9)], w=["dk2"])
            dump("krot", dk2[64:96, :], ["dk2"])
        if stop_after == "K":
            P.emit()
            return nc
        BARRIER()

        ar.off = RB
        ypad = ar.alloc([8, NOWN + 30], BF16)
        assert ar.off <= RC
        ar.off = RC
        xts = [ar.alloc([D], F32) for _ in range(2)]
        sqj = ar.alloc([D], F32)
        xns = [ar.alloc([D], BF16) for _ in range(2)]
        sss = [ar.alloc([1], F32) for _ in range(2)]
        rss = [ar.alloc([1], F32) for _ in range(2)]
        hTo = ar.alloc([8, 17 * 128], BF16)
        wch = [ar.alloc([8, 128], BF16) for _ in range(4)]
        sg = [ar.alloc([512], F32) for _ in range(2)]
        yh = ar.alloc([8, 128], BF16)
        sqb2 = ar.alloc([2, 512], BF16)
        rstd = ar.alloc([512], F32)
        for t in range(17):
            s = t % 2
            norm_tile(xo[t], hTo[:, :, t * 128:(t + 1) * 128], t, A1[:, :, 0], modT[:, 0:8, 0],
                      xts[s], sqj, xns[s], sss[s], rss[s], 2 + s, ("no", s), ("hTo", t))
        DMA(hts.rearrange("p (k t) -> p k t", k=8), hTo[:, :, 0:NOWN],
            r=[(("hTo", t), 0) for t in range(16)], w=["hts"])
        blocks = [(tb * 512, 512, [4 * tb + i for i in range(4)]) for tb in range(4)] + [(2048, 128, [16])]
        load_w(wch[0], win_v[:, :, 0:128], 1024, ("wch", 0))
        load_w(wch[1], win_v[:, :, 128:256], 1024, ("wch", 1))
        for bi in range(4):
            t0, n, tiles = blocks[bi]
            hr = [(("hTo", t), 0) for t in tiles]
            for cc in range(2):
                for k in range(8):
                    MM(ps[4 + cc][:], wch[cc][:, k, :], hTo[:, k, t0:t0 + n], start=(k == 0), stop=(k == 7),
                       r=hr + [("wch", cc)], w=[("ps", 4 + cc)])
                ACT(sqb2[:, cc, :], ps[4 + cc][:], AF.Square, r=[("ps", 4 + cc)], w=[("sqb2", cc)])
            MM(ps[6][:], onesb[:], sqb2[:, 0, :], start=True, stop=False, r=[("sqb2", 0)], w=[("ps", 6)])
            MM(ps[6][:], onesb[:], sqb2[:, 1, :], start=False, stop=True, r=[("sqb2", 1)], w=[("ps", 6)])
            ACT(rstd[:], ps[6][:], AF.Sqrt, r=[("ps", 6)], w=["rstd"], scale=1.0 / 256, bias=epsb[:])
            RECIP(rstd[:], rstd[:], r=["rstd"], w=["rstd"])
            for cc in range(2):
                STT(cqnT[:, cc, t0:t0 + n], ps[4 + cc][:], vecs[:, V_QG + cc:V_QG + cc + 1], rstd[:], ALU.mult, ALU.mult,
                    r=[("ps", 4 + cc), "rstd"], w=[("cqnT", cc, bi)])
        for c in range(8):
            wa, wg = (2 * c) % 4, (2 * c + 1) % 4
            load_w(wch[wa], win_v[:, :, OFF_CONV + c * 128:OFF_CONV + (c + 1) * 128], 1024, ("wch", wa))
            load_w(wch[wg], win_v[:, :, OFF_CONV + 1024 + c * 128:OFF_CONV + 1024 + (c + 1) * 128], 1024, ("wch", wg))
            for bi, (t0, n, tiles) in enumerate(blocks):
                hr = [(("hTo", t), 0) for t in tiles]
                ba, bg_ = (4, 5) if bi % 2 == 0 else (6, 7)
                for k in range(8):
                    MM(ps[ba][:, 0:n], wch[wa][:, k, :], hTo[:, k, t0:t0 + n], start=(k == 0), stop=(k == 7),
                       r=hr + [("wch", wa)], w=[("ps", ba)])
                for k in range(8):
                    MM(ps[bg_][:, 0:n], wch[wg][:, k, :], hTo[:, k, t0:t0 + n], start=(k == 0), stop=(k == 7),
                       r=hr + [("wch", wg)], w=[("ps", bg_)])
                sgb = sg[bi % 2]
                ACT(sgb[:, 0:n], ps[bg_][:, 0:n], AF.Sigmoid, r=[("ps", bg_)], w=[("sg", bi % 2)])
                dst = ypad[:, c, 15 + t0:15 + t0 + n] if bi < 4 else yh[:, c, :]
                TT("vector", dst, ps[ba][:, 0:n], sgb[:, 0:n], ALU.mult, r=[("ps", ba), ("sg", bi % 2)],
                   w=[("ypad", c, bi)])
            TS("vector", ypad[:, c, 0:15], yh[:, c, 0:15], vecs[:, V_HM:V_HM + 1], None, ALU.mult,
               r=[("ypad", c, 4), "vecs"], w=[("ypad", c, 5)])
            TS("vector", ypad[:, c, NOWN + 15:NOWN + 30], yh[:, c, 15:30], vecs[:, V_HM + 1:V_HM + 2], None, ALU.mult,
               r=[("ypad", c, 4), "vecs"], w=[("ypad", c, 6)])
        if debug:
            dump("cqnT", cqnT[:].rearrange("p a b -> p (a b)"), [("cqnT", cc, bi) for cc in range(2) for bi in range(4)])
            dump("ypad", ypad[:].rearrange("p a b -> p (a b)"), [("ypad", c, i) for c in range(8) for i in range(7)])
        if stop_after == "O":
            P.emit()
            return nc
        BARRIER()

        dg = [ar.alloc([31, 128], BF16) for _ in range(2)]
        vT = ar.alloc([8, NOWN], BF16)
        sqv = [ar.alloc([512], BF16) for _ in range(2)]
        mean = ar.alloc([512], F32)
        msq = ar.alloc([512], F32)
        rstdc = ar.alloc([512], F32)
        tmpc = [ar.alloc([512], F32) for _ in range(2)]
        for c in range(8):
            d_ = dg[c % 2]
            TT("vector", d_[:], identb[:].unsqueeze(1).to_broadcast([128, 31, 128]),
               vecs[:, V_CW + c * 31:V_CW + (c + 1) * 31].unsqueeze(2).to_broadcast([128, 31, 128]), ALU.mult,
               r=[], w=[("dg", c % 2)])
            for tb in range(4):
                bank = 4 + (tb % 2)
                for k in range(31):
                    MM(ps[bank][:], d_[:, k, :], ypad[:, c, tb * 512 + k:tb * 512 + k + 512], start=(k == 0), stop=(k == 30),
                       r=[("dg", c % 2)], w=[("ps", bank)])
                ACT(vT[:, c, tb * 512:(tb + 1) * 512], ps[bank][:], AF.Identity, r=[("ps", bank)], w=[("vT", c, tb)],
                    bias=vecs[:, V_CB + c:V_CB + c + 1])
        for tb in range(4):
            tsl = slice(tb * 512, (tb + 1) * 512)
            for c in range(8):
                sv = sqv[c % 2]
                ACT(sv[:], vT[:, c, tsl], AF.Square, r=[("vT", c, tb)], w=[("sqv", c % 2)])
                MM(ps[6][:], onesb[:], vT[:, c, tsl], start=(c == 0), stop=(c == 7), r=[("vT", c, tb)], w=[("ps", 6)])
                MM(ps[7][:], onesb[:], sv[:], start=(c == 0), stop=(c == 7), r=[("sqv", c % 2)], w=[("ps", 7)])
            ACT(mean[:], ps[6][:], AF.Identity, r=[("ps", 6)], w=["mean"], scale=1.0 / D)
            ACT(msq[:], ps[7][:], AF.Identity, r=[("ps", 7)], w=["msq"], scale=1.0 / D)
            TT("vector", rstdc[:], mean[:], mean[:], ALU.mult, r=["mean"], w=["rstdc"])
            TT("vector", rstdc[:], msq[:], rstdc[:], ALU.subtract, r=["msq", "rstdc"], w=["rstdc"])
            ACT(rstdc[:], rstdc[:], AF.Sqrt, r=["rstdc"], w=["rstdc"], bias=epsb[:])
            RECIP(rstdc[:], rstdc[:], r=["rstdc"], w=["rstdc"])
            for c in range(8):
                tm = tmpc[c % 2]
                TT("vector", tm[:], vT[:, c, tsl], mean[:], ALU.subtract, r=[("vT", c, tb), "mean"], w=[("tmpc", c % 2)])
                TT("vector", tm[:], tm[:], rstdc[:], ALU.mult, r=[("tmpc", c % 2), "rstdc"], w=[("tmpc", c % 2)])
                ACT(vT[:, c, tsl], tm[:], AF.Silu, r=[("tmpc", c % 2)], w=[("vT", c, tb)],
                    scale=vecs[:, V_LNG + c:V_LNG + c + 1], bias=vecs[:, V_LNB + c:V_LNB + c + 1])
        zkeys = [("vT", c, tb) for c in range(8) for tb in range(4)]
        DMA(zts.rearrange("p (k t) -> p k t", k=8), vT[:], r=zkeys, w=["zts"])
        if debug:
            dump("zT", vT[:].rearrange("p a b -> p (a b)"), zkeys)
        if stop_after == "C":
            P.emit()
            return nc
        BARRIER()

        ar.off = RB
        OT = ar.alloc([8, NOWN], BF16)
        assert ar.off <= RC
        ar.off = RC
        VH = [ar.alloc([34, 128], BF16) for _ in range(2)]
        QT = [ar.alloc([NOWN], BF16) for _ in range(2)]
        PT = [ar.alloc([512], BF16) for _ in range(4)]
        wuqb = ar.alloc([2, 1536], BF16)
        wuqpb = ar.alloc([2, 1536], BF16)
        wukvb = ar.alloc([2048], BF16)
        rq = ar.alloc([2, NOWN], F32)
        qt1 = ar.alloc([512], F32)
        qt2 = ar.alloc([512], F32)
        rden = ar.alloc([512], F32)
        rdenB = ar.alloc([512], F32)
        wuq_v = wuq.rearrange("(k p) f -> p k f", p=128)
        wuqp_v = wuqp.rearrange("(k p) f -> p k f", p=128)
        for k in range(2):
            for hh in range(2):
                load_w(wuqb[:, k, hh * 768:(hh + 1) * 768], wuq_v[:, k, hh * 768:(hh + 1) * 768], 768, ("wuqb", k, hh))
                load_w(wuqpb[:, k, hh * 768:(hh + 1) * 768], wuqp_v[:, k, hh * 768:(hh + 1) * 768], 768, ("wuqpb", k, hh))
        for hh in range(2):
            load_w(wukvb[:, hh * 1024:(hh + 1) * 1024], wukv[:, hh * 1024:(hh + 1) * 1024], 1024, ("wukvb", hh))
        wq_keys = [("wuqb", k, hh) for k in range(2) for hh in range(2)]
        wqp_keys = [("wuqpb", k, hh) for k in range(2) for hh in range(2)]
        wkv_keys = [("wukvb", 0), ("wukvb", 1)]
        DMA(rq[0:96, 0, :], ropeq[0, 0:96, :], w=[("rq", 0)])
        DMA(rq[0:96, 1, :], ropeq[1, 0:96, :], w=[("rq", 1)], q="scalar")
        for hb in range(2):
            MEMSET("gpsimd", VH[hb][:], 0.0, w=[("VH", hb)])
        MEMSET("gpsimd", VH[0][:, :, 64:65], 1.0, w=[("VH", 0)])
        MEMSET("gpsimd", VH[1][:, :, 0:1], 1.0, w=[("VH", 1)])
        ckeys = [("ckvnT", b_) for b_ in range(9)]
        cvf = [ar.alloc([2, 1024], F32) for _ in range(2)]
        cvb = [ar.alloc([2, 1024], BF16) for _ in range(2)]
        cv_steps = [(tab, tabb, nm, g) for (tab, tabb, nm) in ((ut, utb, "u"), (vt, vtb, "v")) for g in range(64)]
        cv_pos = [0]

        def conv_step():
            if cv_pos[0] >= len(cv_steps):
                return
            tab, tabb, nm, g = cv_steps[cv_pos[0]]
            s = cv_pos[0] % 2
            DMA(cvf[s][:], tab[g * 2:(g + 1) * 2].rearrange("i p f -> p i f"), w=[("cvf", s)], q="sync")
            CP(("gpsimd", "vector")[cv_pos[0] % 2], cvb[s][:], cvf[s][:], r=[("cvf", s)], w=[("cvb", s)])
            DMA(tabb[g], cvb[s][:].rearrange("p a b -> p (a b)"), r=[("cvb", s)], w=[("tabb", nm, g)], q="sync",
                sem_key=("tabb", s))
            cv_pos[0] += 1

        def prep_steps(h):
            hb = h % 2
            voff = 0 if hb == 0 else 64
            steps = []
            for blk in range(9):
                def st_k(blk=blk):
                    n = 512 if blk < 8 else 256
                    bank = 5 + (blk % 2)
                    MM(ps[bank][0:64, 0:n], wukvb[:, h * 128:h * 128 + 64], ckvnT[:, blk * 512:blk * 512 + n],
                       r=wkv_keys + ckeys, w=[("ps", bank)])
                    CP("vector", KT[hb][0:64, blk * 512:blk * 512 + n], ps[bank][0:64, 0:n], r=[("ps", bank)], w=[("KTn", hb)])
                steps.append(st_k)
            for g in range(5):
                def st_v(g=g):
                    cnt = 8 if g < 4 else 2
                    bank = 5 + (g % 2)
                    for j in range(cnt):
                        kt = g * 8 + j
                        MM(ps[bank][:, j * 64:(j + 1) * 64], ckvnT[:, kt * 128:(kt + 1) * 128], wukvb[:, h * 128 + 64:h * 128 + 128],
                           r=wkv_keys + ckeys, w=[("ps", bank)])
                    CP("vector", VH[hb][:, g * 8:g * 8 + cnt, voff:voff + 64],
                       ps[bank][:, 0:cnt * 64].rearrange("p (a b) -> p a b", b=64), r=[("ps", bank)], w=[("VH", hb)])
                steps.append(st_v)
            for qb in range(4):
                def st_q(qb=qb):
                    qs = slice(qb * 512, (qb + 1) * 512)
                    for k in range(2):
                        MM(ps[5][0:96, :], wuqb[:, k, h * 96:(h + 1) * 96], cqnT[:, k, qs], start=(k == 0), stop=(k == 1),
                           r=wq_keys + [("cqnT", k, qb)], w=[("ps", 5)])
                    for k in range(2):
                        MM(ps[6][0:96, :], wuqpb[:, k, h * 96:(h + 1) * 96], cqnT[:, k, qs], start=(k == 0), stop=(k == 1),
                           r=wqp_keys + [("cqnT", k, qb)], w=[("ps", 6)])
                    TT("vector", qt1[0:96, :], ps[5][0:96, :], rq[0:96, 0, qs], ALU.mult, r=[("ps", 5), ("rq", 0)], w=["qt1"])
                    TT("vector", qt2[0:96, :], ps[6][0:96, :], rq[0:96, 1, qs], ALU.mult, r=[("ps", 6), ("rq", 1)], w=["qt2"])
                    TT("vector", QT[hb][0:96, qs], qt1[0:96, :], qt2[0:96, :], ALU.add, r=["qt1", "qt2"], w=[("QT", hb)])
                steps.append(st_q)
            return steps

        def prep(h):
            for st_ in prep_steps(h):
                st_()

        prep(0)
        gi = [0]
        for h in range(NH):
            hb = h % 2
            ktr = [("KTn", hb)] + [("KTr", hb, b_) for b_ in range(9)]
            items = [(qb, kt) for qb in range(4) for kt in range(34)]
            base = gi[0]

            def S(idx):
                qb, kt = items[idx]
                sb_ = (base + idx) % 3
                MM(ps[sb_][:], KT[hb][0:96, kt * 128:(kt + 1) * 128], QT[hb][0:96, qb * 512:(qb + 1) * 512],
                   r=ktr + [("QT", hb)], w=[("ps", sb_)])

            pending = []
            nsteps = prep_steps(h + 1) if h + 1 < NH else []
            S(0)
            S(1)
            for idx, (qb, kt) in enumerate(items):
                if idx + 2 < len(items):
                    S(idx + 2)
                sb_ = (base + idx) % 3
                pb_ = (base + idx) % 4
                ob = 3 + (qb % 2)
                qs = slice(qb * 512, (qb + 1) * 512)
                ACT(PT[pb_][:], ps[sb_][:], AF.Exp, r=[("ps", sb_)], w=[("PT", pb_)], scale=ATTN_SCALE)
                MM(ps[ob][:], VH[hb][:, kt, :], PT[pb_][:], start=(kt == 0), stop=(kt == 33),
                   r=[("VH", hb), ("PT", pb_)], w=[("ps", ob)])
                if pending and pending[0][0] <= idx:
                    _, (r0, prow, ob2, qs2) = pending.pop(0)
                    MM(ps[7][:], onesf[r0:r0 + 1, :], rden[r0:r0 + 1, :], r=["rden"], w=[("ps", 7)])
                    CP("vector", rdenB[:], ps[7][:], r=[("ps", 7)], w=["rdenB"])
                    TT("vector", OT[prow, h // 2, qs2], ps[ob2][prow, :], rdenB[prow, :], ALU.mult, r=[("ps", ob2), "rdenB"],
                       w=[("OT", h, qs2.start // 512)])
                if kt == 33:
                    r0 = 64 if hb == 0 else 0
                    prow = slice(0, 64) if hb == 0 else slice(64, 128)
                    RECIP(rden[r0:r0 + 1, :], ps[ob][r0:r0 + 1, :], r=[("ps", ob)], w=["rden"])
                    pending.append((idx + 6 if qb < 3 else idx, (r0, prow, ob, qs)))
                if idx % 17 == 8:
                    conv_step()
                if idx >= 10 and idx % 5 == 0 and nsteps:
                    nsteps.pop(0)()
            while pending:
                _, (r0, prow, ob2, qs2) = pending.pop(0)
                MM(ps[7][:], onesf[r0:r0 + 1, :], rden[r0:r0 + 1, :], r=["rden"], w=[("ps", 7)])
                CP("vector", rdenB[:], ps[7][:], r=[("ps", 7)], w=["rdenB"])
                TT("vector", OT[prow, h // 2, qs2], ps[ob2][prow, :], rdenB[prow, :], ALU.mult, r=[("ps", ob2), "rdenB"],
                   w=[("OT", h, qs2.start // 512)])
            while nsteps:
                nsteps.pop(0)()
            gi[0] += len(items)
        while cv_pos[0] < len(cv_steps):
            conv_step()
        okeys = [("OT", h, qb) for h in range(NH) for qb in range(4)]
        if debug:
            dump("OT", OT[:].rearrange("p a b -> p (a b)"), okeys)
        if stop_after == "ATT":
            P.emit()
            return nc
        BARRIER()

        ar.off = RA
        mT = ar.alloc([8, NOWN], BF16)
        assert ar.off <= RB
        ar.off = RC
        hTo2 = ar.alloc([8, NOWN], BF16)
        zT2 = ar.alloc([8, NOWN], BF16)
        DMA(hTo2[:], hts.rearrange("p (k t) -> p k t", k=8), w=["hTo2"])
        DMA(zT2[:], zts.rearrange("p (k t) -> p k t", k=8), w=["zT2"], q="scalar")
        wm = [[ar.alloc([8, 128], BF16) for _ in range(4)] for _ in range(2)]
        sga = [ar.alloc([512], F32) for _ in range(2)]
        sgc = [ar.alloc([512], F32) for _ in range(2)]
        m1 = [ar.alloc([512], F32) for _ in range(2)]
        womla_v = womla.rearrange("(k p) f -> p k f", p=128)
        wpw_v = wpw.rearrange("(k p) f -> p k f", p=128)
        for c in range(8):
            ws = c % 2
            cs = slice(c * 128, (c + 1) * 128)
            load_w(wm[ws][0], womla_v[:, :, cs], 1024, ("wm", ws, 0))
            load_w(wm[ws][1], wpw_v[:, :, cs], 1024, ("wm", ws, 1))
            load_w(wm[ws][2], win_v[:, :, OFF_GATE + c * 128:OFF_GATE + (c + 1) * 128], 1024, ("wm", ws, 2))
            load_w(wm[ws][3], win_v[:, :, OFF_GATE + 1024 + c * 128:OFF_GATE + 1024 + (c + 1) * 128], 1024, ("wm", ws, 3))
            for tb in range(4):
                tsl = slice(tb * 512, (tb + 1) * 512)
                banks = (0, 1, 2, 3) if tb % 2 == 0 else (4, 5, 6, 7)
                srcs = [(OT, "OT"), (zT2, "zT2"), (hTo2, "hTo2"), (hTo2, "hTo2")]
                for i in range(4):
                    for k in range(8):
                        MM(ps[banks[i]][:], wm[ws][i][:, k, :], srcs[i][0][:, k, tsl], start=(k == 0), stop=(k == 7),
                           r=[("wm", ws, i), srcs[i][1]], w=[("ps", banks[i])])
                p2 = tb % 2
                ACT(sga[p2][:], ps[banks[2]][:], AF.Sigmoid, r=[("ps", banks[2])], w=[("sga", p2)])
                ACT(sgc[p2][:], ps[banks[3]][:], AF.Sigmoid, r=[("ps", banks[3])], w=[("sgc", p2)])
                TT("vector", m1[p2][:], ps[banks[0]][:], sga[p2][:], ALU.mult, r=[("ps", banks[0]), ("sga", p2)], w=[("m1", p2)])
                TT("vector", sgc[p2][:], ps[banks[1]][:], sgc[p2][:], ALU.mult, r=[("ps", banks[1]), ("sgc", p2)], w=[("sgc", p2)])
                TT("vector", mT[:, c, tsl], m1[p2][:], sgc[p2][:], ALU.add, r=[("m1", p2), ("sgc", p2)], w=[("mT", c, tb)])
        if stop_after == "M":
            P.emit()
            return nc
        BARRIER()

        ar.off = RB
        h2T = ar.alloc([8, NOWN], BF16)
        assert ar.off <= RC
        ar.off = RC
        woutb = ar.alloc([8, 1024], BF16)
        wout_v = wout.rearrange("(k p) f -> p k f", p=128)
        for k in range(8):
            load_w(woutb[:, k, :], wout_v[:, k, :], 1024, ("woutb", k))
        wok = [("woutb", k) for k in range(8)]
        xts = [ar.alloc([D], F32) for _ in range(2)]
        x1t = [ar.alloc([D], F32) for _ in range(2)]
        sqj = ar.alloc([D], F32)
        xns = [ar.alloc([D], BF16) for _ in range(2)]
        sss = [ar.alloc([1], F32) for _ in range(2)]
        rss = [ar.alloc([1], F32) for _ in range(2)]
        for tt in range(16):
            s = tt % 2
            DMA(xts[s][:], xo[tt], w=[("xo2", s)], q=dmaq[tt % 2])
            for half in range(2):
                bank = (4 + half) if s == 0 else (6 + half)
                hs = slice(half * 512, (half + 1) * 512)
                for k in range(8):
                    MM(ps[bank][:], mT[:, k, tt * 128:(tt + 1) * 128], woutb[:, k, hs], start=(k == 0), stop=(k == 7),
                       r=wok + ["mT"], w=[("ps", bank)])
                TT("vector", x1t[s][:, hs], ps[bank][:], g1row[:, hs], ALU.mult, r=[("ps", bank)], w=[("x1t", s, half)])
                TT("gpsimd", x1t[s][:, hs], x1t[s][:, hs], xts[s][:, hs], ALU.add, r=[("x1t", s, half), ("xo2", s)], w=[("x1t", s, half)])
            DMA(x1s[tt], x1t[s][:], r=[("x1t", s, 0), ("x1t", s, 1)], w=[("x1s", tt)])
            P.op("gpsimd", lambda e: e.memset(junk[:, 1:2], 0.0), reads=[("x1t", s, 0), ("x1t", s, 1)], writes=[(("n2", s), "xt")])
            norm_tile(None, h2T[:, :, tt * 128:(tt + 1) * 128], tt, A2[:], modT[:, 24:32, 0],
                      x1t[s][:], sqj[:], xns[s][:], sss[s][:], rss[s][:], 2 + s, ("n2", s), ("h2T", tt))
        if stop_after == "M2":
            P.emit()
            return nc
        BARRIER()

        ar.off = RA
        E1T = ar.alloc([NOWN], BF16)
        E2T = ar.alloc([NOWN], BF16)
        WT = ar.alloc([NOWN], F32)
        ar.off = RC
        subkb = ar.alloc([16, 128], BF16)
        wpqc = [ar.alloc([8, 128], BF16) for _ in range(2)]
        qT = ar.alloc([16, 512], BF16)
        scs = [ar.alloc([16, 128], F32) for _ in range(2)]
        sc2 = ar.alloc([16, 128], F32)
        stop_ = ar.alloc([16, 16], F32)
        itopu = ar.alloc([16, 16], U32)
        itopf = ar.alloc([16, 16], F32)
        cand = ar.alloc([8, 256], F32)
        cand2 = ar.alloc([8, 256], F32)
        best = ar.alloc([8, 16], F32)
        ciu = ar.alloc([8, 16], U32)
        cif = ar.alloc([8, 16], F32)
        c1f = ar.alloc([8, 16], F32)
        c2f = ar.alloc([8, 16], F32)
        eq = ar.alloc([8, 16, 16], F32)
        e1f = ar.alloc([8, 16], F32)
        e2f = ar.alloc([8, 16], F32)
        ex = ar.alloc([8, 16], F32)
        se = ar.alloc([8], F32)
        wg = ar.alloc([8, 16], F32)
        iota16 = ar.alloc([16], F32)
        thr16 = ar.alloc([16], F32)
        P.op("gpsimd", lambda e: e.iota(iota16[:], pattern=[[1, 16]], base=0, channel_multiplier=0,
                                        allow_small_or_imprecise_dtypes=True), writes=["iota16"])
        P.op("gpsimd", lambda e: e.iota(thr16[:], pattern=[[16, 16]], base=16, channel_multiplier=0,
                                        allow_small_or_imprecise_dtypes=True), writes=["thr16"])
        MEMSET("gpsimd", thr16[:, 15:16], 1.0e9, w=["thr16"])
        load_w(subkb[:, 0:8, :], subkT_d[:, 0:1024].rearrange("p (a b) -> p a b", b=128), 1024, ("subkb", 0))
        load_w(subkb[:, 8:16, :], subkT_d[:, 1024:2048].rearrange("p (a b) -> p a b", b=128), 1024, ("subkb", 1))
        wpq_v = wpq.rearrange("(k p) f -> p k f", p=128)
        itop_v = itopf[:].rearrange("p (h two) m -> p h two m", two=2)
        stop_v = stop_[:].rearrange("p (h two) m -> p h two m", two=2)
        B4 = [128, 8, 16, 16]
        for tb in range(4):
            for j in range(16):
                ws = j % 2
                load_w(wpqc[ws], wpq_v[:, :, j * 128:(j + 1) * 128], 1024, ("wpqc", ws))
                for k in range(8):
                    MM(ps[4 + ws][:], wpqc[ws][:, k, :], h2T[:, k, tb * 512:(tb + 1) * 512], start=(k == 0), stop=(k == 7),
                       r=[("wpqc", ws), "h2T"], w=[("ps", 4 + ws)])
                CP("vector" if j % 2 else "scalar", qT[:, j, :], ps[4 + ws][:], r=[("ps", 4 + ws)], w=[("qT", j)])
            def score(tt_):
                tl_ = tt_ % 4
                sc_ = scs[tt_ % 2]
                for j in range(16):
                    MM(ps[j // 4][:, (j % 4) * 128:(j % 4 + 1) * 128], qT[:, j, tl_ * 128:(tl_ + 1) * 128], subkb[:, j, :],
                       r=[("qT", j), ("subkb", 0), ("subkb", 1)], w=[("ps", j // 4)])
                for g in range(4):
                    CP("scalar", sc_[:, g * 4:(g + 1) * 4, :], ps[g][:].rearrange("p (a b) -> p a b", b=128), r=[("ps", g)],
                       w=[("sc", tt_ % 2, g)])

            score(tb * 4)
            for tl in range(4):
                tt = tb * 4 + tl
                if tl < 3:
                    score(tt + 1)
                sc = scs[tt % 2]
                for j in range(16):
                    g = j // 4
                    skey = ("sc", tt % 2, g)
                    P.op("vector", lambda e, j=j, sc=sc: e.max(out=stop_[:, j, 0:8], in_=sc[:, j, :]), reads=[skey], writes=[("stop", j)])
                    P.op("vector", lambda e, j=j, sc=sc: e.max_index(out=itopu[:, j, 0:8], in_max=stop_[:, j, 0:8], in_values=sc[:, j, :]),
                         reads=[skey, ("stop", j)], writes=[("itopu", j)])
                    P.op("vector", lambda e, j=j, sc=sc: e.match_replace(out=sc2[:, j, :], in_to_replace=stop_[:, j, 0:8], in_values=sc[:, j, :], imm_value=-1.0e30),
                         reads=[skey, ("stop", j)], writes=[("sc2", j)])
                    P.op("vector", lambda e, j=j: e.max(out=stop_[:, j, 8:16], in_=sc2[:, j, :]), reads=[("sc2", j)], writes=[("stop", j)])
                    P.op("vector", lambda e, j=j: e.max_index(out=itopu[:, j, 8:16], in_max=stop_[:, j, 8:16], in_values=sc2[:, j, :]),
                         reads=[("sc2", j), ("stop", j)], writes=[("itopu", j)])
                sk = [("stop", j) for j in range(16)]
                ik = [("itopu", j) for j in range(16)]
                CP("vector", itopf[:], itopu[:], r=ik, w=["itopf"])
                TT("vector", cand[:].rearrange("p h (a b) -> p h a b", b=16), stop_v[:, :, 0, :].unsqueeze(3).to_broadcast(B4),
                   stop_v[:, :, 1, :].unsqueeze(2).to_broadcast(B4), ALU.add, r=sk, w=["cand"])
                for h in range(8):
                    P.op("vector", lambda e, h=h: e.max(out=best[:, h, 0:8], in_=cand[:, h, :]), reads=["cand"], writes=[("best", h)])
                    P.op("vector", lambda e, h=h: e.max_index(out=ciu[:, h, 0:8], in_max=best[:, h, 0:8], in_values=cand[:, h, :]),
                         reads=["cand", ("best", h)], writes=[("ciu", h)])
                    P.op("vector", lambda e, h=h: e.match_replace(out=cand2[:, h, :], in_to_replace=best[:, h, 0:8], in_values=cand[:, h, :], imm_value=-1.0e30),
                         reads=["cand", ("best", h)], writes=[("cand2", h)])
                    P.op("vector", lambda e, h=h: e.max(out=best[:, h, 8:16], in_=cand2[:, h, :]), reads=[("cand2", h)], writes=[("best", h)])
                    P.op("vector", lambda e, h=h: e.max_index(out=ciu[:, h, 8:16], in_max=best[:, h, 8:16], in_values=cand2[:, h, :]),
                         reads=[("cand2", h), ("best", h)], writes=[("ciu", h)])
                bk = [("best", h) for h in range(8)]
                ck = [("ciu", h) for h in range(8)]
                CP("vector", cif[:], ciu[:], r=ck, w=["cif"])
                TT("vector", eq[:], cif[:].unsqueeze(3).to_broadcast(B4), thr16[:].unsqueeze(1).unsqueeze(1).to_broadcast(B4), ALU.is_ge,
                   r=["cif", "thr16"], w=["eq"])
                P.op("vector", lambda e: e.tensor_reduce(out=c1f[:], in_=eq[:], axis=AX.X, op=ALU.add), reads=["eq"], writes=["c1f"])
                STT(c2f[:], c1f[:], -16.0, cif[:], ALU.mult, ALU.add, r=["c1f", "cif"], w=["c2f"])
                for (cf_, two, ef_) in ((c1f, 0, e1f), (c2f, 1, e2f)):
                    TT("vector", eq[:], cf_[:].unsqueeze(3).to_broadcast(B4), iota16[:].unsqueeze(1).unsqueeze(1).to_broadcast(B4), ALU.is_equal,
                       r=["c1f", "c2f", "iota16"], w=["eq"])
                    TT("vector", eq[:], eq[:], itop_v[:, :, two, :].unsqueeze(2).to_broadcast(B4), ALU.mult, r=["eq", "itopf"], w=["eq"])
                    P.op("vector", lambda e, ef_=ef_: e.tensor_reduce(out=ef_[:], in_=eq[:], axis=AX.X, op=ALU.add), reads=["eq"], writes=[("ef", two)])
                TT("vector", ex[:], best[:], best[:, :, 0:1].to_broadcast([128, 8, 16]), ALU.subtract, r=bk, w=["ex"])
                ACT(ex[:], ex[:], AF.Exp, r=["ex"], w=["ex"])
                P.op("vector", lambda e: e.tensor_reduce(out=se[:], in_=ex[:], axis=AX.X, op=ALU.add), reads=["ex"], writes=["se"])
                RECIP(se[:], se[:], r=["se"], w=["se"])
                TT("vector", wg[:], ex[:], se[:].unsqueeze(2).to_broadcast([128, 8, 16]), ALU.mult, r=["ex", "se"], w=["wg"])
                tsl = slice(tt * 128, (tt + 1) * 128)
                TR(ps[6][:, 0:128], e1f[:].rearrange("p a b -> p (a b)"), identf[:], r=[("ef", 0)], w=[("ps", 6)])
                TR(ps[6][:, 128:256], e2f[:].rearrange("p a b -> p (a b)"), identf[:], r=[("ef", 1)], w=[("ps", 6)])
                TR(ps[6][:, 256:384], wg[:].rearrange("p a b -> p (a b)"), identf[:], r=["wg"], w=[("ps", 6)])
                CP("scalar", E1T[:, tsl], ps[6][:, 0:128], r=[("ps", 6)], w=[("E1T", tt)])
                CP("scalar", E2T[:, tsl], ps[6][:, 128:256], r=[("ps", 6)], w=[("E2T", tt)])
                CP("scalar", WT[:, tsl], ps[6][:, 256:384], r=[("ps", 6)], w=[("WT", tt)])
        if stop_after == "P0":
            P.emit()
            return nc
        BARRIER()

        ar.off = RA + 16 * 1024
        E1Tm = ar.alloc([NOWN], BF16)
        iota3 = ar.alloc([8, 128], BF16)
        iotaH = ar.alloc([8, 64], BF16)
        xfin = ar.alloc([D], F32)
        x1r = ar.alloc([D], F32)
        assert ar.off <= RB
        ar.off = RC
        Wh = [ar.alloc([256, 64], BF16) for _ in range(2)]
        NSLOT = 3
        UTb = [ar.alloc([2, 1024], BF16) for _ in range(NSLOT)]
        Vb = [ar.alloc([2, 1024], BF16) for _ in range(NSLOT)]
        Gs = [ar.alloc([256], BF16) for _ in range(3)]
        GWs = [ar.alloc([256], BF16) for _ in range(3)]
        ssf = ar.alloc([1], F32)
        E1p = [ar.alloc([8, 64], BF16) for _ in range(3)]
        E2p = [ar.alloc([8, 128], BF16) for _ in range(3)]
        xsq = x1r.bitcast(BF16)[:, 0:D]
        P.op("gpsimd", lambda e: e.iota(iota3[:], pattern=[[0, 8], [1, 128]], base=0, channel_multiplier=0,
                                        allow_small_or_imprecise_dtypes=True), writes=["iota3"])
        P.op("gpsimd", lambda e: e.iota(iotaH[:], pattern=[[0, 8], [1, 64]], base=0, channel_multiplier=0,
                                        allow_small_or_imprecise_dtypes=True), writes=["iotaH"])
        TS("vector", E1Tm[:], E1T[:], -64.0, None, ALU.add, r=[], w=["E1Tm"])

        def b_eq(tb_, hf_, m):
            t0 = tb_ * 256 + m * 8
            bi = m % 3
            e1src = E1T if hf_ == 0 else E1Tm
            TT("vector", E1p[bi][:], iotaH[:], e1src[:, t0:t0 + 8].unsqueeze(2).to_broadcast([128, 8, 64]), ALU.is_equal,
               r=["iotaH", "E1Tm"], w=[("E1p", bi)])
            TT("vector", E2p[bi][:], iota3[:], E2T[:, t0:t0 + 8].unsqueeze(2).to_broadcast([128, 8, 128]), ALU.is_equal,
               r=["iota3"], w=[("E2p", bi)])

        def b_mul(tb_, hf_, m):
            t0 = tb_ * 256 + m * 8
            bi = m % 3
            TT("gpsimd", E2p[bi][:], E2p[bi][:], WT[:, t0:t0 + 8].unsqueeze(2).to_broadcast([128, 8, 128]), ALU.mult,
               r=[("E2p", bi)], w=[("E2p", bi)])

        def b_mm(tb_, hf_, m):
            bi = m % 3
            for tq in range(8):
                MM(ps[6][:, tq * 64:(tq + 1) * 64], E2p[bi][:, tq, :], E1p[bi][:, tq, :], r=[("E1p", bi), ("E2p", bi)], w=[("ps", 6)])
            CP("scalar", Wh[hf_][:, m * 8:m * 8 + 8, :], ps[6][:].rearrange("p (a b) -> p a b", b=64), r=[("ps", 6)], w=[("Wh", hf_)])

        def loadg(cg):
            s = cg % NSLOT
            DMA(UTb[s][:].rearrange("p a b -> p (a b)"), utb[cg], w=[("UTb", s)], q="sync")
            DMA(Vb[s][:].rearrange("p a b -> p (a b)"), vtb[cg], w=[("Vb", s)], q="sync")

        for m in range(32):
            b_eq(0, 0, m)
            b_mul(0, 0, m)
            b_mm(0, 0, m)
        for tb in range(8):
            T0 = tb * 256
            ABANK = (4, 5, 7)

            def Amm(i):
                s = (i // 2) % NSLOT
                ab = i % 3
                for k in range(8):
                    MM(ps[ABANK[ab]][:, 0:256], UTb[s][:, i % 2, k * 128:(k + 1) * 128], h2T[:, k, T0:T0 + 256], start=(k == 0), stop=(k == 7),
                       r=[("UTb", s), "h2T"], w=[("ps", ABANK[ab])])

            for c0 in range(NSLOT):
                loadg(c0)
            Amm(0)
            Amm(1)
            for i in range(128):
                hf2 = i // 64
                if i + 2 < 128:
                    Amm(i + 2)
                cg = i // 2
                s = cg % NSLOT
                ab = i % 3
                nxt = (tb, 1) if hf2 == 0 else ((tb + 1, 0) if tb + 1 < 8 else None)
                m = (i % 64) // 2
                if nxt is not None and i % 2 == 0 and m >= 2:
                    b_mm(nxt[0], nxt[1], m - 2)
                ACT(Gs[ab][:], ps[ABANK[ab]][:, 0:256], AF.Gelu, r=[("ps", ABANK[ab])], w=[("Gs", ab)])
                TT("vector", GWs[ab][:], Gs[ab][:], Wh[hf2][:, :, i - 64 * hf2], ALU.mult, r=[("Gs", ab), ("Wh", hf2)], w=[("GWs", ab)])
                for tl in range(2):
                    for half in range(2):
                        MM(ps[tl * 2 + half][:], GWs[ab][:, tl * 128:(tl + 1) * 128], Vb[s][:, i % 2, half * 512:(half + 1) * 512],
                           start=(i == 0), stop=(i == 127), r=[("GWs", ab), ("Vb", s)], w=[("ps", tl * 2 + half)])
                if i % 2 == 1 and cg + NSLOT < 64:
                    loadg(cg + NSLOT)
                if nxt is not None:
                    if i % 2 == 0:
                        b_eq(nxt[0], nxt[1], m)
                    else:
                        b_mul(nxt[0], nxt[1], m)
                        if m == 31:
                            b_mm(nxt[0], nxt[1], 30)
                            b_mm(nxt[0], nxt[1], 31)
            for tl in range(2):
                tt = tb * 2 + tl
                DMA(x1r[:], x1s[tt], w=["x1r"])
                for half in range(2):
                    hs = slice(half * 512, (half + 1) * 512)
                    TT("vector", xfin[:, hs], ps[tl * 2 + half][:], g2row[:, hs], ALU.mult, r=[("ps", tl * 2 + half)], w=["xfin"])
                TT("vector", xfin[:], xfin[:], x1r[:], ALU.add, r=["xfin", "x1r"], w=["xfin"])
                ACT(xsq, xfin[:], AF.Square, r=["xfin"], w=["x1r", "ssf"], accum=ssf[:])
                ACT(ssf[:], ssf[:], AF.Sqrt, r=["ssf"], w=["ssf"], scale=1.0 / D, bias=epsb[:])
                RECIP(ssf[:], ssf[:], r=["ssf"], w=["ssf"])
                STT(xfin[:], xfin[:], ssf[:, 0:1], fgrow[:], ALU.mult, ALU.mult, r=["xfin", "ssf"], w=["xfin"])
                DMA(out_d[tt], xfin[:], r=["xfin"], w=[("out", tt)], sem_key="out")

        P.emit()
    return nc


def _rope_tabs(pos):
    n_freq = 8
    freq = (np.float32(10000.0) ** (-np.arange(n_freq, dtype=np.float32) / np.float32(n_freq))).astype(np.float32)
    r = (pos // 64).astype(np.float32)
    c = (pos % 64).astype(np.float32)
    ang = np.stack([r[:, None] * freq[None, :], c[:, None] * freq[None, :]], axis=1).astype(np.float32)
    cos = np.cos(ang).astype(np.float32)
    sin = np.sin(ang).astype(np.float32)
    ct = np.zeros((32, pos.shape[0]), np.float32)
    stb = np.zeros((32, pos.shape[0]), np.float32)
    perm = np.zeros(32, np.int64)
    for a in range(2):
        for half in range(2):
            for f in range(8):
                d = a * 16 + half * 8 + f
                perm[d] = a * 16 + (1 - half) * 8 + f
                ct[d] = cos[:, a, f]
                stb[d] = -sin[:, a, f] if half == 0 else sin[:, a, f]
    return ct, stb, perm


def prep_inputs(inp):
    f = lambda a: np.ascontiguousarray(np.asarray(a, dtype=np.float32))
    x, c, ctx, c_ctx = f(inp["x"]), f(inp["c"]), f(inp["ctx"]), f(inp["c_ctx"])
    w_in = f(inp["w_in"])[0]
    _, _, perm = _rope_tabs(np.arange(4))
    shared = {}
    shared["wmod"] = f(inp["w_mod"])[0]
    b_mod = f(inp["b_mod"])[0]
    shared["bmodg"] = np.ascontiguousarray(np.broadcast_to(np.concatenate([b_mod[2048:3072], b_mod[5120:6144]])[None, :], (128, 2048)))
    shared["fgrow"] = np.ascontiguousarray(np.broadcast_to(f(inp["final_g"])[None, :], (128, D)))
    shared["win"] = w_in
    wkr = np.zeros((D, 192), np.float32)
    wkr[:, 64:96] = w_in[:, OFF_KR:OFF_KR + 32]
    wkr[:, 160:192] = w_in[:, OFF_KR + perm]
    shared["wkr"] = wkr
    w_uq = f(inp["w_uq"])[0]
    shared["wuq"] = w_uq
    colp = np.arange(1536).reshape(16, 96).copy()
    for h in range(16):
        colp[h, 64:96] = h * 96 + 64 + perm
    shared["wuqp"] = np.ascontiguousarray(w_uq[:, colp.reshape(-1)])
    shared["wukv"] = f(inp["w_ukv"])[0]
    shared["womla"] = f(inp["w_o_mla"])[0]
    shared["wpw"] = f(inp["w_pw"])[0]
    shared["wout"] = f(inp["w_out"])[0]
    shared["wpq"] = f(inp["w_pq"])[0]
    sk = f(inp["sub_keys"])[0].reshape(16, 128, 128)
    shared["subkT"] = np.ascontiguousarray(sk.transpose(2, 0, 1).reshape(128, 16 * 128))
    u = f(inp["u_experts"])[0]
    shared["ut"] = np.ascontiguousarray(u.reshape(128, 128, 8, 128).transpose(0, 3, 2, 1).reshape(128, 128, 1024))
    shared["vt"] = f(inp["v_experts"])[0].reshape(128, 128, 1024)
    ck, sk_, _ = _rope_tabs(np.arange(SEQ))
    ropek = np.zeros((2, 128, SEQ), np.float32)
    ropek[0, 64:96] = ck
    ropek[1, 64:96] = sk_
    shared["ropek"] = ropek

    def tomaj(v):
        return v.reshape(-1, 128).T

    maps = []
    for core in range(8):
        b, hf = core // 2, core % 2
        m = dict(shared)
        m["xk"] = np.ascontiguousarray(np.concatenate([x[b], ctx[b]], axis=0).reshape(34, 128, D))
        xo = np.zeros((17 * 128, D), np.float32)
        lo = hf * NOWN
        xo[:NOWN] = x[b, lo:lo + NOWN]
        if hf == 1:
            xo[NOWN:NOWN + 15] = x[b, lo - 15:lo]
        else:
            xo[NOWN + 15:NOWN + 30] = x[b, lo + NOWN:lo + NOWN + 15]
        m["xo"] = xo.reshape(17, 128, D)
        vecs = np.zeros((128, NV), np.float32)
        vecs[:, 0:8] = tomaj(f(inp["norm1_g"])[0])
        vecs[:, 8:16] = tomaj(f(inp["norm2_g"])[0])
        vecs[:, 16:18] = tomaj(f(inp["q_norm_g"])[0])
        vecs[:, 18:19] = tomaj(f(inp["kv_norm_g"])[0])
        vecs[:, 19:27] = tomaj(f(inp["conv_b"])[0])
        vecs[:, 27:35] = tomaj(f(inp["conv_ln_g"])[0])
        vecs[:, 35:43] = tomaj(f(inp["conv_ln_b"])[0])
        cw = f(inp["conv_w"])[0]
        vecs[:, 43:291] = cw.reshape(31, 8, 128).transpose(2, 1, 0).reshape(128, 248)
        vecs[:, 291:339] = tomaj(b_mod)
        ct2 = np.stack([tomaj(c[b]), tomaj(c_ctx)], axis=2)
        vecs[:, 339:355] = ct2.reshape(128, 16)
        vecs[:, 355] = 1.0 if hf == 1 else 0.0
        vecs[:, 356] = 1.0 if hf == 0 else 0.0
        m["vecs"] = vecs
        cq_, sq_, _ = _rope_tabs(np.arange(lo, lo + NOWN))
        ropeq = np.zeros((2, 128, NOWN), np.float32)
        ropeq[0, 0:64] = 1.0
        ropeq[0, 64:96] = cq_
        ropeq[1, 64:96] = sq_
        m["ropeq"] = ropeq
        maps.append(m)
    return maps


_NC_CACHE = {}


def kernel(**inputs):
    maps = prep_inputs(inputs)
    if "nc" not in _NC_CACHE:
        _NC_CACHE["nc"] = build_nc()
    nc = _NC_CACHE["nc"]
    res = run_bass_kernel_spmd(nc, maps, core_ids=list(range(8)))
    out = np.zeros((4, SEQ, D), np.float32)
    for core in range(8):
        b, hf = core // 2, core % 2
        out[b, hf * NOWN:(hf + 1) * NOWN] = np.asarray(res.results[core]["out"]).reshape(NOWN, D)
    return out
```

```python
import numpy as np
from contextlib import ExitStack
import concourse.bass as bass
import concourse.mybir as mybir
from concourse.bass_utils import run_bass_kernel_spmd

F32 = mybir.dt.float32
BF16 = mybir.dt.bfloat16
U32 = mybir.dt.uint32
ALU = mybir.AluOpType
AF = mybir.ActivationFunctionType
AX = mybir.AxisListType
ENGS = ("tensor", "vector", "scalar", "gpsimd", "sync")

D = 1024
SEQ = 4096
CTX = 256
NKEY = SEQ + CTX
NOWN = 2048
NH = 16
EPS = 1e-6
ATTN_SCALE = 96 ** -0.5
OFF_KV = 256
OFF_KR = 384
OFF_CONV = 416
OFF_GATE = 2464
NV = 360
CUT = 0


class _Op:
    __slots__ = ("eng", "fn", "waits", "signal", "sem", "val", "is_dma", "inc")

    def __init__(self, eng, fn):
        self.eng = eng
        self.fn = fn
        self.waits = []
        self.signal = False
        self.sem = None
        self.val = 0
        self.is_dma = False
        self.inc = 1


class Prog:
    def __init__(self, nc):
        self.nc = nc
        self.ops = {e: [] for e in ENGS}
        self.last_w = {}
        self.readers = {}
        self.all_ops = []
        self.epoch = None
        self.last_by_sem = {}

    def op(self, eng, fn, reads=(), writes=(), dma=False, sem_key=None):
        o = _Op(eng, fn)
        o.is_dma = dma
        deps = []
        for r in reads:
            w = self.last_w.get(r, self.epoch)
            if w is not None:
                deps.append((w, True))
        for r in writes:
            w = self.last_w.get(r, self.epoch)
            if w is not None:
                deps.append((w, False))
            for rd in self.readers.get(r, {}).values():
                deps.append((rd, False))
        seen = set()
        for d, raw in deps:
            if id(d) in seen:
                continue
            same = (not d.is_dma) and (not dma) and d.eng == eng
            if same and eng == "tensor":
                continue
            seen.add(id(d))
            o.waits.append(d)
            d.signal = True
        if dma:
            o.sem = ("dma", sem_key if sem_key is not None else (writes[0] if writes else reads[0]))
            o.inc = 16
            o.signal = True
        else:
            o.sem = ("eng", eng)
        for r in reads:
            self.readers.setdefault(r, {})[o.sem] = o
        for r in writes:
            self.last_w[r] = o
            self.readers[r] = {}
        self.ops[eng].append(o)
        self.all_ops.append(o)
        self.last_by_sem[o.sem] = o
        return o

    def barrier(self, fn):
        o = _Op("gpsimd", fn)
        o.sem = ("eng", "gpsimd")
        for d in self.last_by_sem.values():
            if d.eng == "gpsimd" and not d.is_dma:
                continue
            o.waits.append(d)
            d.signal = True
        o.signal = True
        self.ops["gpsimd"].append(o)
        self.all_ops.append(o)
        self.last_by_sem[o.sem] = o
        self.last_w = {}
        self.readers = {}
        self.epoch = o

    def emit(self, final_wait_eng="sync"):
        nc = self.nc
        last_dma = {}
        for o in self.all_ops:
            if o.is_dma:
                last_dma[o.sem] = o
        counters = {}
        for o in self.all_ops:
            if o.signal:
                counters[o.sem] = counters.get(o.sem, 0) + o.inc
                o.val = counters[o.sem]
        sem_keys = list(counters.keys())
        self.n_sems = len(sem_keys)
        with ExitStack() as st:
            sems = {}
            for i, k in enumerate(sem_keys):
                sems[k] = st.enter_context(nc.semaphore("s%d" % i))
            block = st.enter_context(nc.Block())

            def make(engname):
                ops = self.ops[engname]

                def body(e):
                    waited = {}
                    for o in ops:
                        for d in o.waits:
                            if waited.get(d.sem, 0) >= d.val:
                                continue
                            e.wait_ge(sems[d.sem], d.val)
                            waited[d.sem] = d.val
                        inst = o.fn(e)
                        if o.signal:
                            inst.then_inc(sems[o.sem], o.inc)
                    if engname == final_wait_eng:
                        for k, o in last_dma.items():
                            if waited.get(k, 0) < o.val:
                                e.wait_ge(sems[k], o.val)
                return body

            for engname in ENGS:
                if not self.ops[engname] and engname != final_wait_eng:
                    continue
                getattr(block, engname)(make(engname))


class Arena:
    def __init__(self, tensor, nbytes):
        self.t = tensor
        self.n = nbytes
        self.off = 0

    def alloc(self, shape_free, dt):
        es = 4 if dt in (F32, U32) else 2
        n = int(np.prod(shape_free)) * es
        n_al = (n + 63) // 64 * 64
        assert self.off + n_al <= self.n, ("arena overflow", self.off, n_al, self.n)
        v = self.t[:, self.off // 2:(self.off + n) // 2]
        self.off += n_al
        if es == 4:
            v = v.bitcast(dt)
        if len(shape_free) == 2:
            v = v.rearrange("p (a b) -> p a b", b=shape_free[1])
        elif len(shape_free) == 3:
            v = v.rearrange("p (a b c) -> p a b c", b=shape_free[1], c=shape_free[2])
        return v

    def mark(self):
        return self.off

    def release(self, m):
        self.off = m


def build_nc(stop_after=None, debug=False):
    nc = bass.Bass("TRN2", target_bir_lowering=False)

    def din(name, shape, dt=F32):
        return nc.dram_tensor(name, list(shape), dt, kind="ExternalInput").ap()

    xk = din("xk", [34, 128, D])
    xo = din("xo", [17, 128, D])
    vecs_d = din("vecs", [128, NV])
    wmod = din("wmod", [D, 6 * D])
    bmodg = din("bmodg", [128, 2048])
    fgrow_d = din("fgrow", [128, D])
    win = din("win", [D, 4512])
    wkr = din("wkr", [D, 192])
    wuq = din("wuq", [256, 1536])
    wuqp = din("wuqp", [256, 1536])
    wukv = din("wukv", [128, 2048])
    womla = din("womla", [D, D])
    wpw = din("wpw", [D, D])
    wout = din("wout", [D, D])
    wpq = din("wpq", [D, 2048])
    subkT_d = din("subkT", [128, 16 * 128])
    ut = din("ut", [128, 128, 1024])
    vt = din("vt", [128, 128, 1024])
    ropek = din("ropek", [2, 128, SEQ])
    ropeq = din("ropeq", [2, 128, NOWN])
    out_d = nc.dram_tensor("out", [16, 128, D], F32, kind="ExternalOutput").ap()
    utb = nc.dram_tensor("utb", [64, 128, 2048], BF16, kind="Internal").ap()
    vtb = nc.dram_tensor("vtb", [64, 128, 2048], BF16, kind="Internal").ap()
    x1s = nc.dram_tensor("x1s", [16, 128, D], F32, kind="Internal").ap()
    hts = nc.dram_tensor("hts", [128, 8 * NOWN], BF16, kind="Internal").ap()
    zts = nc.dram_tensor("zts", [128, 8 * NOWN], BF16, kind="Internal").ap()
    dbg = {}
    if debug:
        for nm, shp in debug.items():
            dt_ = F32
            if isinstance(shp, tuple) and len(shp) == 2 and shp[1] == "bf16":
                shp, dt_ = shp[0], BF16
            dbg[nm] = nc.dram_tensor("dbg_" + nm, list(shp), dt_, kind="ExternalOutput").ap()

    st = ExitStack()
    with st:
        def sbuf(name, shape, dt):
            return st.enter_context(nc.sbuf_tensor("sb_" + name, list(shape), dt))

        ARENA_BYTES = 172 * 1024
        arena_t = sbuf("arena", [128, ARENA_BYTES // 2], BF16)
        ar = Arena(arena_t, ARENA_BYTES)
        identb = sbuf("identb", [128, 128], BF16)
        identf = sbuf("identf", [128, 128], F32)
        onesb = sbuf("onesb", [128, 128], BF16)
        onesf = sbuf("onesf", [128, 128], F32)
        vecs = sbuf("vecs", [128, NV], F32)
        scT = sbuf("scT", [128, 8, 2], F32)
        screp = sbuf("screp", [128, 8, 128], F32)
        modT = sbuf("modT", [128, 48, 2], F32)
        A1 = sbuf("A1", [128, 8, 2], F32)
        A2 = sbuf("A2", [128, 8], F32)
        g1row = sbuf("g1row", [128, D], F32)
        g2row = sbuf("g2row", [128, D], F32)
        fgrow = sbuf("fgrow", [128, D], F32)
        stage = sbuf("stage", [128, 2, 1024], F32)
        junk = sbuf("junk", [128, 8], F32)
        ps = [st.enter_context(nc.psum_tensor("ps%d" % i, [128, 512], F32)) for i in range(8)]

        P = Prog(nc)

        def MM(out, lhsT, rhs, start=True, stop=True, r=(), w=()):
            P.op("tensor", lambda e: e.matmul(out, lhsT=lhsT, rhs=rhs, start=start, stop=stop), reads=r, writes=w)

        def TR(out, in_, ident, r=(), w=()):
            P.op("tensor", lambda e: e.transpose(out=out, in_=in_, identity=ident), reads=r, writes=w)

        def ACT(out, in_, func, r=(), w=(), scale=None, bias=None, accum=None):
            kw = {}
            if scale is not None:
                kw["scale"] = scale
            if bias is not None:
                kw["bias"] = bias
            if accum is not None:
                kw["accum_out"] = accum
            P.op("scalar", lambda e: e.activation(out=out, in_=in_, func=func, **kw), reads=r, writes=w)

        def TT(eng, out, in0, in1, op, r=(), w=()):
            P.op(eng, lambda e: e.tensor_tensor(out=out, in0=in0, in1=in1, op=op), reads=r, writes=w)

        def TS(eng, out, in0, s1, s2, op0, op1=None, r=(), w=()):
            if op1 is None:
                P.op(eng, lambda e: e.tensor_scalar(out=out, in0=in0, scalar1=s1, scalar2=None, op0=op0), reads=r, writes=w)
            else:
                P.op(eng, lambda e: e.tensor_scalar(out=out, in0=in0, scalar1=s1, scalar2=s2, op0=op0, op1=op1), reads=r, writes=w)

        def STT(out, in0, scalar, in1, op0, op1, r=(), w=()):
            P.op("vector", lambda e: e.scalar_tensor_tensor(out=out, in0=in0, scalar=scalar, in1=in1, op0=op0, op1=op1), reads=r, writes=w)

        def CP(eng, out, in_, r=(), w=()):
            if eng == "scalar":
                P.op(eng, lambda e: e.copy(out=out, in_=in_), reads=r, writes=w)
            else:
                P.op(eng, lambda e: e.tensor_copy(out=out, in_=in_), reads=r, writes=w)

        def RECIP(out, in_, r=(), w=()):
            P.op("vector", lambda e: e.reciprocal(out=out, in_=in_), reads=r, writes=w)

        def MEMSET(eng, ap, val, w=()):
            P.op(eng, lambda e: e.memset(ap, val), writes=w)

        dmaq = ["sync", "scalar"]
        dma_i = [0]

        def DMA(out, in_, r=(), w=(), q=None, sem_key=None):
            if q is None:
                q = "sync"
            P.op(q, lambda e: e.dma_start(out=out, in_=in_), reads=r, writes=w, dma=True, sem_key=sem_key)

        def BARRIER():
            P.barrier(lambda e: e.memset(junk[:, 0:1], 0.0))
            ar.off = RC

        RA, RB, RC = 0, 34 * 1024, 67 * 1024
        stage_i = [0]
        cv_i = [0]

        def load_w(dst, src, ncols, wkey, conv_eng=None):
            assert ncols <= 1024
            s = stage_i[0] % 2
            stage_i[0] += 1
            sv = stage[:, s, 0:ncols]
            if len(src.shape) == 3:
                sv = sv.rearrange("p (a b) -> p a b", b=src.shape[2])
            DMA(sv, src, w=[("stage", s)])
            if conv_eng is None:
                conv_eng = ("gpsimd", "vector")[cv_i[0] % 2]
                cv_i[0] += 1
            CP(conv_eng, dst, sv, r=[("stage", s)], w=[wkey])

        def dump(name, src, r):
            if debug and name in dbg:
                DMA(dbg[name], src, r=r, w=[("dbg", name)])

        MEMSET("gpsimd", identf[:], 0.0, w=["identf"])
        P.op("gpsimd", lambda e: e.affine_select(out=identf[:], in_=identf[:], pattern=[[-1, 128]], base=0,
                                                 channel_multiplier=1, compare_op=ALU.not_equal, fill=1.0),
             reads=["identf"], writes=["identf"])
        CP("vector", identb[:], identf[:], r=["identf"], w=["identb"])
        MEMSET("vector", onesb[:], 1.0, w=["onesb"])
        MEMSET("vector", onesf[:], 1.0, w=["onesf"])
        DMA(vecs[:], vecs_d, w=["vecs"])
        DMA(fgrow[:], fgrow_d, w=["fgrow"])
        epsb = sbuf("epsb", [128, 1], F32)
        MEMSET("vector", epsb[:], EPS, w=["epsb"])
        MEMSET("vector", modT[:], 0.0, w=["modT"])
        V_N1, V_N2, V_QG, V_KVG, V_CB, V_LNG, V_LNB, V_CW, V_BM, V_CT, V_HM = 0, 8, 16, 18, 19, 27, 35, 43, 291, 339, 355

        cT = vecs[:, V_CT:V_CT + 16].rearrange("p (k j) -> p k j", j=2)
        ACT(scT[:], cT, AF.Silu, r=["vecs"], w=["scT"])
        CP("vector", screp[:], scT[:, :, 0:1].to_broadcast([128, 8, 128]), r=["scT"], w=["screp"])
        ar.off = RC
        wmb = [ar.alloc([8, 512], F32) for _ in range(2)]
        bg = ar.alloc([2048], F32)
        DMA(bg, bmodg, w=["bg"])
        wmod_v = wmod.rearrange("(k p) f -> p k f", p=128)
        for blk in range(12):
            s = blk % 2
            DMA(wmb[s], wmod_v[:, :, blk * 512:(blk + 1) * 512], w=[("wmb", s)], q=dmaq[blk % 2])
            if blk in (4, 5, 10, 11):
                row = g1row if blk < 6 else g2row
                half = blk % 2
                for k in range(8):
                    MM(ps[0][:], screp[:, k, :], wmb[s][:, k, :], start=(k == 0), stop=(k == 7),
                       r=["screp", ("wmb", s)], w=[("ps", 0)])
                goff = (0 if blk < 6 else 1024) + half * 512
                TT("vector", row[:, half * 512:(half + 1) * 512], ps[0][:], bg[:, goff:goff + 512], ALU.add,
                   r=[("ps", 0), "bg"], w=[("row", blk)])
            else:
                for fc in range(4):
                    for k in range(8):
                        MM(ps[1][:, fc * 2:fc * 2 + 2], wmb[s][:, k, fc * 128:(fc + 1) * 128], scT[:, k, :],
                           start=(k == 0), stop=(k == 7), r=["scT", ("wmb", s)], w=[("ps", 1)])
                for fc in range(4):
                    f = blk * 4 + fc
                    TS("vector", modT[:, f, :], ps[1][:, fc * 2:fc * 2 + 2], vecs[:, V_BM + f:V_BM + f + 1], None, ALU.add,
                       r=[("ps", 1), "vecs"], w=["modT"])
        for j in range(2):
            STT(A1[:, :, j], modT[:, 8:16, j], 1.0, vecs[:, V_N1:V_N1 + 8], ALU.add, ALU.mult, r=["modT", "vecs"], w=["A1"])
        STT(A2[:], modT[:, 32:40, 0], 1.0, vecs[:, V_N2:V_N2 + 8], ALU.add, ALU.mult, r=["modT", "vecs"], w=["A2"])
        dump("modT", modT[:].rearrange("p a b -> p (a b)"), ["modT"])
        dump("g1row", g1row[:], [("row", 4), ("row", 5)])
        if stop_after == "mod":
            P.emit()
            return nc
        BARRIER()

        ar.off = RA
        ckvnT = ar.alloc([NKEY], BF16)
        KT = [ar.alloc([NKEY], BF16) for _ in range(2)]
        cqnT = ar.alloc([2, NOWN], BF16)
        assert ar.off <= RB
        ar.off = RC

        def norm_tile(src_tile, dst, j, Asc, Bsh, xt, sq, xn, ss, rs, psb, tag, dkey):
            if src_tile is not None:
                DMA(xt, src_tile, w=[(tag, "xt")], q=dmaq[j % 2])
            if CUT == 5:
                return
            ACT(sq, xt, AF.Square, r=[(tag, "xt")], w=[(tag, "sq"), (tag, "ss")], accum=ss)
            if CUT == 6:
                return
            ACT(rs, ss, AF.Sqrt, r=[(tag, "ss")], w=[(tag, "rs")], scale=1.0 / D, bias=epsb[:])
            if CUT == 7:
                return
            RECIP(rs, rs, r=[(tag, "rs")], w=[(tag, "rs")])
            TS("vector", xn, xt, rs, None, ALU.mult, r=[(tag, "xt"), (tag, "rs")], w=[(tag, "xn")])
            if CUT == 8:
                return
            pb = ps[psb][:].bitcast(BF16)
            for k in range(8):
                TR(pb[:, k * 128:(k + 1) * 128], xn[:, k * 128:(k + 1) * 128], identb[:], r=[(tag, "xn"), "identb"], w=[("ps", psb)])
            if CUT == 9:
                return
            pb3 = pb.rearrange("p (k t) -> p k t", k=8)
            tmpn = tmpns[j % 2]
            TT("vector", tmpn[:], pb3, Asc.unsqueeze(2).to_broadcast([128, 8, 128]), ALU.mult,
               r=[("ps", psb), "A1", "A2", "modT"], w=[("tmpn", j % 2)])
            TT("gpsimd", dst, tmpn[:], Bsh.unsqueeze(2).to_broadcast([128, 8, 128]), ALU.add,
               r=[("tmpn", j % 2), "A1", "A2", "modT"], w=[(dkey, 0)])

        tmpns = [sbuf("tmpn%d" % i, [128, 8, 128], F32) for i in range(2)]

        xts = [ar.alloc([D], F32) for _ in range(2)]
        sqj = ar.alloc([D], F32)
        xns = [ar.alloc([D], BF16) for _ in range(2)]
        sss = [ar.alloc([1], F32) for _ in range(2)]
        rss = [ar.alloc([1], F32) for _ in range(2)]
        hTb = [ar.alloc([8, 512], BF16) for _ in range(2)]
        wkvb = ar.alloc([8, 320], BF16)
        sqb = ar.alloc([512], BF16)
        rstd = ar.alloc([512], F32)
        rk = [ar.alloc([2, 512], F32) for _ in range(2)]
        t1 = ar.alloc([512], F32)
        t2 = ar.alloc([512], F32)
        win_v = win.rearrange("(k p) f -> p k f", p=128)
        wkr_v = wkr.rearrange("(k p) f -> p k f", p=128)
        load_w(wkvb[:, :, 0:128], win_v[:, :, OFF_KV:OFF_KV + 128], 8 * 128, ("wkvb", 0))
        load_w(wkvb[:, 0:4, 128:320], wkr_v[:, 0:4, :], 4 * 192, ("wkvb", 1))
        load_w(wkvb[:, 4:8, 128:320], wkr_v[:, 4:8, :], 4 * 192, ("wkvb", 2))
        ti = 0
        if CUT == 1:
            P.emit()
            return nc
        for blk in range(9):
            ntile = 4 if blk < 8 else 2
            n = ntile * 128
            hb = hTb[blk % 2]
            jm = 0 if blk < 8 else 1
            for tl in range(ntile):
                t = blk * 4 + tl
                s = ti % 2
                norm_tile(xk[t], hb[:, :, tl * 128:(tl + 1) * 128], ti, A1[:, :, jm], modT[:, 0:8, jm],
                          xts[s], sqj, xns[s], sss[s], rss[s], 2 + s, ("nk", s), ("hTb", blk % 2, s))
                ti += 1
            hr = [(("hTb", blk % 2, s_), 0) for s_ in range(2)]
            if CUT == 2 or CUT >= 5:
                P.emit()
                return nc
            for k in range(8):
                MM(ps[4][:, 0:n], wkvb[:, k, 0:128], hb[:, k, 0:n], start=(k == 0), stop=(k == 7),
                   r=hr + [("wkvb", 0)], w=[("ps", 4)])
            for k in range(8):
                MM(ps[5][0:96, 0:n], wkvb[:, k, 128:224], hb[:, k, 0:n], start=(k == 0), stop=(k == 7),
                   r=hr + [("wkvb", 1), ("wkvb", 2)], w=[("ps", 5)])
            if blk < 8:
                for k in range(8):
                    MM(ps[6][0:96, 0:n], wkvb[:, k, 224:320], hb[:, k, 0:n], start=(k == 0), stop=(k == 7),
                       r=hr + [("wkvb", 1), ("wkvb", 2)], w=[("ps", 6)])
            if CUT == 3:
                P.emit()
                return nc
            ACT(sqb[:, 0:n], ps[4][:, 0:n], AF.Square, r=[("ps", 4)], w=["sqb"])
            MM(ps[7][:, 0:n], onesb[:], sqb[:, 0:n], r=["onesb", "sqb"], w=[("ps", 7)])
            ACT(rstd[:, 0:n], ps[7][:, 0:n], AF.Sqrt, r=[("ps", 7)], w=["rstd"], scale=1.0 / 128, bias=epsb[:])
            RECIP(rstd[:, 0:n], rstd[:, 0:n], r=["rstd"], w=["rstd"])
            STT(ckvnT[:, blk * 512:blk * 512 + n], ps[4][:, 0:n], vecs[:, V_KVG:V_KVG + 1], rstd[:, 0:n], ALU.mult, ALU.mult,
                r=[("ps", 4), "rstd", "vecs"], w=[("ckvnT", blk)])
            if CUT == 4:
                P.emit()
                return nc
            if blk < 8:
                rkb = rk[blk % 2]
                DMA(rkb[64:96, 0, :], ropek[0, 64:96, blk * 512:(blk + 1) * 512], w=[("rk", blk % 2, 0)], q="scalar")
                DMA(rkb[64:96, 1, :], ropek[1, 64:96, blk * 512:(blk + 1) * 512], w=[("rk", blk % 2, 1)], q="scalar")
                TT("vector", t1[64:96, :], ps[5][64:96, :], rkb[64:96, 0, :], ALU.mult, r=[("ps", 5), ("rk", blk % 2, 0)], w=["t1"])
                TT("vector", t2[64:96, :], ps[6][64:96, :], rkb[64:96, 1, :], ALU.mult, r=[("ps", 6), ("rk", blk % 2, 1)], w=["t2"])
                TT("vector", KT[0][64:96, blk * 512:(blk + 1) * 512], t1[64:96, :], t2[64:96, :], ALU.add, r=["t1", "t2"], w=[("KTr", 0, blk)])
                CP("gpsimd", KT[1][64:96, blk * 512:(blk + 1) * 512], KT[0][64:96, blk * 512:(blk + 1) * 512], r=[("KTr", 0, blk)], w=[("KTr", 1, blk)])
            else:
                CP("vector", KT[0][64:96, blk * 512:blk * 512 + n], ps[5][64:96, 0:n], r=[("ps", 5)], w=[("KTr", 0, blk)])
                CP("gpsimd", KT[1][64:96, blk * 512:blk * 512 + n], KT[0][64:96, blk * 512:blk * 512 + n], r=[("KTr", 0, blk)], w=[("KTr", 1, blk)])
        if debug:
            dk = ar.alloc([NKEY], F32)
            CP("vector", dk[:], ckvnT[:], r=[("ckvnT", b) for b in range(9)], w=["dk"])
            dump("ckvnT", dk[:], ["dk"])
            dk2 = ar.alloc([NKEY], F32)
            CP("vector", dk2[64:96, :], KT[1][64:96, :], r=[("KTr", 1, b) for b in range(9)], w=["dk2"])
            dump("krot", dk2[64:96, :], ["dk2"])
        if stop_after == "K":
            P.emit()
            return nc
        BARRIER()

        ar.off = RB
        ypad = ar.alloc([8, NOWN + 30], BF16)
        assert ar.off <= RC
        ar.off = RC
        xts = [ar.alloc([D], F32) for _ in range(2)]
        sqj = ar.alloc([D], F32)
        xns = [ar.alloc([D], BF16) for _ in range(2)]
        sss = [ar.alloc([1], F32) for _ in range(2)]
        rss = [ar.alloc([1], F32) for _ in range(2)]
        hTo = ar.alloc([8, 17 * 128], BF16)
        wch = [ar.alloc([8, 128], BF16) for _ in range(4)]
        sg = [ar.alloc([512], F32) for _ in range(2)]
        yh = ar.alloc([8, 128], BF16)
        sqb2 = ar.alloc([2, 512], BF16)
        rstd = ar.alloc([512], F32)
        for t in range(17):
            s = t % 2
            norm_tile(xo[t], hTo[:, :, t * 128:(t + 1) * 128], t, A1[:, :, 0], modT[:, 0:8, 0],
                      xts[s], sqj, xns[s], sss[s], rss[s], 2 + s, ("no", s), ("hTo", t))
        DMA(hts.rearrange("p (k t) -> p k t", k=8), hTo[:, :, 0:NOWN],
            r=[(("hTo", t), 0) for t in range(16)], w=["hts"])
        blocks = [(tb * 512, 512, [4 * tb + i for i in range(4)]) for tb in range(4)] + [(2048, 128, [16])]
        load_w(wch[0], win_v[:, :, 0:128], 1024, ("wch", 0))
        load_w(wch[1], win_v[:, :, 128:256], 1024, ("wch", 1))
        for bi in range(4):
            t0, n, tiles = blocks[bi]
            hr = [(("hTo", t), 0) for t in tiles]
            for cc in range(2):
                for k in range(8):
                    MM(ps[4 + cc][:], wch[cc][:, k, :], hTo[:, k, t0:t0 + n], start=(k == 0), stop=(k == 7),
                       r=hr + [("wch", cc)], w=[("ps", 4 + cc)])
                ACT(sqb2[:, cc, :], ps[4 + cc][:], AF.Square, r=[("ps", 4 + cc)], w=[("sqb2", cc)])
            MM(ps[6][:], onesb[:], sqb2[:, 0, :], start=True, stop=False, r=[("sqb2", 0)], w=[("ps", 6)])
            MM(ps[6][:], onesb[:], sqb2[:, 1, :], start=False, stop=True, r=[("sqb2", 1)], w=[("ps", 6)])
            ACT(rstd[:], ps[6][:], AF.Sqrt, r=[("ps", 6)], w=["rstd"], scale=1.0 / 256, bias=epsb[:])
            RECIP(rstd[:], rstd[:], r=["rstd"], w=["rstd"])
            for cc in range(2):
                STT(cqnT[:, cc, t0:t0 + n], ps[4 + cc][:], vecs[:, V_QG + cc:V_QG + cc + 1], rstd[:], ALU.mult, ALU.mult,
                    r=[("ps", 4 + cc), "rstd"], w=[("cqnT", cc, bi)])
        for c in range(8):
            wa, wg = (2 * c) % 4, (2 * c + 1) % 4
            load_w(wch[wa], win_v[:, :, OFF_CONV + c * 128:OFF_CONV + (c + 1) * 128], 1024, ("wch", wa))
            load_w(wch[wg], win_v[:, :, OFF_CONV + 1024 + c * 128:OFF_CONV + 1024 + (c + 1) * 128], 1024, ("wch", wg))
            for bi, (t0, n, tiles) in enumerate(blocks):
                hr = [(("hTo", t), 0) for t in tiles]
                ba, bg_ = (4, 5) if bi % 2 == 0 else (6, 7)
                for k in range(8):
                    MM(ps[ba][:, 0:n], wch[wa][:, k, :], hTo[:, k, t0:t0 + n], start=(k == 0), stop=(k == 7),
                       r=hr + [("wch", wa)], w=[("ps", ba)])
                for k in range(8):
                    MM(ps[bg_][:, 0:n], wch[wg][:, k, :], hTo[:, k, t0:t0 + n], start=(k == 0), stop=(k == 7),
                       r=hr + [("wch", wg)], w=[("ps", bg_)])
                sgb = sg[bi % 2]
                ACT(sgb[:, 0:n], ps[bg_][:, 0:n], AF.Sigmoid, r=[("ps", bg_)], w=[("sg", bi % 2)])
                dst = ypad[:, c, 15 + t0:15 + t0 + n] if bi < 4 else yh[:, c, :]
                TT("vector", dst, ps[ba][:, 0:n], sgb[:, 0:n], ALU.mult, r=[("ps", ba), ("sg", bi % 2)],
                   w=[("ypad", c, bi)])
            TS("vector", ypad[:, c, 0:15], yh[:, c, 0:15], vecs[:, V_HM:V_HM + 1], None, ALU.mult,
               r=[("ypad", c, 4), "vecs"], w=[("ypad", c, 5)])
            TS("vector", ypad[:, c, NOWN + 15:NOWN + 30], yh[:, c, 15:30], vecs[:, V_HM + 1:V_HM + 2], None, ALU.mult,
               r=[("ypad", c, 4), "vecs"], w=[("ypad", c, 6)])
        if debug:
            dump("cqnT", cqnT[:].rearrange("p a b -> p (a b)"), [("cqnT", cc, bi) for cc in range(2) for bi in range(4)])
            dump("ypad", ypad[:].rearrange("p a b -> p (a b)"), [("ypad", c, i) for c in range(8) for i in range(7)])
        if stop_after == "O":
            P.emit()
            return nc
        BARRIER()

        dg = [ar.alloc([31, 128], BF16) for _ in range(2)]
        vT = ar.alloc([8, NOWN], BF16)
        sqv = [ar.alloc([512], BF16) for _ in range(2)]
        mean = ar.alloc([512], F32)
        msq = ar.alloc([512], F32)
        rstdc = ar.alloc([512], F32)
        tmpc = [ar.alloc([512], F32) for _ in range(2)]
        for c in range(8):
            d_ = dg[c % 2]
            TT("vector", d_[:], identb[:].unsqueeze(1).to_broadcast([128, 31, 128]),
               vecs[:, V_CW + c * 31:V_CW + (c + 1) * 31].unsqueeze(2).to_broadcast([128, 31, 128]), ALU.mult,
               r=[], w=[("dg", c % 2)])
            for tb in range(4):
                bank = 4 + (tb % 2)
                for k in range(31):
                    MM(ps[bank][:], d_[:, k, :], ypad[:, c, tb * 512 + k:tb * 512 + k + 512], start=(k == 0), stop=(k == 30),
                       r=[("dg", c % 2)], w=[("ps", bank)])
                ACT(vT[:, c, tb * 512:(tb + 1) * 512], ps[bank][:], AF.Identity, r=[("ps", bank)], w=[("vT", c, tb)],
                    bias=vecs[:, V_CB + c:V_CB + c + 1])
        for tb in range(4):
            tsl = slice(tb * 512, (tb + 1) * 512)
            for c in range(8):
                sv = sqv[c % 2]
                ACT(sv[:], vT[:, c, tsl], AF.Square, r=[("vT", c, tb)], w=[("sqv", c % 2)])
                MM(ps[6][:], onesb[:], vT[:, c, tsl], start=(c == 0), stop=(c == 7), r=[("vT", c, tb)], w=[("ps", 6)])
                MM(ps[7][:], onesb[:], sv[:], start=(c == 0), stop=(c == 7), r=[("sqv", c % 2)], w=[("ps", 7)])
            ACT(mean[:], ps[6][:], AF.Identity, r=[("ps", 6)], w=["mean"], scale=1.0 / D)
            ACT(msq[:], ps[7][:], AF.Identity, r=[("ps", 7)], w=["msq"], scale=1.0 / D)
            TT("vector", rstdc[:], mean[:], mean[:], ALU.mult, r=["mean"], w=["rstdc"])
            TT("vector", rstdc[:], msq[:], rstdc[:], ALU.subtract, r=["msq", "rstdc"], w=["rstdc"])
            ACT(rstdc[:], rstdc[:], AF.Sqrt, r=["rstdc"], w=["rstdc"], bias=epsb[:])
            RECIP(rstdc[:], rstdc[:], r=["rstdc"], w=["rstdc"])
            for c in range(8):
                tm = tmpc[c % 2]
                TT("vector", tm[:], vT[:, c, tsl], mean[:], ALU.subtract, r=[("vT", c, tb), "mean"], w=[("tmpc", c % 2)])
                TT("vector", tm[:], tm[:], rstdc[:], ALU.mult, r=[("tmpc", c % 2), "rstdc"], w=[("tmpc", c % 2)])
                ACT(vT[:, c, tsl], tm[:], AF.Silu, r=[("tmpc", c % 2)], w=[("vT", c, tb)],
                    scale=vecs[:, V_LNG + c:V_LNG + c + 1], bias=vecs[:, V_LNB + c:V_LNB + c + 1])
        zkeys = [("vT", c, tb) for c in range(8) for tb in range(4)]
        DMA(zts.rearrange("p (k t) -> p k t", k=8), vT[:], r=zkeys, w=["zts"])
        if debug:
            dump("zT", vT[:].rearrange("p a b -> p (a b)"), zkeys)
        if stop_after == "C":
            P.emit()
            return nc
        BARRIER()

        ar.off = RB
        OT = ar.alloc([8, NOWN], BF16)
        assert ar.off <= RC
        ar.off = RC
        VH = [ar.alloc([34, 128], BF16) for _ in range(2)]
        QT = [ar.alloc([NOWN], BF16) for _ in range(2)]
        PT = [ar.alloc([512], BF16) for _ in range(4)]
        wuqb = ar.alloc([2, 1536], BF16)
        wuqpb = ar.alloc([2, 1536], BF16)
        wukvb = ar.alloc([2048], BF16)
        rq = ar.alloc([2, NOWN], F32)
        qt1 = ar.alloc([512], F32)
        qt2 = ar.alloc([512], F32)
        rden = ar.alloc([512], F32)
        rdenB = ar.alloc([512], F32)
        wuq_v = wuq.rearrange("(k p) f -> p k f", p=128)
        wuqp_v = wuqp.rearrange("(k p) f -> p k f", p=128)
        for k in range(2):
            for hh in range(2):
                load_w(wuqb[:, k, hh * 768:(hh + 1) * 768], wuq_v[:, k, hh * 768:(hh + 1) * 768], 768, ("wuqb", k, hh))
                load_w(wuqpb[:, k, hh * 768:(hh + 1) * 768], wuqp_v[:, k, hh * 768:(hh + 1) * 768], 768, ("wuqpb", k, hh))
        for hh in range(2):
            load_w(wukvb[:, hh * 1024:(hh + 1) * 1024], wukv[:, hh * 1024:(hh + 1) * 1024], 1024, ("wukvb", hh))
        wq_keys = [("wuqb", k, hh) for k in range(2) for hh in range(2)]
        wqp_keys = [("wuqpb", k, hh) for k in range(2) for hh in range(2)]
        wkv_keys = [("wukvb", 0), ("wukvb", 1)]
        DMA(rq[0:96, 0, :], ropeq[0, 0:96, :], w=[("rq", 0)])
        DMA(rq[0:96, 1, :], ropeq[1, 0:96, :], w=[("rq", 1)], q="scalar")
        for hb in range(2):
            MEMSET("gpsimd", VH[hb][:], 0.0, w=[("VH", hb)])
        MEMSET("gpsimd", VH[0][:, :, 64:65], 1.0, w=[("VH", 0)])
        MEMSET("gpsimd", VH[1][:, :, 0:1], 1.0, w=[("VH", 1)])
        ckeys = [("ckvnT", b_) for b_ in range(9)]
        cvf = [ar.alloc([2, 1024], F32) for _ in range(2)]
        cvb = [ar.alloc([2, 1024], BF16) for _ in range(2)]
        cv_steps = [(tab, tabb, nm, g) for (tab, tabb, nm) in ((ut, utb, "u"), (vt, vtb, "v")) for g in range(64)]
        cv_pos = [0]

        def conv_step():
            if cv_pos[0] >= len(cv_steps):
                return
            tab, tabb, nm, g = cv_steps[cv_pos[0]]
            s = cv_pos[0] % 2
            DMA(cvf[s][:], tab[g * 2:(g + 1) * 2].rearrange("i p f -> p i f"), w=[("cvf", s)], q="sync")
            CP(("gpsimd", "vector")[cv_pos[0] % 2], cvb[s][:], cvf[s][:], r=[("cvf", s)], w=[("cvb", s)])
            DMA(tabb[g], cvb[s][:].rearrange("p a b -> p (a b)"), r=[("cvb", s)], w=[("tabb", nm, g)], q="sync",
                sem_key=("tabb", s))
            cv_pos[0] += 1

        def prep_steps(h):
            hb = h % 2
            voff = 0 if hb == 0 else 64
            steps = []
            for blk in range(9):
                def st_k(blk=blk):
                    n = 512 if blk < 8 else 256
                    bank = 5 + (blk % 2)
                    MM(ps[bank][0:64, 0:n], wukvb[:, h * 128:h * 128 + 64], ckvnT[:, blk * 512:blk * 512 + n],
                       r=wkv_keys + ckeys, w=[("ps", bank)])
                    CP("vector", KT[hb][0:64, blk * 512:blk * 512 + n], ps[bank][0:64, 0:n], r=[("ps", bank)], w=[("KTn", hb)])
                steps.append(st_k)
            for g in range(5):
                def st_v(g=g):
                    cnt = 8 if g < 4 else 2
                    bank = 5 + (g % 2)
                    for j in range(cnt):
                        kt = g * 8 + j
                        MM(ps[bank][:, j * 64:(j + 1) * 64], ckvnT[:, kt * 128:(kt + 1) * 128], wukvb[:, h * 128 + 64:h * 128 + 128],
                           r=wkv_keys + ckeys, w=[("ps", bank)])
                    CP("vector", VH[hb][:, g * 8:g * 8 + cnt, voff:voff + 64],
                       ps[bank][:, 0:cnt * 64].rearrange("p (a b) -> p a b", b=64), r=[("ps", bank)], w=[("VH", hb)])
                steps.append(st_v)
            for qb in range(4):
                def st_q(qb=qb):
                    qs = slice(qb * 512, (qb + 1) * 512)
                    for k in range(2):
                        MM(ps[5][0:96, :], wuqb[:, k, h * 96:(h + 1) * 96], cqnT[:, k, qs], start=(k == 0), stop=(k == 1),
                           r=wq_keys + [("cqnT", k, qb)], w=[("ps", 5)])
                    for k in range(2):
                        MM(ps[6][0:96, :], wuqpb[:, k, h * 96:(h + 1) * 96], cqnT[:, k, qs], start=(k == 0), stop=(k == 1),
                           r=wqp_keys + [("cqnT", k, qb)], w=[("ps", 6)])
                    TT("vector", qt1[0:96, :], ps[5][0:96, :], rq[0:96, 0, qs], ALU.mult, r=[("ps", 5), ("rq", 0)], w=["qt1"])
                    TT("vector", qt2[0:96, :], ps[6][0:96, :], rq[0:96, 1, qs], ALU.mult, r=[("ps", 6), ("rq", 1)], w=["qt2"])
                    TT("vector", QT[hb][0:96, qs], qt1[0:96, :], qt2[0:96, :], ALU.add, r=["qt1", "qt2"], w=[("QT", hb)])
                steps.append(st_q)
            return steps

        def prep(h):
            for st_ in prep_steps(h):
                st_()

        prep(0)
        gi = [0]
        for h in range(NH):
            hb = h % 2
            ktr = [("KTn", hb)] + [("KTr", hb, b_) for b_ in range(9)]
            items = [(qb, kt) for qb in range(4) for kt in range(34)]
            base = gi[0]

            def S(idx):
                qb, kt = items[idx]
                sb_ = (base + idx) % 3
                MM(ps[sb_][:], KT[hb][0:96, kt * 128:(kt + 1) * 128], QT[hb][0:96, qb * 512:(qb + 1) * 512],
                   r=ktr + [("QT", hb)], w=[("ps", sb_)])

            pending = []
            nsteps = prep_steps(h + 1) if h + 1 < NH else []
            S(0)
            S(1)
            for idx, (qb, kt) in enumerate(items):
                if idx + 2 < len(items):
                    S(idx + 2)
                sb_ = (base + idx) % 3
                pb_ = (base + idx) % 4
                ob = 3 + (qb % 2)
                qs = slice(qb * 512, (qb + 1) * 512)
                ACT(PT[pb_][:], ps[sb_][:], AF.Exp, r=[("ps", sb_)], w=[("PT", pb_)], scale=ATTN_SCALE)
                MM(ps[ob][:], VH[hb][:, kt, :], PT[pb_][:], start=(kt == 0), stop=(kt == 33),
                   r=[("VH", hb), ("PT", pb_)], w=[("ps", ob)])
                if pending and pending[0][0] <= idx:
                    _, (r0, prow, ob2, qs2) = pending.pop(0)
                    MM(ps[7][:], onesf[r0:r0 + 1, :], rden[r0:r0 + 1, :], r=["rden"], w=[("ps", 7)])
                    CP("vector", rdenB[:], ps[7][:], r=[("ps", 7)], w=["rdenB"])
                    TT("vector", OT[prow, h // 2, qs2], ps[ob2][prow, :], rdenB[prow, :], ALU.mult, r=[("ps", ob2), "rdenB"],
                       w=[("OT", h, qs2.start // 512)])
                if kt == 33:
                    r0 = 64 if hb == 0 else 0
                    prow = slice(0, 64) if hb == 0 else slice(64, 128)
                    RECIP(rden[r0:r0 + 1, :], ps[ob][r0:r0 + 1, :], r=[("ps", ob)], w=["rden"])
                    pending.append((idx + 6 if qb < 3 else idx, (r0, prow, ob, qs)))
                if idx % 17 == 8:
                    conv_step()
                if idx >= 10 and idx % 5 == 0 and nsteps:
                    nsteps.pop(0)()
            while pending:
                _, (r0, prow, ob2, qs2) = pending.pop(0)
                MM(ps[7][:], onesf[r0:r0 + 1, :], rden[r0:r0 + 1, :], r=["rden"], w=[("ps", 7)])
                CP("vector", rdenB[:], ps[7][:], r=[("ps", 7)], w=["rdenB"])
                TT("vector", OT[prow, h // 2, qs2], ps[ob2][prow, :], rdenB[prow, :], ALU.mult, r=[("ps", ob2), "rdenB"],
                   w=[("OT", h, qs2.start // 512)])
            while nsteps:
                nsteps.pop(0)()
            gi[0] += len(items)
        while cv_pos[0] < len(cv_steps):
            conv_step()
        okeys = [("OT", h, qb) for h in range(NH) for qb in range(4)]
        if debug:
            dump("OT", OT[:].rearrange("p a b -> p (a b)"), okeys)
        if stop_after == "ATT":
            P.emit()
            return nc
        BARRIER()

        ar.off = RA
        mT = ar.alloc([8, NOWN], BF16)
        assert ar.off <= RB
        ar.off = RC
        hTo2 = ar.alloc([8, NOWN], BF16)
        zT2 = ar.alloc([8, NOWN], BF16)
        DMA(hTo2[:], hts.rearrange("p (k t) -> p k t", k=8), w=["hTo2"])
        DMA(zT2[:], zts.rearrange("p (k t) -> p k t", k=8), w=["zT2"], q="scalar")
        wm = [[ar.alloc([8, 128], BF16) for _ in range(4)] for _ in range(2)]
        sga = [ar.alloc([512], F32) for _ in range(2)]
        sgc = [ar.alloc([512], F32) for _ in range(2)]
        m1 = [ar.alloc([512], F32) for _ in range(2)]
        womla_v = womla.rearrange("(k p) f -> p k f", p=128)
        wpw_v = wpw.rearrange("(k p) f -> p k f", p=128)
        for c in range(8):
            ws = c % 2
            cs = slice(c * 128, (c + 1) * 128)
            load_w(wm[ws][0], womla_v[:, :, cs], 1024, ("wm", ws, 0))
            load_w(wm[ws][1], wpw_v[:, :, cs], 1024, ("wm", ws, 1))
            load_w(wm[ws][2], win_v[:, :, OFF_GATE + c * 128:OFF_GATE + (c + 1) * 128], 1024, ("wm", ws, 2))
            load_w(wm[ws][3], win_v[:, :, OFF_GATE + 1024 + c * 128:OFF_GATE + 1024 + (c + 1) * 128], 1024, ("wm", ws, 3))
            for tb in range(4):
                tsl = slice(tb * 512, (tb + 1) * 512)
                banks = (0, 1, 2, 3) if tb % 2 == 0 else (4, 5, 6, 7)
                srcs = [(OT, "OT"), (zT2, "zT2"), (hTo2, "hTo2"), (hTo2, "hTo2")]
                for i in range(4):
                    for k in range(8):
                        MM(ps[banks[i]][:], wm[ws][i][:, k, :], srcs[i][0][:, k, tsl], start=(k == 0), stop=(k == 7),
                           r=[("wm", ws, i), srcs[i][1]], w=[("ps", banks[i])])
                p2 = tb % 2
                ACT(sga[p2][:], ps[banks[2]][:], AF.Sigmoid, r=[("ps", banks[2])], w=[("sga", p2)])
                ACT(sgc[p2][:], ps[banks[3]][:], AF.Sigmoid, r=[("ps", banks[3])], w=[("sgc", p2)])
                TT("vector", m1[p2][:], ps[banks[0]][:], sga[p2][:], ALU.mult, r=[("ps", banks[0]), ("sga", p2)], w=[("m1", p2)])
                TT("vector", sgc[p2][:], ps[banks[1]][:], sgc[p2][:], ALU.mult, r=[("ps", banks[1]), ("sgc", p2)], w=[("sgc", p2)])
                TT("vector", mT[:, c, tsl], m1[p2][:], sgc[p2][:], ALU.add, r=[("m1", p2), ("sgc", p2)], w=[("mT", c, tb)])
        if stop_after == "M":
            P.emit()
            return nc
        BARRIER()

        ar.off = RB
        h2T = ar.alloc([8, NOWN], BF16)
        assert ar.off <= RC
        ar.off = RC
        woutb = ar.alloc([8, 1024], BF16)
        wout_v = wout.rearrange("(k p) f -> p k f", p=128)
        for k in range(8):
            load_w(woutb[:, k, :], wout_v[:, k, :], 1024, ("woutb", k))
        wok = [("woutb", k) for k in range(8)]
        xts = [ar.alloc([D], F32) for _ in range(2)]
        x1t = [ar.alloc([D], F32) for _ in range(2)]
        sqj = ar.alloc([D], F32)
        xns = [ar.alloc([D], BF16) for _ in range(2)]
        sss = [ar.alloc([1], F32) for _ in range(2)]
        rss = [ar.alloc([1], F32) for _ in range(2)]
        for tt in range(16):
            s = tt % 2
            DMA(xts[s][:], xo[tt], w=[("xo2", s)], q=dmaq[tt % 2])
            for half in range(2):
                bank = (4 + half) if s == 0 else (6 + half)
                hs = slice(half * 512, (half + 1) * 512)
                for k in range(8):
                    MM(ps[bank][:], mT[:, k, tt * 128:(tt + 1) * 128], woutb[:, k, hs], start=(k == 0), stop=(k == 7),
                       r=wok + ["mT"], w=[("ps", bank)])
                TT("vector", x1t[s][:, hs], ps[bank][:], g1row[:, hs], ALU.mult, r=[("ps", bank)], w=[("x1t", s, half)])
                TT("gpsimd", x1t[s][:, hs], x1t[s][:, hs], xts[s][:, hs], ALU.add, r=[("x1t", s, half), ("xo2", s)], w=[("x1t", s, half)])
            DMA(x1s[tt], x1t[s][:], r=[("x1t", s, 0), ("x1t", s, 1)], w=[("x1s", tt)])
            P.op("gpsimd", lambda e: e.memset(junk[:, 1:2], 0.0), reads=[("x1t", s, 0), ("x1t", s, 1)], writes=[(("n2", s), "xt")])
            norm_tile(None, h2T[:, :, tt * 128:(tt + 1) * 128], tt, A2[:], modT[:, 24:32, 0],
                      x1t[s][:], sqj[:], xns[s][:], sss[s][:], rss[s][:], 2 + s, ("n2", s), ("h2T", tt))
        if stop_after == "M2":
            P.emit()
            return nc
        BARRIER()

        ar.off = RA
        E1T = ar.alloc([NOWN], BF16)
        E2T = ar.alloc([NOWN], BF16)
        WT = ar.alloc([NOWN], F32)
        ar.off = RC
        subkb = ar.alloc([16, 128], BF16)
        wpqc = [ar.alloc([8, 128], BF16) for _ in range(2)]
        qT = ar.alloc([16, 512], BF16)
        scs = [ar.alloc([16, 128], F32) for _ in range(2)]
        sc2 = ar.alloc([16, 128], F32)
        stop_ = ar.alloc([16, 16], F32)
        itopu = ar.alloc([16, 16], U32)
        itopf = ar.alloc([16, 16], F32)
        cand = ar.alloc([8, 256], F32)
        cand2 = ar.alloc([8, 256], F32)
        best = ar.alloc([8, 16], F32)
        ciu = ar.alloc([8, 16], U32)
        cif = ar.alloc([8, 16], F32)
        c1f = ar.alloc([8, 16], F32)
        c2f = ar.alloc([8, 16], F32)
        eq = ar.alloc([8, 16, 16], F32)
        e1f = ar.alloc([8, 16], F32)
        e2f = ar.alloc([8, 16], F32)
        ex = ar.alloc([8, 16], F32)
        se = ar.alloc([8], F32)
        wg = ar.alloc([8, 16], F32)
        iota16 = ar.alloc([16], F32)
        thr16 = ar.alloc([16], F32)
        P.op("gpsimd", lambda e: e.iota(iota16[:], pattern=[[1, 16]], base=0, channel_multiplier=0,
                                        allow_small_or_imprecise_dtypes=True), writes=["iota16"])
        P.op("gpsimd", lambda e: e.iota(thr16[:], pattern=[[16, 16]], base=16, channel_multiplier=0,
                                        allow_small_or_imprecise_dtypes=True), writes=["thr16"])
        MEMSET("gpsimd", thr16[:, 15:16], 1.0e9, w=["thr16"])
        load_w(subkb[:, 0:8, :], subkT_d[:, 0:1024].rearrange("p (a b) -> p a b", b=128), 1024, ("subkb", 0))
        load_w(subkb[:, 8:16, :], subkT_d[:, 1024:2048].rearrange("p (a b) -> p a b", b=128), 1024, ("subkb", 1))
        wpq_v = wpq.rearrange("(k p) f -> p k f", p=128)
        itop_v = itopf[:].rearrange("p (h two) m -> p h two m", two=2)
        stop_v = stop_[:].rearrange("p (h two) m -> p h two m", two=2)
        B4 = [128, 8, 16, 16]
        for tb in range(4):
            for j in range(16):
                ws = j % 2
                load_w(wpqc[ws], wpq_v[:, :, j * 128:(j + 1) * 128], 1024, ("wpqc", ws))
                for k in range(8):
                    MM(ps[4 + ws][:], wpqc[ws][:, k, :], h2T[:, k, tb * 512:(tb + 1) * 512], start=(k == 0), stop=(k == 7),
                       r=[("wpqc", ws), "h2T"], w=[("ps", 4 + ws)])
                CP("vector" if j % 2 else "scalar", qT[:, j, :], ps[4 + ws][:], r=[("ps", 4 + ws)], w=[("qT", j)])
            def score(tt_):
                tl_ = tt_ % 4
                sc_ = scs[tt_ % 2]
                for j in range(16):
                    MM(ps[j // 4][:, (j % 4) * 128:(j % 4 + 1) * 128], qT[:, j, tl_ * 128:(tl_ + 1) * 128], subkb[:, j, :],
                       r=[("qT", j), ("subkb", 0), ("subkb", 1)], w=[("ps", j // 4)])
                for g in range(4):
                    CP("scalar", sc_[:, g * 4:(g + 1) * 4, :], ps[g][:].rearrange("p (a b) -> p a b", b=128), r=[("ps", g)],
                       w=[("sc", tt_ % 2, g)])

            score(tb * 4)
            for tl in range(4):
                tt = tb * 4 + tl
                if tl < 3:
                    score(tt + 1)
                sc = scs[tt % 2]
                for j in range(16):
                    g = j // 4
                    skey = ("sc", tt % 2, g)
                    P.op("vector", lambda e, j=j, sc=sc: e.max(out=stop_[:, j, 0:8], in_=sc[:, j, :]), reads=[skey], writes=[("stop", j)])
                    P.op("vector", lambda e, j=j, sc=sc: e.max_index(out=itopu[:, j, 0:8], in_max=stop_[:, j, 0:8], in_values=sc[:, j, :]),
                         reads=[skey, ("stop", j)], writes=[("itopu", j)])
                    P.op("vector", lambda e, j=j, sc=sc: e.match_replace(out=sc2[:, j, :], in_to_replace=stop_[:, j, 0:8], in_values=sc[:, j, :], imm_value=-1.0e30),
                         reads=[skey, ("stop", j)], writes=[("sc2", j)])
                    P.op("vector", lambda e, j=j: e.max(out=stop_[:, j, 8:16], in_=sc2[:, j, :]), reads=[("sc2", j)], writes=[("stop", j)])
                    P.op("vector", lambda e, j=j: e.max_index(out=itopu[:, j, 8:16], in_max=stop_[:, j, 8:16], in_values=sc2[:, j, :]),
                         reads=[("sc2", j), ("stop", j)], writes=[("itopu", j)])
                sk = [("stop", j) for j in range(16)]
                ik = [("itopu", j) for j in range(16)]
                CP("vector", itopf[:], itopu[:], r=ik, w=["itopf"])
                TT("vector", cand[:].rearrange("p h (a b) -> p h a b", b=16), stop_v[:, :, 0, :].unsqueeze(3).to_broadcast(B4),
                   stop_v[:, :, 1, :].unsqueeze(2).to_broadcast(B4), ALU.add, r=sk, w=["cand"])
                for h in range(8):
                    P.op("vector", lambda e, h=h: e.max(out=best[:, h, 0:8], in_=cand[:, h, :]), reads=["cand"], writes=[("best", h)])
                    P.op("vector", lambda e, h=h: e.max_index(out=ciu[:, h, 0:8], in_max=best[:, h, 0:8], in_values=cand[:, h, :]),
                         reads=["cand", ("best", h)], writes=[("ciu", h)])
                    P.op("vector", lambda e, h=h: e.match_replace(out=cand2[:, h, :], in_to_replace=best[:, h, 0:8], in_values=cand[:, h, :], imm_value=-1.0e30),
                         reads=["cand", ("best", h)], writes=[("cand2", h)])
                    P.op("vector", lambda e, h=h: e.max(out=best[:, h, 8:16], in_=cand2[:, h, :]), reads=[("cand2", h)], writes=[("best", h)])
                    P.op("vector", lambda e, h=h: e.max_index(out=ciu[:, h, 8:16], in_max=best[:, h, 8:16], in_values=cand2[:, h, :]),
                         reads=[("cand2", h), ("best", h)], writes=[("ciu", h)])
                bk = [("best", h) for h in range(8)]
                ck = [("ciu", h) for h in range(8)]
                CP("vector", cif[:], ciu[:], r=ck, w=["cif"])
                TT("vector", eq[:], cif[:].unsqueeze(3).to_broadcast(B4), thr16[:].unsqueeze(1).unsqueeze(1).to_broadcast(B4), ALU.is_ge,
                   r=["cif", "thr16"], w=["eq"])
                P.op("vector", lambda e: e.tensor_reduce(out=c1f[:], in_=eq[:], axis=AX.X, op=ALU.add), reads=["eq"], writes=["c1f"])
                STT(c2f[:], c1f[:], -16.0, cif[:], ALU.mult, ALU.add, r=["c1f", "cif"], w=["c2f"])
                for (cf_, two, ef_) in ((c1f, 0, e1f), (c2f, 1, e2f)):
                    TT("vector", eq[:], cf_[:].unsqueeze(3).to_broadcast(B4), iota16[:].unsqueeze(1).unsqueeze(1).to_broadcast(B4), ALU.is_equal,
                       r=["c1f", "c2f", "iota16"], w=["eq"])
                    TT("vector", eq[:], eq[:], itop_v[:, :, two, :].unsqueeze(2).to_broadcast(B4), ALU.mult, r=["eq", "itopf"], w=["eq"])
                    P.op("vector", lambda e, ef_=ef_: e.tensor_reduce(out=ef_[:], in_=eq[:], axis=AX.X, op=ALU.add), reads=["eq"], writes=[("ef", two)])
                TT("vector", ex[:], best[:], best[:, :, 0:1].to_broadcast([128, 8, 16]), ALU.subtract, r=bk, w=["ex"])
                ACT(ex[:], ex[:], AF.Exp, r=["ex"], w=["ex"])
                P.op("vector", lambda e: e.tensor_reduce(out=se[:], in_=ex[:], axis=AX.X, op=ALU.add), reads=["ex"], writes=["se"])
                RECIP(se[:], se[:], r=["se"], w=["se"])
                TT("vector", wg[:], ex[:], se[:].unsqueeze(2).to_broadcast([128, 8, 16]), ALU.mult, r=["ex", "se"], w=["wg"])
                tsl = slice(tt * 128, (tt + 1) * 128)
                TR(ps[6][:, 0:128], e1f[:].rearrange("p a b -> p (a b)"), identf[:], r=[("ef", 0)], w=[("ps", 6)])
                TR(ps[6][:, 128:256], e2f[:].rearrange("p a b -> p (a b)"), identf[:], r=[("ef", 1)], w=[("ps", 6)])
                TR(ps[6][:, 256:384], wg[:].rearrange("p a b -> p (a b)"), identf[:], r=["wg"], w=[("ps", 6)])
                CP("scalar", E1T[:, tsl], ps[6][:, 0:128], r=[("ps", 6)], w=[("E1T", tt)])
                CP("scalar", E2T[:, tsl], ps[6][:, 128:256], r=[("ps", 6)], w=[("E2T", tt)])
                CP("scalar", WT[:, tsl], ps[6][:, 256:384], r=[("ps", 6)], w=[("WT", tt)])
        if stop_after == "P0":
            P.emit()
            return nc
        BARRIER()

        ar.off = RA + 16 * 1024
        E1Tm = ar.alloc([NOWN], BF16)
        iota3 = ar.alloc([8, 128], BF16)
        iotaH = ar.alloc([8, 64], BF16)
        xfin = ar.alloc([D], F32)
        x1r = ar.alloc([D], F32)
        assert ar.off <= RB
        ar.off = RC
        Wh = [ar.alloc([256, 64], BF16) for _ in range(2)]
        NSLOT = 3
        UTb = [ar.alloc([2, 1024], BF16) for _ in range(NSLOT)]
        Vb = [ar.alloc([2, 1024], BF16) for _ in range(NSLOT)]
        Gs = [ar.alloc([256], BF16) for _ in range(3)]
        GWs = [ar.alloc([256], BF16) for _ in range(3)]
        ssf = ar.alloc([1], F32)
        E1p = [ar.alloc([8, 64], BF16) for _ in range(3)]
        E2p = [ar.alloc([8, 128], BF16) for _ in range(3)]
        xsq = x1r.bitcast(BF16)[:, 0:D]
        P.op("gpsimd", lambda e: e.iota(iota3[:], pattern=[[0, 8], [1, 128]], base=0, channel_multiplier=0,
                                        allow_small_or_imprecise_dtypes=True), writes=["iota3"])
        P.op("gpsimd", lambda e: e.iota(iotaH[:], pattern=[[0, 8], [1, 64]], base=0, channel_multiplier=0,
                                        allow_small_or_imprecise_dtypes=True), writes=["iotaH"])
        TS("vector", E1Tm[:], E1T[:], -64.0, None, ALU.add, r=[], w=["E1Tm"])

        def b_eq(tb_, hf_, m):
            t0 = tb_ * 256 + m * 8
            bi = m % 3
            e1src = E1T if hf_ == 0 else E1Tm
            TT("vector", E1p[bi][:], iotaH[:], e1src[:, t0:t0 + 8].unsqueeze(2).to_broadcast([128, 8, 64]), ALU.is_equal,
               r=["iotaH", "E1Tm"], w=[("E1p", bi)])
            TT("vector", E2p[bi][:], iota3[:], E2T[:, t0:t0 + 8].unsqueeze(2).to_broadcast([128, 8, 128]), ALU.is_equal,
               r=["iota3"], w=[("E2p", bi)])

        def b_mul(tb_, hf_, m):
            t0 = tb_ * 256 + m * 8
            bi = m % 3
            TT("gpsimd", E2p[bi][:], E2p[bi][:], WT[:, t0:t0 + 8].unsqueeze(2).to_broadcast([128, 8, 128]), ALU.mult,
               r=[("E2p", bi)], w=[("E2p", bi)])

        def b_mm_pe(tb_, hf_, m):
            bi = m % 3
            for tq in range(8):
                MM(ps[6][:, tq * 64:(tq + 1) * 64], E2p[bi][:, tq, :], E1p[bi][:, tq, :], r=[("E1p", bi), ("E2p", bi)], w=[("ps", 6)])

        def b_mm_act(tb_, hf_, m):
            CP("scalar", Wh[hf_][:, m * 8:m * 8 + 8, :], ps[6][:].rearrange("p (a b) -> p a b", b=64), r=[("ps", 6)], w=[("Wh", hf_)])

        def b_mm(tb_, hf_, m):
            b_mm_pe(tb_, hf_, m)
            b_mm_act(tb_, hf_, m)

        def loadg(cg):
            s = cg % NSLOT
            DMA(UTb[s][:].rearrange("p a b -> p (a b)"), utb[cg], w=[("UTb", s)], q="sync")
            DMA(Vb[s][:].rearrange("p a b -> p (a b)"), vtb[cg], w=[("Vb", s)], q="sync")

        for m in range(32):
            b_eq(0, 0, m)
            b_mul(0, 0, m)
            b_mm(0, 0, m)
        for tb in range(8):
            T0 = tb * 256
            ABANK = (4, 5, 7)

            def Amm(i):
                s = (i // 2) % NSLOT
                ab = i % 3
                for k in range(8):
                    MM(ps[ABANK[ab]][:, 0:256], UTb[s][:, i % 2, k * 128:(k + 1) * 128], h2T[:, k, T0:T0 + 256], start=(k == 0), stop=(k == 7),
                       r=[("UTb", s), "h2T"], w=[("ps", ABANK[ab])])

            for c0 in range(NSLOT):
                loadg(c0)
            Amm(0)
            Amm(1)
            for i in range(128):
                hf2 = i // 64
                if i + 2 < 128:
                    Amm(i + 2)
                cg = i // 2
                s = cg % NSLOT
                ab = i % 3
                nxt = (tb, 1) if hf2 == 0 else ((tb + 1, 0) if tb + 1 < 8 else None)
                m = (i % 64) // 2
                do_b = nxt is not None and i % 2 == 0 and m >= 2
                if do_b:
                    b_mm_pe(nxt[0], nxt[1], m - 2)
                ACT(Gs[ab][:], ps[ABANK[ab]][:, 0:256], AF.Gelu, r=[("ps", ABANK[ab])], w=[("Gs", ab)])
                if do_b:
                    b_mm_act(nxt[0], nxt[1], m - 2)
                TT("vector", GWs[ab][:], Gs[ab][:], Wh[hf2][:, :, i - 64 * hf2], ALU.mult, r=[("Gs", ab), ("Wh", hf2)], w=[("GWs", ab)])
                for tl in range(2):
                    for half in range(2):
                        MM(ps[tl * 2 + half][:], GWs[ab][:, tl * 128:(tl + 1) * 128], Vb[s][:, i % 2, half * 512:(half + 1) * 512],
                           start=(i == 0), stop=(i == 127), r=[("GWs", ab), ("Vb", s)], w=[("ps", tl * 2 + half)])
                if i % 2 == 1 and cg + NSLOT < 64:
                    loadg(cg + NSLOT)
                if nxt is not None:
                    if i % 2 == 0:
                        b_eq(nxt[0], nxt[1], m)
                    else:
                        b_mul(nxt[0], nxt[1], m)
                        if m == 31:
                            b_mm(nxt[0], nxt[1], 30)
                            b_mm(nxt[0], nxt[1], 31)
            for tl in range(2):
                tt = tb * 2 + tl
                DMA(x1r[:], x1s[tt], w=["x1r"])
                for half in range(2):
                    hs = slice(half * 512, (half + 1) * 512)
                    TT("vector", xfin[:, hs], ps[tl * 2 + half][:], g2row[:, hs], ALU.mult, r=[("ps", tl * 2 + half)], w=["xfin"])
                TT("vector", xfin[:], xfin[:], x1r[:], ALU.add, r=["xfin", "x1r"], w=["xfin"])
                ACT(xsq, xfin[:], AF.Square, r=["xfin"], w=["x1r", "ssf"], accum=ssf[:])
                ACT(ssf[:], ssf[:], AF.Sqrt, r=["ssf"], w=["ssf"], scale=1.0 / D, bias=epsb[:])
                RECIP(ssf[:], ssf[:], r=["ssf"], w=["ssf"])
                STT(xfin[:], xfin[:], ssf[:, 0:1], fgrow[:], ALU.mult, ALU.mult, r=["xfin", "ssf"], w=["xfin"])
                DMA(out_d[tt], xfin[:], r=["xfin"], w=[("out", tt)], sem_key="out")

        P.emit()
    return nc


def _rope_tabs(pos):
    n_freq = 8
    freq = (np.float32(10000.0) ** (-np.arange(n_freq, dtype=np.float32) / np.float32(n_freq))).astype(np.float32)
    r = (pos // 64).astype(np.float32)
    c = (pos % 64).astype(np.float32)
    ang = np.stack([r[:, None] * freq[None, :], c[:, None] * freq[None, :]], axis=1).astype(np.float32)
    cos = np.cos(ang).astype(np.float32)
    sin = np.sin(ang).astype(np.float32)
    ct = np.zeros((32, pos.shape[0]), np.float32)
    stb = np.zeros((32, pos.shape[0]), np.float32)
    perm = np.zeros(32, np.int64)
    for a in range(2):
        for half in range(2):
            for f in range(8):
                d = a * 16 + half * 8 + f
                perm[d] = a * 16 + (1 - half) * 8 + f
                ct[d] = cos[:, a, f]
                stb[d] = -sin[:, a, f] if half == 0 else sin[:, a, f]
    return ct, stb, perm


def prep_inputs(inp):
    f = lambda a: np.ascontiguousarray(np.asarray(a, dtype=np.float32))
    x, c, ctx, c_ctx = f(inp["x"]), f(inp["c"]), f(inp["ctx"]), f(inp["c_ctx"])
    w_in = f(inp["w_in"])[0]
    _, _, perm = _rope_tabs(np.arange(4))
    shared = {}
    shared["wmod"] = f(inp["w_mod"])[0]
    b_mod = f(inp["b_mod"])[0]
    shared["bmodg"] = np.ascontiguousarray(np.broadcast_to(np.concatenate([b_mod[2048:3072], b_mod[5120:6144]])[None, :], (128, 2048)))
    shared["fgrow"] = np.ascontiguousarray(np.broadcast_to(f(inp["final_g"])[None, :], (128, D)))
    shared["win"] = w_in
    wkr = np.zeros((D, 192), np.float32)
    wkr[:, 64:96] = w_in[:, OFF_KR:OFF_KR + 32]
    wkr[:, 160:192] = w_in[:, OFF_KR + perm]
    shared["wkr"] = wkr
    w_uq = f(inp["w_uq"])[0]
    shared["wuq"] = w_uq
    colp = np.arange(1536).reshape(16, 96).copy()
    for h in range(16):
        colp[h, 64:96] = h * 96 + 64 + perm
    shared["wuqp"] = np.ascontiguousarray(w_uq[:, colp.reshape(-1)])
    shared["wukv"] = f(inp["w_ukv"])[0]
    shared["womla"] = f(inp["w_o_mla"])[0]
    shared["wpw"] = f(inp["w_pw"])[0]
    shared["wout"] = f(inp["w_out"])[0]
    shared["wpq"] = f(inp["w_pq"])[0]
    sk = f(inp["sub_keys"])[0].reshape(16, 128, 128)
    shared["subkT"] = np.ascontiguousarray(sk.transpose(2, 0, 1).reshape(128, 16 * 128))
    u = f(inp["u_experts"])[0]
    shared["ut"] = np.ascontiguousarray(u.reshape(128, 128, 8, 128).transpose(0, 3, 2, 1).reshape(128, 128, 1024))
    shared["vt"] = f(inp["v_experts"])[0].reshape(128, 128, 1024)
    ck, sk_, _ = _rope_tabs(np.arange(SEQ))
    ropek = np.zeros((2, 128, SEQ), np.float32)
    ropek[0, 64:96] = ck
    ropek[1, 64:96] = sk_
    shared["ropek"] = ropek

    def tomaj(v):
        return v.reshape(-1, 128).T

    maps = []
    for core in range(8):
        b, hf = core // 2, core % 2
        m = dict(shared)
        m["xk"] = np.ascontiguousarray(np.concatenate([x[b], ctx[b]], axis=0).reshape(34, 128, D))
        xo = np.zeros((17 * 128, D), np.float32)
        lo = hf * NOWN
        xo[:NOWN] = x[b, lo:lo + NOWN]
        if hf == 1:
            xo[NOWN:NOWN + 15] = x[b, lo - 15:lo]
        else:
            xo[NOWN + 15:NOWN + 30] = x[b, lo + NOWN:lo + NOWN + 15]
        m["xo"] = xo.reshape(17, 128, D)
        vecs = np.zeros((128, NV), np.float32)
        vecs[:, 0:8] = tomaj(f(inp["norm1_g"])[0])
        vecs[:, 8:16] = tomaj(f(inp["norm2_g"])[0])
        vecs[:, 16:18] = tomaj(f(inp["q_norm_g"])[0])
        vecs[:, 18:19] = tomaj(f(inp["kv_norm_g"])[0])
        vecs[:, 19:27] = tomaj(f(inp["conv_b"])[0])
        vecs[:, 27:35] = tomaj(f(inp["conv_ln_g"])[0])
        vecs[:, 35:43] = tomaj(f(inp["conv_ln_b"])[0])
        cw = f(inp["conv_w"])[0]
        vecs[:, 43:291] = cw.reshape(31, 8, 128).transpose(2, 1, 0).reshape(128, 248)
        vecs[:, 291:339] = tomaj(b_mod)
        ct2 = np.stack([tomaj(c[b]), tomaj(c_ctx)], axis=2)
        vecs[:, 339:355] = ct2.reshape(128, 16)
        vecs[:, 355] = 1.0 if hf == 1 else 0.0
        vecs[:, 356] = 1.0 if hf == 0 else 0.0
        m["vecs"] = vecs
        cq_, sq_, _ = _rope_tabs(np.arange(lo, lo + NOWN))
        ropeq = np.zeros((2, 128, NOWN), np.float32)
        ropeq[0, 0:64] = 1.0
        ropeq[0, 64:96] = cq_
        ropeq[1, 64:96] = sq_
        m["ropeq"] = ropeq
        maps.append(m)
    return maps


_NC_CACHE = {}


def kernel(**inputs):
    maps = prep_inputs(inputs)
    if "nc" not in _NC_CACHE:
        _NC_CACHE["nc"] = build_nc()
    nc = _NC_CACHE["nc"]
    res = run_bass_kernel_spmd(nc, maps, core_ids=list(range(8)))
    out = np.zeros((4, SEQ, D), np.float32)
    for core in range(8):
        b, hf = core // 2, core % 2
        out[b, hf * NOWN:(hf + 1) * NOWN] = np.asarray(res.results[core]["out"]).reshape(NOWN, D)
    return out
```

```python
import numpy as np
from contextlib import ExitStack
import concourse.bass as bass
import concourse.mybir as mybir
from concourse.bass_utils import run_bass_kernel_spmd

F32 = mybir.dt.float32
BF16 = mybir.dt.bfloat16
U32 = mybir.dt.uint32
ALU = mybir.AluOpType
AF = mybir.ActivationFunctionType
AX = mybir.AxisListType
ENGS = ("tensor", "vector", "scalar", "gpsimd", "sync")

D = 1024
SEQ = 4096
CTX = 256
NKEY = SEQ + CTX
NOWN = 2048
NH = 16
EPS = 1e-6
ATTN_SCALE = 96 ** -0.5
OFF_KV = 256
OFF_KR = 384
OFF_CONV = 416
OFF_GATE = 2464
NV = 360
CUT = 0


class _Op:
    __slots__ = ("eng", "fn", "waits", "signal", "sem", "val", "is_dma", "inc")

    def __init__(self, eng, fn):
        self.eng = eng
        self.fn = fn
        self.waits = []
        self.signal = False
        self.sem = None
        self.val = 0
        self.is_dma = False
        self.inc = 1


class Prog:
    def __init__(self, nc):
        self.nc = nc
        self.ops = {e: [] for e in ENGS}
        self.last_w = {}
        self.readers = {}
        self.all_ops = []
        self.epoch = None
        self.last_by_sem = {}

    def op(self, eng, fn, reads=(), writes=(), dma=False, sem_key=None):
        o = _Op(eng, fn)
        o.is_dma = dma
        deps = []
        for r in reads:
            w = self.last_w.get(r, self.epoch)
            if w is not None:
                deps.append((w, True))
        for r in writes:
            w = self.last_w.get(r, self.epoch)
            if w is not None:
                deps.append((w, False))
            for rd in self.readers.get(r, {}).values():
                deps.append((rd, False))
        seen = set()
        for d, raw in deps:
            if id(d) in seen:
                continue
            same = (not d.is_dma) and (not dma) and d.eng == eng
            if same and eng == "tensor":
                continue
            seen.add(id(d))
            o.waits.append(d)
            d.signal = True
        if dma:
            o.sem = ("dma", sem_key if sem_key is not None else (writes[0] if writes else reads[0]))
            o.inc = 16
            o.signal = True
        else:
            o.sem = ("eng", eng)
        for r in reads:
            self.readers.setdefault(r, {})[o.sem] = o
        for r in writes:
            self.last_w[r] = o
            self.readers[r] = {}
        self.ops[eng].append(o)
        self.all_ops.append(o)
        self.last_by_sem[o.sem] = o
        return o

    def barrier(self, fn):
        o = _Op("gpsimd", fn)
        o.sem = ("eng", "gpsimd")
        for d in self.last_by_sem.values():
            if d.eng == "gpsimd" and not d.is_dma:
                continue
            o.waits.append(d)
            d.signal = True
        o.signal = True
        self.ops["gpsimd"].append(o)
        self.all_ops.append(o)
        self.last_by_sem[o.sem] = o
        self.last_w = {}
        self.readers = {}
        self.epoch = o

    def emit(self, final_wait_eng="sync"):
        nc = self.nc
        last_dma = {}
        for o in self.all_ops:
            if o.is_dma:
                last_dma[o.sem] = o
        counters = {}
        for o in self.all_ops:
            if o.signal:
                counters[o.sem] = counters.get(o.sem, 0) + o.inc
                o.val = counters[o.sem]
        sem_keys = list(counters.keys())
        self.n_sems = len(sem_keys)
        with ExitStack() as st:
            sems = {}
            for i, k in enumerate(sem_keys):
                sems[k] = st.enter_context(nc.semaphore("s%d" % i))
            block = st.enter_context(nc.Block())

            def make(engname):
                ops = self.ops[engname]

                def body(e):
                    waited = {}
                    for o in ops:
                        for d in o.waits:
                            if waited.get(d.sem, 0) >= d.val:
                                continue
                            e.wait_ge(sems[d.sem], d.val)
                            waited[d.sem] = d.val
                        inst = o.fn(e)
                        if o.signal:
                            inst.then_inc(sems[o.sem], o.inc)
                    if engname == final_wait_eng:
                        for k, o in last_dma.items():
                            if waited.get(k, 0) < o.val:
                                e.wait_ge(sems[k], o.val)
                return body

            for engname in ENGS:
                if not self.ops[engname] and engname != final_wait_eng:
                    continue
                getattr(block, engname)(make(engname))


class Arena:
    def __init__(self, tensor, nbytes):
        self.t = tensor
        self.n = nbytes
        self.off = 0

    def alloc(self, shape_free, dt):
        es = 4 if dt in (F32, U32) else 2
        n = int(np.prod(shape_free)) * es
        n_al = (n + 63) // 64 * 64
        assert self.off + n_al <= self.n, ("arena overflow", self.off, n_al, self.n)
        v = self.t[:, self.off // 2:(self.off + n) // 2]
        self.off += n_al
        if es == 4:
            v = v.bitcast(dt)
        if len(shape_free) == 2:
            v = v.rearrange("p (a b) -> p a b", b=shape_free[1])
        elif len(shape_free) == 3:
            v = v.rearrange("p (a b c) -> p a b c", b=shape_free[1], c=shape_free[2])
        return v

    def mark(self):
        return self.off

    def release(self, m):
        self.off = m


def build_nc(stop_after=None, debug=False):
    nc = bass.Bass("TRN2", target_bir_lowering=False)

    def din(name, shape, dt=F32):
        return nc.dram_tensor(name, list(shape), dt, kind="ExternalInput").ap()

    xk = din("xk", [34, 128, D])
    xo = din("xo", [17, 128, D])
    vecs_d = din("vecs", [128, NV])
    wmod = din("wmod", [D, 6 * D])
    bmodg = din("bmodg", [128, 2048])
    fgrow_d = din("fgrow", [128, D])
    win = din("win", [D, 4512])
    wkr = din("wkr", [D, 192])
    wuq = din("wuq", [256, 1536])
    wuqp = din("wuqp", [256, 1536])
    wukv = din("wukv", [128, 2048])
    womla = din("womla", [D, D])
    wpw = din("wpw", [D, D])
    wout = din("wout", [D, D])
    wpq = din("wpq", [D, 2048])
    subkT_d = din("subkT", [128, 16 * 128])
    ut = din("ut", [128, 128, 1024])
    vt = din("vt", [128, 128, 1024])
    ropek = din("ropek", [2, 128, SEQ])
    ropeq = din("ropeq", [2, 128, NOWN])
    out_d = nc.dram_tensor("out", [16, 128, D], F32, kind="ExternalOutput").ap()
    utb = nc.dram_tensor("utb", [64, 128, 2048], BF16, kind="Internal").ap()
    vtb = nc.dram_tensor("vtb", [64, 128, 2048], BF16, kind="Internal").ap()
    x1s = nc.dram_tensor("x1s", [16, 128, D], F32, kind="Internal").ap()
    hts = nc.dram_tensor("hts", [128, 8 * NOWN], BF16, kind="Internal").ap()
    zts = nc.dram_tensor("zts", [128, 8 * NOWN], BF16, kind="Internal").ap()
    dbg = {}
    if debug:
        for nm, shp in debug.items():
            dt_ = F32
            if isinstance(shp, tuple) and len(shp) == 2 and shp[1] == "bf16":
                shp, dt_ = shp[0], BF16
            dbg[nm] = nc.dram_tensor("dbg_" + nm, list(shp), dt_, kind="ExternalOutput").ap()

    st = ExitStack()
    with st:
        def sbuf(name, shape, dt):
            return st.enter_context(nc.sbuf_tensor("sb_" + name, list(shape), dt))

        ARENA_BYTES = 172 * 1024
        arena_t = sbuf("arena", [128, ARENA_BYTES // 2], BF16)
        ar = Arena(arena_t, ARENA_BYTES)
        identb = sbuf("identb", [128, 128], BF16)
        identf = sbuf("identf", [128, 128], F32)
        onesb = sbuf("onesb", [128, 128], BF16)
        onesf = sbuf("onesf", [128, 128], F32)
        vecs = sbuf("vecs", [128, NV], F32)
        scT = sbuf("scT", [128, 8, 2], F32)
        screp = sbuf("screp", [128, 8, 128], F32)
        modT = sbuf("modT", [128, 48, 2], F32)
        A1 = sbuf("A1", [128, 8, 2], F32)
        A2 = sbuf("A2", [128, 8], F32)
        g1row = sbuf("g1row", [128, D], F32)
        g2row = sbuf("g2row", [128, D], F32)
        fgrow = sbuf("fgrow", [128, D], F32)
        stage = sbuf("stage", [128, 2, 1024], F32)
        junk = sbuf("junk", [128, 8], F32)
        ps = [st.enter_context(nc.psum_tensor("ps%d" % i, [128, 512], F32)) for i in range(8)]

        P = Prog(nc)

        def MM(out, lhsT, rhs, start=True, stop=True, r=(), w=()):
            P.op("tensor", lambda e: e.matmul(out, lhsT=lhsT, rhs=rhs, start=start, stop=stop), reads=r, writes=w)

        def TR(out, in_, ident, r=(), w=()):
            P.op("tensor", lambda e: e.transpose(out=out, in_=in_, identity=ident), reads=r, writes=w)

        def ACT(out, in_, func, r=(), w=(), scale=None, bias=None, accum=None):
            kw = {}
            if scale is not None:
                kw["scale"] = scale
            if bias is not None:
                kw["bias"] = bias
            if accum is not None:
                kw["accum_out"] = accum
            P.op("scalar", lambda e: e.activation(out=out, in_=in_, func=func, **kw), reads=r, writes=w)

        def TT(eng, out, in0, in1, op, r=(), w=()):
            P.op(eng, lambda e: e.tensor_tensor(out=out, in0=in0, in1=in1, op=op), reads=r, writes=w)

        def TS(eng, out, in0, s1, s2, op0, op1=None, r=(), w=()):
            if op1 is None:
                P.op(eng, lambda e: e.tensor_scalar(out=out, in0=in0, scalar1=s1, scalar2=None, op0=op0), reads=r, writes=w)
            else:
                P.op(eng, lambda e: e.tensor_scalar(out=out, in0=in0, scalar1=s1, scalar2=s2, op0=op0, op1=op1), reads=r, writes=w)

        def STT(out, in0, scalar, in1, op0, op1, r=(), w=()):
            P.op("vector", lambda e: e.scalar_tensor_tensor(out=out, in0=in0, scalar=scalar, in1=in1, op0=op0, op1=op1), reads=r, writes=w)

        def CP(eng, out, in_, r=(), w=()):
            if eng == "scalar":
                P.op(eng, lambda e: e.copy(out=out, in_=in_), reads=r, writes=w)
            else:
                P.op(eng, lambda e: e.tensor_copy(out=out, in_=in_), reads=r, writes=w)

        def RECIP(out, in_, r=(), w=()):
            P.op("vector", lambda e: e.reciprocal(out=out, in_=in_), reads=r, writes=w)

        def MEMSET(eng, ap, val, w=()):
            P.op(eng, lambda e: e.memset(ap, val), writes=w)

        dmaq = ["sync", "scalar"]
        dma_i = [0]

        def DMA(out, in_, r=(), w=(), q=None, sem_key=None):
            if q is None:
                q = "sync"
            P.op(q, lambda e: e.dma_start(out=out, in_=in_), reads=r, writes=w, dma=True, sem_key=sem_key)

        def BARRIER():
            P.barrier(lambda e: e.memset(junk[:, 0:1], 0.0))
            ar.off = RC

        RA, RB, RC = 0, 34 * 1024, 67 * 1024
        stage_i = [0]
        cv_i = [0]

        def load_w(dst, src, ncols, wkey, conv_eng=None):
            assert ncols <= 1024
            s = stage_i[0] % 2
            stage_i[0] += 1
            sv = stage[:, s, 0:ncols]
            if len(src.shape) == 3:
                sv = sv.rearrange("p (a b) -> p a b", b=src.shape[2])
            DMA(sv, src, w=[("stage", s)])
            if conv_eng is None:
                conv_eng = ("gpsimd", "vector")[cv_i[0] % 2]
                cv_i[0] += 1
            CP(conv_eng, dst, sv, r=[("stage", s)], w=[wkey])

        def dump(name, src, r):
            if debug and name in dbg:
                DMA(dbg[name], src, r=r, w=[("dbg", name)])

        MEMSET("gpsimd", identf[:], 0.0, w=["identf"])
        P.op("gpsimd", lambda e: e.affine_select(out=identf[:], in_=identf[:], pattern=[[-1, 128]], base=0,
                                                 channel_multiplier=1, compare_op=ALU.not_equal, fill=1.0),
             reads=["identf"], writes=["identf"])
        CP("vector", identb[:], identf[:], r=["identf"], w=["identb"])
        MEMSET("vector", onesb[:], 1.0, w=["onesb"])
        MEMSET("vector", onesf[:], 1.0, w=["onesf"])
        DMA(vecs[:], vecs_d, w=["vecs"])
        DMA(fgrow[:], fgrow_d, w=["fgrow"])
        epsb = sbuf("epsb", [128, 1], F32)
        MEMSET("vector", epsb[:], EPS, w=["epsb"])
        MEMSET("vector", modT[:], 0.0, w=["modT"])
        V_N1, V_N2, V_QG, V_KVG, V_CB, V_LNG, V_LNB, V_CW, V_BM, V_CT, V_HM = 0, 8, 16, 18, 19, 27, 35, 43, 291, 339, 355

        cT = vecs[:, V_CT:V_CT + 16].rearrange("p (k j) -> p k j", j=2)
        ACT(scT[:], cT, AF.Silu, r=["vecs"], w=["scT"])
        CP("vector", screp[:], scT[:, :, 0:1].to_broadcast([128, 8, 128]), r=["scT"], w=["screp"])
        ar.off = RC
        wmb = [ar.alloc([8, 512], F32) for _ in range(2)]
        bg = ar.alloc([2048], F32)
        DMA(bg, bmodg, w=["bg"])
        wmod_v = wmod.rearrange("(k p) f -> p k f", p=128)
        for blk in range(12):
            s = blk % 2
            DMA(wmb[s], wmod_v[:, :, blk * 512:(blk + 1) * 512], w=[("wmb", s)], q=dmaq[blk % 2])
            if blk in (4, 5, 10, 11):
                row = g1row if blk < 6 else g2row
                half = blk % 2
                for k in range(8):
                    MM(ps[0][:], screp[:, k, :], wmb[s][:, k, :], start=(k == 0), stop=(k == 7),
                       r=["screp", ("wmb", s)], w=[("ps", 0)])
                goff = (0 if blk < 6 else 1024) + half * 512
                TT("vector", row[:, half * 512:(half + 1) * 512], ps[0][:], bg[:, goff:goff + 512], ALU.add,
                   r=[("ps", 0), "bg"], w=[("row", blk)])
            else:
                for fc in range(4):
                    for k in range(8):
                        MM(ps[1][:, fc * 2:fc * 2 + 2], wmb[s][:, k, fc * 128:(fc + 1) * 128], scT[:, k, :],
                           start=(k == 0), stop=(k == 7), r=["scT", ("wmb", s)], w=[("ps", 1)])
                for fc in range(4):
                    f = blk * 4 + fc
                    TS("vector", modT[:, f, :], ps[1][:, fc * 2:fc * 2 + 2], vecs[:, V_BM + f:V_BM + f + 1], None, ALU.add,
                       r=[("ps", 1), "vecs"], w=["modT"])
        for j in range(2):
            STT(A1[:, :, j], modT[:, 8:16, j], 1.0, vecs[:, V_N1:V_N1 + 8], ALU.add, ALU.mult, r=["modT", "vecs"], w=["A1"])
        STT(A2[:], modT[:, 32:40, 0], 1.0, vecs[:, V_N2:V_N2 + 8], ALU.add, ALU.mult, r=["modT", "vecs"], w=["A2"])
        dump("modT", modT[:].rearrange("p a b -> p (a b)"), ["modT"])
        dump("g1row", g1row[:], [("row", 4), ("row", 5)])
        if stop_after == "mod":
            P.emit()
            return nc
        BARRIER()

        ar.off = RA
        ckvnT = ar.alloc([NKEY], BF16)
        KT = [ar.alloc([NKEY], BF16) for _ in range(2)]
        cqnT = ar.alloc([2, NOWN], BF16)
        assert ar.off <= RB
        ar.off = RC

        def norm_tile(src_tile, dst, j, Asc, Bsh, xt, sq, xn, ss, rs, psb, tag, dkey):
            if src_tile is not None:
                DMA(xt, src_tile, w=[(tag, "xt")], q=dmaq[j % 2])
            if CUT == 5:
                return
            ACT(sq, xt, AF.Square, r=[(tag, "xt")], w=[(tag, "sq"), (tag, "ss")], accum=ss)
            if CUT == 6:
                return
            ACT(rs, ss, AF.Sqrt, r=[(tag, "ss")], w=[(tag, "rs")], scale=1.0 / D, bias=epsb[:])
            if CUT == 7:
                return
            RECIP(rs, rs, r=[(tag, "rs")], w=[(tag, "rs")])
            TS("vector", xn, xt, rs, None, ALU.mult, r=[(tag, "xt"), (tag, "rs")], w=[(tag, "xn")])
            if CUT == 8:
                return
            pb = ps[psb][:].bitcast(BF16)
            for k in range(8):
                TR(pb[:, k * 128:(k + 1) * 128], xn[:, k * 128:(k + 1) * 128], identb[:], r=[(tag, "xn"), "identb"], w=[("ps", psb)])
            if CUT == 9:
                return
            pb3 = pb.rearrange("p (k t) -> p k t", k=8)
            tmpn = tmpns[j % 2]
            TT("vector", tmpn[:], pb3, Asc.unsqueeze(2).to_broadcast([128, 8, 128]), ALU.mult,
               r=[("ps", psb), "A1", "A2", "modT"], w=[("tmpn", j % 2)])
            TT("gpsimd", dst, tmpn[:], Bsh.unsqueeze(2).to_broadcast([128, 8, 128]), ALU.add,
               r=[("tmpn", j % 2), "A1", "A2", "modT"], w=[(dkey, 0)])

        tmpns = [sbuf("tmpn%d" % i, [128, 8, 128], F32) for i in range(2)]

        xts = [ar.alloc([D], F32) for _ in range(2)]
        sqj = ar.alloc([D], F32)
        xns = [ar.alloc([D], BF16) for _ in range(2)]
        sss = [ar.alloc([1], F32) for _ in range(2)]
        rss = [ar.alloc([1], F32) for _ in range(2)]
        hTb = [ar.alloc([8, 512], BF16) for _ in range(2)]
        wkvb = ar.alloc([8, 320], BF16)
        sqb = ar.alloc([512], BF16)
        rstd = ar.alloc([512], F32)
        rk = [ar.alloc([2, 512], F32) for _ in range(2)]
        t1 = ar.alloc([512], F32)
        t2 = ar.alloc([512], F32)
        win_v = win.rearrange("(k p) f -> p k f", p=128)
        wkr_v = wkr.rearrange("(k p) f -> p k f", p=128)
        load_w(wkvb[:, :, 0:128], win_v[:, :, OFF_KV:OFF_KV + 128], 8 * 128, ("wkvb", 0))
        load_w(wkvb[:, 0:4, 128:320], wkr_v[:, 0:4, :], 4 * 192, ("wkvb", 1))
        load_w(wkvb[:, 4:8, 128:320], wkr_v[:, 4:8, :], 4 * 192, ("wkvb", 2))
        ti = 0
        if CUT == 1:
            P.emit()
            return nc
        for blk in range(9):
            ntile = 4 if blk < 8 else 2
            n = ntile * 128
            hb = hTb[blk % 2]
            jm = 0 if blk < 8 else 1
            for tl in range(ntile):
                t = blk * 4 + tl
                s = ti % 2
                norm_tile(xk[t], hb[:, :, tl * 128:(tl + 1) * 128], ti, A1[:, :, jm], modT[:, 0:8, jm],
                          xts[s], sqj, xns[s], sss[s], rss[s], 2 + s, ("nk", s), ("hTb", blk % 2, s))
                ti += 1
            hr = [(("hTb", blk % 2, s_), 0) for s_ in range(2)]
            if CUT == 2 or CUT >= 5:
                P.emit()
                return nc
            for k in range(8):
                MM(ps[4][:, 0:n], wkvb[:, k, 0:128], hb[:, k, 0:n], start=(k == 0), stop=(k == 7),
                   r=hr + [("wkvb", 0)], w=[("ps", 4)])
            for k in range(8):
                MM(ps[5][0:96, 0:n], wkvb[:, k, 128:224], hb[:, k, 0:n], start=(k == 0), stop=(k == 7),
                   r=hr + [("wkvb", 1), ("wkvb", 2)], w=[("ps", 5)])
            if blk < 8:
                for k in range(8):
                    MM(ps[6][0:96, 0:n], wkvb[:, k, 224:320], hb[:, k, 0:n], start=(k == 0), stop=(k == 7),
                       r=hr + [("wkvb", 1), ("wkvb", 2)], w=[("ps", 6)])
            if CUT == 3:
                P.emit()
                return nc
            ACT(sqb[:, 0:n], ps[4][:, 0:n], AF.Square, r=[("ps", 4)], w=["sqb"])
            MM(ps[7][:, 0:n], onesb[:], sqb[:, 0:n], r=["onesb", "sqb"], w=[("ps", 7)])
            ACT(rstd[:, 0:n], ps[7][:, 0:n], AF.Sqrt, r=[("ps", 7)], w=["rstd"], scale=1.0 / 128, bias=epsb[:])
            RECIP(rstd[:, 0:n], rstd[:, 0:n], r=["rstd"], w=["rstd"])
            STT(ckvnT[:, blk * 512:blk * 512 + n], ps[4][:, 0:n], vecs[:, V_KVG:V_KVG + 1], rstd[:, 0:n], ALU.mult, ALU.mult,
                r=[("ps", 4), "rstd", "vecs"], w=[("ckvnT", blk)])
            if CUT == 4:
                P.emit()
                return nc
            if blk < 8:
                rkb = rk[blk % 2]
                DMA(rkb[64:96, 0, :], ropek[0, 64:96, blk * 512:(blk + 1) * 512], w=[("rk", blk % 2, 0)], q="scalar")
                DMA(rkb[64:96, 1, :], ropek[1, 64:96, blk * 512:(blk + 1) * 512], w=[("rk", blk % 2, 1)], q="scalar")
                TT("vector", t1[64:96, :], ps[5][64:96, :], rkb[64:96, 0, :], ALU.mult, r=[("ps", 5), ("rk", blk % 2, 0)], w=["t1"])
                TT("vector", t2[64:96, :], ps[6][64:96, :], rkb[64:96, 1, :], ALU.mult, r=[("ps", 6), ("rk", blk % 2, 1)], w=["t2"])
                TT("vector", KT[0][64:96, blk * 512:(blk + 1) * 512], t1[64:96, :], t2[64:96, :], ALU.add, r=["t1", "t2"], w=[("KTr", 0, blk)])
                CP("gpsimd", KT[1][64:96, blk * 512:(blk + 1) * 512], KT[0][64:96, blk * 512:(blk + 1) * 512], r=[("KTr", 0, blk)], w=[("KTr", 1, blk)])
            else:
                CP("vector", KT[0][64:96, blk * 512:blk * 512 + n], ps[5][64:96, 0:n], r=[("ps", 5)], w=[("KTr", 0, blk)])
                CP("gpsimd", KT[1][64:96, blk * 512:blk * 512 + n], KT[0][64:96, blk * 512:blk * 512 + n], r=[("KTr", 0, blk)], w=[("KTr", 1, blk)])
        if debug:
            dk = ar.alloc([NKEY], F32)
            CP("vector", dk[:], ckvnT[:], r=[("ckvnT", b) for b in range(9)], w=["dk"])
            dump("ckvnT", dk[:], ["dk"])
            dk2 = ar.alloc([NKEY], F32)
            CP("vector", dk2[64:96, :], KT[1][64:96, :], r=[("KTr", 1, b) for b in range(9)], w=["dk2"])
            dump("krot", dk2[64:96, :], ["dk2"])
        if stop_after == "K":
            P.emit()
            return nc
        BARRIER()

        ar.off = RB
        ypad = ar.alloc([8, NOWN + 30], BF16)
        assert ar.off <= RC
        ar.off = RC
        xts = [ar.alloc([D], F32) for _ in range(2)]
        sqj = ar.alloc([D], F32)
        xns = [ar.alloc([D], BF16) for _ in range(2)]
        sss = [ar.alloc([1], F32) for _ in range(2)]
        rss = [ar.alloc([1], F32) for _ in range(2)]
        hTo = ar.alloc([8, 17 * 128], BF16)
        wch = [ar.alloc([8, 128], BF16) for _ in range(4)]
        sg = [ar.alloc([512], F32) for _ in range(2)]
        yh = ar.alloc([8, 128], BF16)
        sqb2 = ar.alloc([2, 512], BF16)
        rstd = ar.alloc([512], F32)
        for t in range(17):
            s = t % 2
            norm_tile(xo[t], hTo[:, :, t * 128:(t + 1) * 128], t, A1[:, :, 0], modT[:, 0:8, 0],
                      xts[s], sqj, xns[s], sss[s], rss[s], 2 + s, ("no", s), ("hTo", t))
        DMA(hts.rearrange("p (k t) -> p k t", k=8), hTo[:, :, 0:NOWN],
            r=[(("hTo", t), 0) for t in range(16)], w=["hts"])
        blocks = [(tb * 512, 512, [4 * tb + i for i in range(4)]) for tb in range(4)] + [(2048, 128, [16])]
        load_w(wch[0], win_v[:, :, 0:128], 1024, ("wch", 0))
        load_w(wch[1], win_v[:, :, 128:256], 1024, ("wch", 1))
        for bi in range(4):
            t0, n, tiles = blocks[bi]
            hr = [(("hTo", t), 0) for t in tiles]
            for cc in range(2):
                for k in range(8):
                    MM(ps[4 + cc][:], wch[cc][:, k, :], hTo[:, k, t0:t0 + n], start=(k == 0), stop=(k == 7),
                       r=hr + [("wch", cc)], w=[("ps", 4 + cc)])
                ACT(sqb2[:, cc, :], ps[4 + cc][:], AF.Square, r=[("ps", 4 + cc)], w=[("sqb2", cc)])
            MM(ps[6][:], onesb[:], sqb2[:, 0, :], start=True, stop=False, r=[("sqb2", 0)], w=[("ps", 6)])
            MM(ps[6][:], onesb[:], sqb2[:, 1, :], start=False, stop=True, r=[("sqb2", 1)], w=[("ps", 6)])
            ACT(rstd[:], ps[6][:], AF.Sqrt, r=[("ps", 6)], w=["rstd"], scale=1.0 / 256, bias=epsb[:])
            RECIP(rstd[:], rstd[:], r=["rstd"], w=["rstd"])
            for cc in range(2):
                STT(cqnT[:, cc, t0:t0 + n], ps[4 + cc][:], vecs[:, V_QG + cc:V_QG + cc + 1], rstd[:], ALU.mult, ALU.mult,
                    r=[("ps", 4 + cc), "rstd"], w=[("cqnT", cc, bi)])
        for c in range(8):
            wa, wg = (2 * c) % 4, (2 * c + 1) % 4
            load_w(wch[wa], win_v[:, :, OFF_CONV + c * 128:OFF_CONV + (c + 1) * 128], 1024, ("wch", wa))
            load_w(wch[wg], win_v[:, :, OFF_CONV + 1024 + c * 128:OFF_CONV + 1024 + (c + 1) * 128], 1024, ("wch", wg))
            for bi, (t0, n, tiles) in enumerate(blocks):
                hr = [(("hTo", t), 0) for t in tiles]
                ba, bg_ = (4, 5) if bi % 2 == 0 else (6, 7)
                for k in range(8):
                    MM(ps[ba][:, 0:n], wch[wa][:, k, :], hTo[:, k, t0:t0 + n], start=(k == 0), stop=(k == 7),
                       r=hr + [("wch", wa)], w=[("ps", ba)])
                for k in range(8):
                    MM(ps[bg_][:, 0:n], wch[wg][:, k, :], hTo[:, k, t0:t0 + n], start=(k == 0), stop=(k == 7),
                       r=hr + [("wch", wg)], w=[("ps", bg_)])
                sgb = sg[bi % 2]
                ACT(sgb[:, 0:n], ps[bg_][:, 0:n], AF.Sigmoid, r=[("ps", bg_)], w=[("sg", bi % 2)])
                dst = ypad[:, c, 15 + t0:15 + t0 + n] if bi < 4 else yh[:, c, :]
                TT("vector", dst, ps[ba][:, 0:n], sgb[:, 0:n], ALU.mult, r=[("ps", ba), ("sg", bi % 2)],
                   w=[("ypad", c, bi)])
            TS("vector", ypad[:, c, 0:15], yh[:, c, 0:15], vecs[:, V_HM:V_HM + 1], None, ALU.mult,
               r=[("ypad", c, 4), "vecs"], w=[("ypad", c, 5)])
            TS("vector", ypad[:, c, NOWN + 15:NOWN + 30], yh[:, c, 15:30], vecs[:, V_HM + 1:V_HM + 2], None, ALU.mult,
               r=[("ypad", c, 4), "vecs"], w=[("ypad", c, 6)])
        if debug:
            dump("cqnT", cqnT[:].rearrange("p a b -> p (a b)"), [("cqnT", cc, bi) for cc in range(2) for bi in range(4)])
            dump("ypad", ypad[:].rearrange("p a b -> p (a b)"), [("ypad", c, i) for c in range(8) for i in range(7)])
        if stop_after == "O":
            P.emit()
            return nc
        BARRIER()

        dg = [ar.alloc([31, 128], BF16) for _ in range(2)]
        vT = ar.alloc([8, NOWN], BF16)
        sqv = [ar.alloc([512], BF16) for _ in range(2)]
        mean = ar.alloc([512], F32)
        msq = ar.alloc([512], F32)
        rstdc = ar.alloc([512], F32)
        tmpc = [ar.alloc([512], F32) for _ in range(2)]
        for c in range(8):
            d_ = dg[c % 2]
            TT("vector", d_[:], identb[:].unsqueeze(1).to_broadcast([128, 31, 128]),
               vecs[:, V_CW + c * 31:V_CW + (c + 1) * 31].unsqueeze(2).to_broadcast([128, 31, 128]), ALU.mult,
               r=[], w=[("dg", c % 2)])
            for tb in range(4):
                bank = 4 + (tb % 2)
                for k in range(31):
                    MM(ps[bank][:], d_[:, k, :], ypad[:, c, tb * 512 + k:tb * 512 + k + 512], start=(k == 0), stop=(k == 30),
                       r=[("dg", c % 2)], w=[("ps", bank)])
                ACT(vT[:, c, tb * 512:(tb + 1) * 512], ps[bank][:], AF.Identity, r=[("ps", bank)], w=[("vT", c, tb)],
                    bias=vecs[:, V_CB + c:V_CB + c + 1])
        for tb in range(4):
            tsl = slice(tb * 512, (tb + 1) * 512)
            for c in range(8):
                sv = sqv[c % 2]
                ACT(sv[:], vT[:, c, tsl], AF.Square, r=[("vT", c, tb)], w=[("sqv", c % 2)])
                MM(ps[6][:], onesb[:], vT[:, c, tsl], start=(c == 0), stop=(c == 7), r=[("vT", c, tb)], w=[("ps", 6)])
                MM(ps[7][:], onesb[:], sv[:], start=(c == 0), stop=(c == 7), r=[("sqv", c % 2)], w=[("ps", 7)])
            ACT(mean[:], ps[6][:], AF.Identity, r=[("ps", 6)], w=["mean"], scale=1.0 / D)
            ACT(msq[:], ps[7][:], AF.Identity, r=[("ps", 7)], w=["msq"], scale=1.0 / D)
            TT("vector", rstdc[:], mean[:], mean[:], ALU.mult, r=["mean"], w=["rstdc"])
            TT("vector", rstdc[:], msq[:], rstdc[:], ALU.subtract, r=["msq", "rstdc"], w=["rstdc"])
            ACT(rstdc[:], rstdc[:], AF.Sqrt, r=["rstdc"], w=["rstdc"], bias=epsb[:])
            RECIP(rstdc[:], rstdc[:], r=["rstdc"], w=["rstdc"])
            for c in range(8):
                tm = tmpc[c % 2]
                TT("vector", tm[:], vT[:, c, tsl], mean[:], ALU.subtract, r=[("vT", c, tb), "mean"], w=[("tmpc", c % 2)])
                TT("vector", tm[:], tm[:], rstdc[:], ALU.mult, r=[("tmpc", c % 2), "rstdc"], w=[("tmpc", c % 2)])
                ACT(vT[:, c, tsl], tm[:], AF.Silu, r=[("tmpc", c % 2)], w=[("vT", c, tb)],
                    scale=vecs[:, V_LNG + c:V_LNG + c + 1], bias=vecs[:, V_LNB + c:V_LNB + c + 1])
        zkeys = [("vT", c, tb) for c in range(8) for tb in range(4)]
        DMA(zts.rearrange("p (k t) -> p k t", k=8), vT[:], r=zkeys, w=["zts"])
        if debug:
            dump("zT", vT[:].rearrange("p a b -> p (a b)"), zkeys)
        if stop_after == "C":
            P.emit()
            return nc
        BARRIER()

        ar.off = RB
        OT = ar.alloc([8, NOWN], BF16)
        assert ar.off <= RC
        ar.off = RC
        VH = [ar.alloc([34, 128], BF16) for _ in range(2)]
        QT = [ar.alloc([NOWN], BF16) for _ in range(2)]
        PT = [ar.alloc([512], BF16) for _ in range(4)]
        wuqb = ar.alloc([2, 1536], BF16)
        wuqpb = ar.alloc([2, 1536], BF16)
        wukvb = ar.alloc([2048], BF16)
        rq = ar.alloc([2, NOWN], F32)
        qt1 = ar.alloc([512], F32)
        qt2 = ar.alloc([512], F32)
        rden = ar.alloc([512], F32)
        rdenB = ar.alloc([512], F32)
        wuq_v = wuq.rearrange("(k p) f -> p k f", p=128)
        wuqp_v = wuqp.rearrange("(k p) f -> p k f", p=128)
        for k in range(2):
            for hh in range(2):
                load_w(wuqb[:, k, hh * 768:(hh + 1) * 768], wuq_v[:, k, hh * 768:(hh + 1) * 768], 768, ("wuqb", k, hh))
                load_w(wuqpb[:, k, hh * 768:(hh + 1) * 768], wuqp_v[:, k, hh * 768:(hh + 1) * 768], 768, ("wuqpb", k, hh))
        for hh in range(2):
            load_w(wukvb[:, hh * 1024:(hh + 1) * 1024], wukv[:, hh * 1024:(hh + 1) * 1024], 1024, ("wukvb", hh))
        wq_keys = [("wuqb", k, hh) for k in range(2) for hh in range(2)]
        wqp_keys = [("wuqpb", k, hh) for k in range(2) for hh in range(2)]
        wkv_keys = [("wukvb", 0), ("wukvb", 1)]
        DMA(rq[0:96, 0, :], ropeq[0, 0:96, :], w=[("rq", 0)])
        DMA(rq[0:96, 1, :], ropeq[1, 0:96, :], w=[("rq", 1)], q="scalar")
        for hb in range(2):
            MEMSET("gpsimd", VH[hb][:], 0.0, w=[("VH", hb)])
        MEMSET("gpsimd", VH[0][:, :, 64:65], 1.0, w=[("VH", 0)])
        MEMSET("gpsimd", VH[1][:, :, 0:1], 1.0, w=[("VH", 1)])
        ckeys = [("ckvnT", b_) for b_ in range(9)]
        cvf = [ar.alloc([2, 1024], F32) for _ in range(2)]
        cvb = [ar.alloc([2, 1024], BF16) for _ in range(2)]
        cv_steps = [(tab, tabb, nm, g) for (tab, tabb, nm) in ((ut, utb, "u"), (vt, vtb, "v")) for g in range(64)]
        cv_pos = [0]

        def conv_step():
            if cv_pos[0] >= len(cv_steps):
                return
            tab, tabb, nm, g = cv_steps[cv_pos[0]]
            s = cv_pos[0] % 2
            DMA(cvf[s][:], tab[g * 2:(g + 1) * 2].rearrange("i p f -> p i f"), w=[("cvf", s)], q="sync")
            CP(("gpsimd", "vector")[cv_pos[0] % 2], cvb[s][:], cvf[s][:], r=[("cvf", s)], w=[("cvb", s)])
            DMA(tabb[g], cvb[s][:].rearrange("p a b -> p (a b)"), r=[("cvb", s)], w=[("tabb", nm, g)], q="sync",
                sem_key=("tabb", s))
            cv_pos[0] += 1

        def prep_steps(h):
            hb = h % 2
            voff = 0 if hb == 0 else 64
            steps = []
            for blk in range(9):
                def st_k(blk=blk):
                    n = 512 if blk < 8 else 256
                    bank = 5 + (blk % 2)
                    MM(ps[bank][0:64, 0:n], wukvb[:, h * 128:h * 128 + 64], ckvnT[:, blk * 512:blk * 512 + n],
                       r=wkv_keys + ckeys, w=[("ps", bank)])
                    CP("vector", KT[hb][0:64, blk * 512:blk * 512 + n], ps[bank][0:64, 0:n], r=[("ps", bank)], w=[("KTn", hb)])
                steps.append(st_k)
            for g in range(5):
                def st_v(g=g):
                    cnt = 8 if g < 4 else 2
                    bank = 5 + (g % 2)
                    for j in range(cnt):
                        kt = g * 8 + j
                        MM(ps[bank][:, j * 64:(j + 1) * 64], ckvnT[:, kt * 128:(kt + 1) * 128], wukvb[:, h * 128 + 64:h * 128 + 128],
                           r=wkv_keys + ckeys, w=[("ps", bank)])
                    CP("vector", VH[hb][:, g * 8:g * 8 + cnt, voff:voff + 64],
                       ps[bank][:, 0:cnt * 64].rearrange("p (a b) -> p a b", b=64), r=[("ps", bank)], w=[("VH", hb)])
                steps.append(st_v)
            for qb in range(4):
                def st_q(qb=qb):
                    qs = slice(qb * 512, (qb + 1) * 512)
                    for k in range(2):
                        MM(ps[5][0:96, :], wuqb[:, k, h * 96:(h + 1) * 96], cqnT[:, k, qs], start=(k == 0), stop=(k == 1),
                           r=wq_keys + [("cqnT", k, qb)], w=[("ps", 5)])
                    for k in range(2):
                        MM(ps[6][0:96, :], wuqpb[:, k, h * 96:(h + 1) * 96], cqnT[:, k, qs], start=(k == 0), stop=(k == 1),
                           r=wqp_keys + [("cqnT", k, qb)], w=[("ps", 6)])
                    TT("vector", qt1[0:96, :], ps[5][0:96, :], rq[0:96, 0, qs], ALU.mult, r=[("ps", 5), ("rq", 0)], w=["qt1"])
                    TT("vector", qt2[0:96, :], ps[6][0:96, :], rq[0:96, 1, qs], ALU.mult, r=[("ps", 6), ("rq", 1)], w=["qt2"])
                    TT("vector", QT[hb][0:96, qs], qt1[0:96, :], qt2[0:96, :], ALU.add, r=["qt1", "qt2"], w=[("QT", hb)])
                steps.append(st_q)
            return steps

        def prep(h):
            for st_ in prep_steps(h):
                st_()

        prep(0)
        gi = [0]
        for h in range(NH):
            hb = h % 2
            ktr = [("KTn", hb)] + [("KTr", hb, b_) for b_ in range(9)]
            items = [(qb, kt) for qb in range(4) for kt in range(34)]
            base = gi[0]

            def S(idx):
                qb, kt = items[idx]
                sb_ = (base + idx) % 3
                MM(ps[sb_][:], KT[hb][0:96, kt * 128:(kt + 1) * 128], QT[hb][0:96, qb * 512:(qb + 1) * 512],
                   r=ktr + [("QT", hb)], w=[("ps", sb_)])

            pending = []
            nsteps = prep_steps(h + 1) if h + 1 < NH else []
            S(0)
            S(1)
            for idx, (qb, kt) in enumerate(items):
                if idx + 2 < len(items):
                    S(idx + 2)
                sb_ = (base + idx) % 3
                pb_ = (base + idx) % 4
                ob = 3 + (qb % 2)
                qs = slice(qb * 512, (qb + 1) * 512)
                ACT(PT[pb_][:], ps[sb_][:], AF.Exp, r=[("ps", sb_)], w=[("PT", pb_)], scale=ATTN_SCALE)
                MM(ps[ob][:], VH[hb][:, kt, :], PT[pb_][:], start=(kt == 0), stop=(kt == 33),
                   r=[("VH", hb), ("PT", pb_)], w=[("ps", ob)])
                if pending and pending[0][0] <= idx:
                    _, (r0, prow, ob2, qs2) = pending.pop(0)
                    MM(ps[7][:], onesf[r0:r0 + 1, :], rden[r0:r0 + 1, :], r=["rden"], w=[("ps", 7)])
                    CP("vector", rdenB[:], ps[7][:], r=[("ps", 7)], w=["rdenB"])
                    TT("vector", OT[prow, h // 2, qs2], ps[ob2][prow, :], rdenB[prow, :], ALU.mult, r=[("ps", ob2), "rdenB"],
                       w=[("OT", h, qs2.start // 512)])
                if kt == 33:
                    r0 = 64 if hb == 0 else 0
                    prow = slice(0, 64) if hb == 0 else slice(64, 128)
                    RECIP(rden[r0:r0 + 1, :], ps[ob][r0:r0 + 1, :], r=[("ps", ob)], w=["rden"])
                    pending.append((idx + 6 if qb < 3 else idx, (r0, prow, ob, qs)))
                if idx % 17 == 8:
                    conv_step()
                if idx >= 10 and idx % 5 == 0 and nsteps:
                    nsteps.pop(0)()
            while pending:
                _, (r0, prow, ob2, qs2) = pending.pop(0)
                MM(ps[7][:], onesf[r0:r0 + 1, :], rden[r0:r0 + 1, :], r=["rden"], w=[("ps", 7)])
                CP("vector", rdenB[:], ps[7][:], r=[("ps", 7)], w=["rdenB"])
                TT("vector", OT[prow, h // 2, qs2], ps[ob2][prow, :], rdenB[prow, :], ALU.mult, r=[("ps", ob2), "rdenB"],
                   w=[("OT", h, qs2.start // 512)])
            while nsteps:
                nsteps.pop(0)()
            gi[0] += len(items)
        while cv_pos[0] < len(cv_steps):
            conv_step()
        okeys = [("OT", h, qb) for h in range(NH) for qb in range(4)]
        if debug:
            dump("OT", OT[:].rearrange("p a b -> p (a b)"), okeys)
        if stop_after == "ATT":
            P.emit()
            return nc
        BARRIER()

        ar.off = RA
        mT = ar.alloc([8, NOWN], BF16)
        assert ar.off <= RB
        ar.off = RC
        hTo2 = ar.alloc([8, NOWN], BF16)
        zT2 = ar.alloc([8, NOWN], BF16)
        DMA(hTo2[:], hts.rearrange("p (k t) -> p k t", k=8), w=["hTo2"])
        DMA(zT2[:], zts.rearrange("p (k t) -> p k t", k=8), w=["zT2"], q="scalar")
        wm = [[ar.alloc([8, 128], BF16) for _ in range(4)] for _ in range(2)]
        sga = [ar.alloc([512], F32) for _ in range(2)]
        sgc = [ar.alloc([512], F32) for _ in range(2)]
        m1 = [ar.alloc([512], F32) for _ in range(2)]
        womla_v = womla.rearrange("(k p) f -> p k f", p=128)
        wpw_v = wpw.rearrange("(k p) f -> p k f", p=128)
        for c in range(8):
            ws = c % 2
            cs = slice(c * 128, (c + 1) * 128)
            load_w(wm[ws][0], womla_v[:, :, cs], 1024, ("wm", ws, 0))
            load_w(wm[ws][1], wpw_v[:, :, cs], 1024, ("wm", ws, 1))
            load_w(wm[ws][2], win_v[:, :, OFF_GATE + c * 128:OFF_GATE + (c + 1) * 128], 1024, ("wm", ws, 2))
            load_w(wm[ws][3], win_v[:, :, OFF_GATE + 1024 + c * 128:OFF_GATE + 1024 + (c + 1) * 128], 1024, ("wm", ws, 3))
            for tb in range(4):
                tsl = slice(tb * 512, (tb + 1) * 512)
                banks = (0, 1, 2, 3) if tb % 2 == 0 else (4, 5, 6, 7)
                srcs = [(OT, "OT"), (zT2, "zT2"), (hTo2, "hTo2"), (hTo2, "hTo2")]
                for i in range(4):
                    for k in range(8):
                        MM(ps[banks[i]][:], wm[ws][i][:, k, :], srcs[i][0][:, k, tsl], start=(k == 0), stop=(k == 7),
                           r=[("wm", ws, i), srcs[i][1]], w=[("ps", banks[i])])
                p2 = tb % 2
                ACT(sga[p2][:], ps[banks[2]][:], AF.Sigmoid, r=[("ps", banks[2])], w=[("sga", p2)])
                ACT(sgc[p2][:], ps[banks[3]][:], AF.Sigmoid, r=[("ps", banks[3])], w=[("sgc", p2)])
                TT("vector", m1[p2][:], ps[banks[0]][:], sga[p2][:], ALU.mult, r=[("ps", banks[0]), ("sga", p2)], w=[("m1", p2)])
                TT("vector", sgc[p2][:], ps[banks[1]][:], sgc[p2][:], ALU.mult, r=[("ps", banks[1]), ("sgc", p2)], w=[("sgc", p2)])
                TT("vector", mT[:, c, tsl], m1[p2][:], sgc[p2][:], ALU.add, r=[("m1", p2), ("sgc", p2)], w=[("mT", c, tb)])
        if stop_after == "M":
            P.emit()
            return nc
        BARRIER()

        ar.off = RB
        h2T = ar.alloc([8, NOWN], BF16)
        assert ar.off <= RC
        ar.off = RC
        woutb = ar.alloc([8, 1024], BF16)
        wout_v = wout.rearrange("(k p) f -> p k f", p=128)
        for k in range(8):
            load_w(woutb[:, k, :], wout_v[:, k, :], 1024, ("woutb", k))
        wok = [("woutb", k) for k in range(8)]
        xts = [ar.alloc([D], F32) for _ in range(2)]
        x1t = [ar.alloc([D], F32) for _ in range(2)]
        sqj = ar.alloc([D], F32)
        xns = [ar.alloc([D], BF16) for _ in range(2)]
        sss = [ar.alloc([1], F32) for _ in range(2)]
        rss = [ar.alloc([1], F32) for _ in range(2)]
        for tt in range(16):
            s = tt % 2
            DMA(xts[s][:], xo[tt], w=[("xo2", s)], q=dmaq[tt % 2])
            for half in range(2):
                bank = (4 + half) if s == 0 else (6 + half)
                hs = slice(half * 512, (half + 1) * 512)
                for k in range(8):
                    MM(ps[bank][:], mT[:, k, tt * 128:(tt + 1) * 128], woutb[:, k, hs], start=(k == 0), stop=(k == 7),
                       r=wok + ["mT"], w=[("ps", bank)])
                TT("vector", x1t[s][:, hs], ps[bank][:], g1row[:, hs], ALU.mult, r=[("ps", bank)], w=[("x1t", s, half)])
                TT("gpsimd", x1t[s][:, hs], x1t[s][:, hs], xts[s][:, hs], ALU.add, r=[("x1t", s, half), ("xo2", s)], w=[("x1t", s, half)])
            DMA(x1s[tt], x1t[s][:], r=[("x1t", s, 0), ("x1t", s, 1)], w=[("x1s", tt)])
            P.op("gpsimd", lambda e: e.memset(junk[:, 1:2], 0.0), reads=[("x1t", s, 0), ("x1t", s, 1)], writes=[(("n2", s), "xt")])
            norm_tile(None, h2T[:, :, tt * 128:(tt + 1) * 128], tt, A2[:], modT[:, 24:32, 0],
                      x1t[s][:], sqj[:], xns[s][:], sss[s][:], rss[s][:], 2 + s, ("n2", s), ("h2T", tt))
        if stop_after == "M2":
            P.emit()
            return nc
        BARRIER()

        ar.off = RA
        E1T = ar.alloc([NOWN], BF16)
        E2T = ar.alloc([NOWN], BF16)
        WT = ar.alloc([NOWN], F32)
        ar.off = RC
        subkb = ar.alloc([16, 128], BF16)
        wpqc = [ar.alloc([8, 128], BF16) for _ in range(2)]
        qT = ar.alloc([16, 512], BF16)
        scs = [ar.alloc([16, 128], F32) for _ in range(2)]
        sc2 = ar.alloc([16, 128], F32)
        stop_ = ar.alloc([16, 16], F32)
        itopu = ar.alloc([16, 16], U32)
        itopf = ar.alloc([16, 16], F32)
        cand = ar.alloc([8, 256], F32)
        cand2 = ar.alloc([8, 256], F32)
        best = ar.alloc([8, 16], F32)
        ciu = ar.alloc([8, 16], U32)
        cif = ar.alloc([8, 16], F32)
        c1f = ar.alloc([8, 16], F32)
        c2f = ar.alloc([8, 16], F32)
        eq = ar.alloc([8, 16, 16], F32)
        e1f = ar.alloc([8, 16], F32)
        e2f = ar.alloc([8, 16], F32)
        ex = ar.alloc([8, 16], F32)
        se = ar.alloc([8], F32)
        wg = ar.alloc([8, 16], F32)
        iota16 = ar.alloc([16], F32)
        thr16 = ar.alloc([16], F32)
        P.op("gpsimd", lambda e: e.iota(iota16[:], pattern=[[1, 16]], base=0, channel_multiplier=0,
                                        allow_small_or_imprecise_dtypes=True), writes=["iota16"])
        P.op("gpsimd", lambda e: e.iota(thr16[:], pattern=[[16, 16]], base=16, channel_multiplier=0,
                                        allow_small_or_imprecise_dtypes=True), writes=["thr16"])
        MEMSET("gpsimd", thr16[:, 15:16], 1.0e9, w=["thr16"])
        load_w(subkb[:, 0:8, :], subkT_d[:, 0:1024].rearrange("p (a b) -> p a b", b=128), 1024, ("subkb", 0))
        load_w(subkb[:, 8:16, :], subkT_d[:, 1024:2048].rearrange("p (a b) -> p a b", b=128), 1024, ("subkb", 1))
        wpq_v = wpq.rearrange("(k p) f -> p k f", p=128)
        itop_v = itopf[:].rearrange("p (h two) m -> p h two m", two=2)
        stop_v = stop_[:].rearrange("p (h two) m -> p h two m", two=2)
        B4 = [128, 8, 16, 16]
        for tb in range(4):
            for j in range(16):
                ws = j % 2
                load_w(wpqc[ws], wpq_v[:, :, j * 128:(j + 1) * 128], 1024, ("wpqc", ws))
                for k in range(8):
                    MM(ps[4 + ws][:], wpqc[ws][:, k, :], h2T[:, k, tb * 512:(tb + 1) * 512], start=(k == 0), stop=(k == 7),
                       r=[("wpqc", ws), "h2T"], w=[("ps", 4 + ws)])
                CP("vector" if j % 2 else "scalar", qT[:, j, :], ps[4 + ws][:], r=[("ps", 4 + ws)], w=[("qT", j)])
            def score(tt_):
                tl_ = tt_ % 4
                sc_ = scs[tt_ % 2]
                for j in range(16):
                    MM(ps[j // 4][:, (j % 4) * 128:(j % 4 + 1) * 128], qT[:, j, tl_ * 128:(tl_ + 1) * 128], subkb[:, j, :],
                       r=[("qT", j), ("subkb", 0), ("subkb", 1)], w=[("ps", j // 4)])
                for g in range(4):
                    CP("scalar", sc_[:, g * 4:(g + 1) * 4, :], ps[g][:].rearrange("p (a b) -> p a b", b=128), r=[("ps", g)],
                       w=[("sc", tt_ % 2, g)])

            score(tb * 4)
            for tl in range(4):
                tt = tb * 4 + tl
                if tl < 3:
                    score(tt + 1)
                sc = scs[tt % 2]
                def sk_(j):
                    return ("sc", tt % 2, j // 4)
                for j in range(16):
                    P.op("vector", lambda e, j=j, sc=sc: e.max(out=stop_[:, j, 0:8], in_=sc[:, j, :]), reads=[sk_(j)], writes=[("stop", j)])
                for j in range(16):
                    P.op("vector", lambda e, j=j, sc=sc: e.max_index(out=itopu[:, j, 0:8], in_max=stop_[:, j, 0:8], in_values=sc[:, j, :]),
                         reads=[sk_(j), ("stop", j)], writes=[("itopu", j)])
                for j in range(16):
                    P.op("vector", lambda e, j=j, sc=sc: e.match_replace(out=sc2[:, j, :], in_to_replace=stop_[:, j, 0:8], in_values=sc[:, j, :], imm_value=-1.0e30),
                         reads=[sk_(j), ("stop", j)], writes=[("sc2", j)])
                for j in range(16):
                    P.op("vector", lambda e, j=j: e.max(out=stop_[:, j, 8:16], in_=sc2[:, j, :]), reads=[("sc2", j)], writes=[("stop", j)])
                for j in range(16):
                    P.op("vector", lambda e, j=j: e.max_index(out=itopu[:, j, 8:16], in_max=stop_[:, j, 8:16], in_values=sc2[:, j, :]),
                         reads=[("sc2", j), ("stop", j)], writes=[("itopu", j)])
                sk = [("stop", j) for j in range(16)]
                ik = [("itopu", j) for j in range(16)]
                CP("vector", itopf[:], itopu[:], r=ik, w=["itopf"])
                TT("vector", cand[:].rearrange("p h (a b) -> p h a b", b=16), stop_v[:, :, 0, :].unsqueeze(3).to_broadcast(B4),
                   stop_v[:, :, 1, :].unsqueeze(2).to_broadcast(B4), ALU.add, r=sk, w=["cand"])
                for h in range(8):
                    P.op("vector", lambda e, h=h: e.max(out=best[:, h, 0:8], in_=cand[:, h, :]), reads=["cand"], writes=[("best", h)])
                for h in range(8):
                    P.op("vector", lambda e, h=h: e.max_index(out=ciu[:, h, 0:8], in_max=best[:, h, 0:8], in_values=cand[:, h, :]),
                         reads=["cand", ("best", h)], writes=[("ciu", h)])
                for h in range(8):
                    P.op("vector", lambda e, h=h: e.match_replace(out=cand2[:, h, :], in_to_replace=best[:, h, 0:8], in_values=cand[:, h, :], imm_value=-1.0e30),
                         reads=["cand", ("best", h)], writes=[("cand2", h)])
                for h in range(8):
                    P.op("vector", lambda e, h=h: e.max(out=best[:, h, 8:16], in_=cand2[:, h, :]), reads=[("cand2", h)], writes=[("best", h)])
                for h in range(8):
                    P.op("vector", lambda e, h=h: e.max_index(out=ciu[:, h, 8:16], in_max=best[:, h, 8:16], in_values=cand2[:, h, :]),
                         reads=[("cand2", h), ("best", h)], writes=[("ciu", h)])
                bk = [("best", h) for h in range(8)]
                ck = [("ciu", h) for h in range(8)]
                CP("vector", cif[:], ciu[:], r=ck, w=["cif"])
                TT("vector", eq[:], cif[:].unsqueeze(3).to_broadcast(B4), thr16[:].unsqueeze(1).unsqueeze(1).to_broadcast(B4), ALU.is_ge,
                   r=["cif", "thr16"], w=["eq"])
                P.op("vector", lambda e: e.tensor_reduce(out=c1f[:], in_=eq[:], axis=AX.X, op=ALU.add), reads=["eq"], writes=["c1f"])
                STT(c2f[:], c1f[:], -16.0, cif[:], ALU.mult, ALU.add, r=["c1f", "cif"], w=["c2f"])
                for (cf_, two, ef_) in ((c1f, 0, e1f), (c2f, 1, e2f)):
                    TT("vector", eq[:], cf_[:].unsqueeze(3).to_broadcast(B4), iota16[:].unsqueeze(1).unsqueeze(1).to_broadcast(B4), ALU.is_equal,
                       r=["c1f", "c2f", "iota16"], w=["eq"])
                    TT("vector", eq[:], eq[:], itop_v[:, :, two, :].unsqueeze(2).to_broadcast(B4), ALU.mult, r=["eq", "itopf"], w=["eq"])
                    P.op("vector", lambda e, ef_=ef_: e.tensor_reduce(out=ef_[:], in_=eq[:], axis=AX.X, op=ALU.add), reads=["eq"], writes=[("ef", two)])
                TT("vector", ex[:], best[:], best[:, :, 0:1].to_broadcast([128, 8, 16]), ALU.subtract, r=bk, w=["ex"])
                ACT(ex[:], ex[:], AF.Exp, r=["ex"], w=["ex"])
                P.op("vector", lambda e: e.tensor_reduce(out=se[:], in_=ex[:], axis=AX.X, op=ALU.add), reads=["ex"], writes=["se"])
                RECIP(se[:], se[:], r=["se"], w=["se"])
                TT("vector", wg[:], ex[:], se[:].unsqueeze(2).to_broadcast([128, 8, 16]), ALU.mult, r=["ex", "se"], w=["wg"])
                tsl = slice(tt * 128, (tt + 1) * 128)
                TR(ps[6][:, 0:128], e1f[:].rearrange("p a b -> p (a b)"), identf[:], r=[("ef", 0)], w=[("ps", 6)])
                TR(ps[6][:, 128:256], e2f[:].rearrange("p a b -> p (a b)"), identf[:], r=[("ef", 1)], w=[("ps", 6)])
                TR(ps[6][:, 256:384], wg[:].rearrange("p a b -> p (a b)"), identf[:], r=["wg"], w=[("ps", 6)])
                CP("scalar", E1T[:, tsl], ps[6][:, 0:128], r=[("ps", 6)], w=[("E1T", tt)])
                CP("scalar", E2T[:, tsl], ps[6][:, 128:256], r=[("ps", 6)], w=[("E2T", tt)])
                CP("scalar", WT[:, tsl], ps[6][:, 256:384], r=[("ps", 6)], w=[("WT", tt)])
        if stop_after == "P0":
            P.emit()
            return nc
        BARRIER()

        ar.off = RA + 16 * 1024
        E1Tm = ar.alloc([NOWN], BF16)
        iota3 = ar.alloc([8, 128], BF16)
        iotaH = ar.alloc([8, 64], BF16)
        xfin = ar.alloc([D], F32)
        x1r = ar.alloc([D], F32)
        assert ar.off <= RB
        ar.off = RC
        Wh = [ar.alloc([256, 64], BF16) for _ in range(2)]
        NSLOT = 3
        UTb = [ar.alloc([2, 1024], BF16) for _ in range(NSLOT)]
        Vb = [ar.alloc([2, 1024], BF16) for _ in range(NSLOT)]
        Gs = [ar.alloc([256], BF16) for _ in range(3)]
        GWs = [ar.alloc([256], BF16) for _ in range(3)]
        ssf = ar.alloc([1], F32)
        E1p = [ar.alloc([8, 64], BF16) for _ in range(3)]
        E2p = [ar.alloc([8, 128], BF16) for _ in range(3)]
        xsq = x1r.bitcast(BF16)[:, 0:D]
        P.op("gpsimd", lambda e: e.iota(iota3[:], pattern=[[0, 8], [1, 128]], base=0, channel_multiplier=0,
                                        allow_small_or_imprecise_dtypes=True), writes=["iota3"])
        P.op("gpsimd", lambda e: e.iota(iotaH[:], pattern=[[0, 8], [1, 64]], base=0, channel_multiplier=0,
                                        allow_small_or_imprecise_dtypes=True), writes=["iotaH"])
        TS("vector", E1Tm[:], E1T[:], -64.0, None, ALU.add, r=[], w=["E1Tm"])

        def b_eq(tb_, hf_, m):
            t0 = tb_ * 256 + m * 8
            bi = m % 3
            e1src = E1T if hf_ == 0 else E1Tm
            TT("vector", E1p[bi][:], iotaH[:], e1src[:, t0:t0 + 8].unsqueeze(2).to_broadcast([128, 8, 64]), ALU.is_equal,
               r=["iotaH", "E1Tm"], w=[("E1p", bi)])
            TT("vector", E2p[bi][:], iota3[:], E2T[:, t0:t0 + 8].unsqueeze(2).to_broadcast([128, 8, 128]), ALU.is_equal,
               r=["iota3"], w=[("E2p", bi)])

        def b_mul(tb_, hf_, m):
            t0 = tb_ * 256 + m * 8
            bi = m % 3
            TT("gpsimd", E2p[bi][:], E2p[bi][:], WT[:, t0:t0 + 8].unsqueeze(2).to_broadcast([128, 8, 128]), ALU.mult,
               r=[("E2p", bi)], w=[("E2p", bi)])

        def b_mm_pe(tb_, hf_, m):
            bi = m % 3
            for tq in range(8):
                MM(ps[6][:, tq * 64:(tq + 1) * 64], E2p[bi][:, tq, :], E1p[bi][:, tq, :], r=[("E1p", bi), ("E2p", bi)], w=[("ps", 6)])

        def b_mm_act(tb_, hf_, m):
            CP("scalar", Wh[hf_][:, m * 8:m * 8 + 8, :], ps[6][:].rearrange("p (a b) -> p a b", b=64), r=[("ps", 6)], w=[("Wh", hf_)])

        def b_mm(tb_, hf_, m):
            b_mm_pe(tb_, hf_, m)
            b_mm_act(tb_, hf_, m)

        def loadg(cg):
            s = cg % NSLOT
            DMA(UTb[s][:].rearrange("p a b -> p (a b)"), utb[cg], w=[("UTb", s)], q="sync")
            DMA(Vb[s][:].rearrange("p a b -> p (a b)"), vtb[cg], w=[("Vb", s)], q="sync")

        for m in range(32):
            b_eq(0, 0, m)
            b_mul(0, 0, m)
            b_mm(0, 0, m)
        for tb in range(8):
            T0 = tb * 256
            ABANK = (4, 5, 7)

            def Amm(i):
                s = (i // 2) % NSLOT
                ab = i % 3
                for k in range(8):
                    MM(ps[ABANK[ab]][:, 0:256], UTb[s][:, i % 2, k * 128:(k + 1) * 128], h2T[:, k, T0:T0 + 256], start=(k == 0), stop=(k == 7),
                       r=[("UTb", s), "h2T"], w=[("ps", ABANK[ab])])

            for c0 in range(NSLOT):
                loadg(c0)
            Amm(0)
            Amm(1)
            for i in range(128):
                hf2 = i // 64
                if i + 2 < 128:
                    Amm(i + 2)
                cg = i // 2
                s = cg % NSLOT
                ab = i % 3
                nxt = (tb, 1) if hf2 == 0 else ((tb + 1, 0) if tb + 1 < 8 else None)
                m = (i % 64) // 2
                do_b = nxt is not None and i % 2 == 0 and m >= 2
                if do_b:
                    b_mm_pe(nxt[0], nxt[1], m - 2)
                ACT(Gs[ab][:], ps[ABANK[ab]][:, 0:256], AF.Gelu, r=[("ps", ABANK[ab])], w=[("Gs", ab)])
                if do_b:
                    b_mm_act(nxt[0], nxt[1], m - 2)
                TT("vector", GWs[ab][:], Gs[ab][:], Wh[hf2][:, :, i - 64 * hf2], ALU.mult, r=[("Gs", ab), ("Wh", hf2)], w=[("GWs", ab)])
                for tl in range(2):
                    for half in range(2):
                        MM(ps[tl * 2 + half][:], GWs[ab][:, tl * 128:(tl + 1) * 128], Vb[s][:, i % 2, half * 512:(half + 1) * 512],
                           start=(i == 0), stop=(i == 127), r=[("GWs", ab), ("Vb", s)], w=[("ps", tl * 2 + half)])
                if i % 2 == 1 and cg + NSLOT < 64:
                    loadg(cg + NSLOT)
                if nxt is not None:
                    if i % 2 == 0:
                        b_eq(nxt[0], nxt[1], m)
                    else:
                        b_mul(nxt[0], nxt[1], m)
                        if m == 31:
                            b_mm(nxt[0], nxt[1], 30)
                            b_mm(nxt[0], nxt[1], 31)
            for tl in range(2):
                tt = tb * 2 + tl
                DMA(x1r[:], x1s[tt], w=["x1r"])
                for half in range(2):
                    hs = slice(half * 512, (half + 1) * 512)
                    TT("vector", xfin[:, hs], ps[tl * 2 + half][:], g2row[:, hs], ALU.mult, r=[("ps", tl * 2 + half)], w=["xfin"])
                TT("vector", xfin[:], xfin[:], x1r[:], ALU.add, r=["xfin", "x1r"], w=["xfin"])
                ACT(xsq, xfin[:], AF.Square, r=["xfin"], w=["x1r", "ssf"], accum=ssf[:])
                ACT(ssf[:], ssf[:], AF.Sqrt, r=["ssf"], w=["ssf"], scale=1.0 / D, bias=epsb[:])
                RECIP(ssf[:], ssf[:], r=["ssf"], w=["ssf"])
                STT(xfin[:], xfin[:], ssf[:, 0:1], fgrow[:], ALU.mult, ALU.mult, r=["xfin", "ssf"], w=["xfin"])
                DMA(out_d[tt], xfin[:], r=["xfin"], w=[("out", tt)], sem_key="out")

        P.emit()
    return nc


def _rope_tabs(pos):
    n_freq = 8
    freq = (np.float32(10000.0) ** (-np.arange(n_freq, dtype=np.float32) / np.float32(n_freq))).astype(np.float32)
    r = (pos // 64).astype(np.float32)
    c = (pos % 64).astype(np.float32)
    ang = np.stack([r[:, None] * freq[None, :], c[:, None] * freq[None, :]], axis=1).astype(np.float32)
    cos = np.cos(ang).astype(np.float32)
    sin = np.sin(ang).astype(np.float32)
    ct = np.zeros((32, pos.shape[0]), np.float32)
    stb = np.zeros((32, pos.shape[0]), np.float32)
    perm = np.zeros(32, np.int64)
    for a in range(2):
        for half in range(2):
            for f in range(8):
                d = a * 16 + half * 8 + f
                perm[d] = a * 16 + (1 - half) * 8 + f
                ct[d] = cos[:, a, f]
                stb[d] = -sin[:, a, f] if half == 0 else sin[:, a, f]
    return ct, stb, perm


def prep_inputs(inp):
    f = lambda a: np.ascontiguousarray(np.asarray(a, dtype=np.float32))
    x, c, ctx, c_ctx = f(inp["x"]), f(inp["c"]), f(inp["ctx"]), f(inp["c_ctx"])
    w_in = f(inp["w_in"])[0]
    _, _, perm = _rope_tabs(np.arange(4))
    shared = {}
    shared["wmod"] = f(inp["w_mod"])[0]
    b_mod = f(inp["b_mod"])[0]
    shared["bmodg"] = np.ascontiguousarray(np.broadcast_to(np.concatenate([b_mod[2048:3072], b_mod[5120:6144]])[None, :], (128, 2048)))
    shared["fgrow"] = np.ascontiguousarray(np.broadcast_to(f(inp["final_g"])[None, :], (128, D)))
    shared["win"] = w_in
    wkr = np.zeros((D, 192), np.float32)
    wkr[:, 64:96] = w_in[:, OFF_KR:OFF_KR + 32]
    wkr[:, 160:192] = w_in[:, OFF_KR + perm]
    shared["wkr"] = wkr
    w_uq = f(inp["w_uq"])[0]
    shared["wuq"] = w_uq
    colp = np.arange(1536).reshape(16, 96).copy()
    for h in range(16):
        colp[h, 64:96] = h * 96 + 64 + perm
    shared["wuqp"] = np.ascontiguousarray(w_uq[:, colp.reshape(-1)])
    shared["wukv"] = f(inp["w_ukv"])[0]
    shared["womla"] = f(inp["w_o_mla"])[0]
    shared["wpw"] = f(inp["w_pw"])[0]
    shared["wout"] = f(inp["w_out"])[0]
    shared["wpq"] = f(inp["w_pq"])[0]
    sk = f(inp["sub_keys"])[0].reshape(16, 128, 128)
    shared["subkT"] = np.ascontiguousarray(sk.transpose(2, 0, 1).reshape(128, 16 * 128))
    u = f(inp["u_experts"])[0]
    shared["ut"] = np.ascontiguousarray(u.reshape(128, 128, 8, 128).transpose(0, 3, 2, 1).reshape(128, 128, 1024))
    shared["vt"] = f(inp["v_experts"])[0].reshape(128, 128, 1024)
    ck, sk_, _ = _rope_tabs(np.arange(SEQ))
    ropek = np.zeros((2, 128, SEQ), np.float32)
    ropek[0, 64:96] = ck
    ropek[1, 64:96] = sk_
    shared["ropek"] = ropek

    def tomaj(v):
        return v.reshape(-1, 128).T

    maps = []
    for core in range(8):
        b, hf = core // 2, core % 2
        m = dict(shared)
        m["xk"] = np.ascontiguousarray(np.concatenate([x[b], ctx[b]], axis=0).reshape(34, 128, D))
        xo = np.zeros((17 * 128, D), np.float32)
        lo = hf * NOWN
        xo[:NOWN] = x[b, lo:lo + NOWN]
        if hf == 1:
            xo[NOWN:NOWN + 15] = x[b, lo - 15:lo]
        else:
            xo[NOWN + 15:NOWN + 30] = x[b, lo + NOWN:lo + NOWN + 15]
        m["xo"] = xo.reshape(17, 128, D)
        vecs = np.zeros((128, NV), np.float32)
        vecs[:, 0:8] = tomaj(f(inp["norm1_g"])[0])
        vecs[:, 8:16] = tomaj(f(inp["norm2_g"])[0])
        vecs[:, 16:18] = tomaj(f(inp["q_norm_g"])[0])
        vecs[:, 18:19] = tomaj(f(inp["kv_norm_g"])[0])
        vecs[:, 19:27] = tomaj(f(inp["conv_b"])[0])
        vecs[:, 27:35] = tomaj(f(inp["conv_ln_g"])[0])
        vecs[:, 35:43] = tomaj(f(inp["conv_ln_b"])[0])
        cw = f(inp["conv_w"])[0]
        vecs[:, 43:291] = cw.reshape(31, 8, 128).transpose(2, 1, 0).reshape(128, 248)
        vecs[:, 291:339] = tomaj(b_mod)
        ct2 = np.stack([tomaj(c[b]), tomaj(c_ctx)], axis=2)
        vecs[:, 339:355] = ct2.reshape(128, 16)
        vecs[:, 355] = 1.0 if hf == 1 else 0.0
        vecs[:, 356] = 1.0 if hf == 0 else 0.0
        m["vecs"] = vecs
        cq_, sq_, _ = _rope_tabs(np.arange(lo, lo + NOWN))
        ropeq = np.zeros((2, 128, NOWN), np.float32)
        ropeq[0, 0:64] = 1.0
        ropeq[0, 64:96] = cq_
        ropeq[1, 64:96] = sq_
        m["ropeq"] = ropeq
        maps.append(m)
    return maps


_NC_CACHE = {}


def kernel(**inputs):
    maps = prep_inputs(inputs)
    if "nc" not in _NC_CACHE:
        _NC_CACHE["nc"] = build_nc()
    nc = _NC_CACHE["nc"]
    res = run_bass_kernel_spmd(nc, maps, core_ids=list(range(8)))
    out = np.zeros((4, SEQ, D), np.float32)
    for core in range(8):
        b, hf = core // 2, core % 2
        out[b, hf * NOWN:(hf + 1) * NOWN] = np.asarray(res.results[core]["out"]).reshape(NOWN, D)
    return out
```

```python
import numpy as np
from contextlib import ExitStack
import concourse.bass as bass
import concourse.mybir as mybir
from concourse.bass_utils import run_bass_kernel_spmd

F32 = mybir.dt.float32
BF16 = mybir.dt.bfloat16
U32 = mybir.dt.uint32
ALU = mybir.AluOpType
AF = mybir.ActivationFunctionType
AX = mybir.AxisListType
ENGS = ("tensor", "vector", "scalar", "gpsimd", "sync")

D = 1024
SEQ = 4096
CTX = 256
NKEY = SEQ + CTX
NOWN = 2048
NH = 16
EPS = 1e-6
ATTN_SCALE = 96 ** -0.5
OFF_KV = 256
OFF_KR = 384
OFF_CONV = 416
OFF_GATE = 2464
NV = 360
CUT = 0


class _Op:
    __slots__ = ("eng", "fn", "waits", "signal", "sem", "val", "is_dma", "inc")

    def __init__(self, eng, fn):
        self.eng = eng
        self.fn = fn
        self.waits = []
        self.signal = False
        self.sem = None
        self.val = 0
        self.is_dma = False
        self.inc = 1


class Prog:
    def __init__(self, nc):
        self.nc = nc
        self.ops = {e: [] for e in ENGS}
        self.last_w = {}
        self.readers = {}
        self.all_ops = []
        self.epoch = None
        self.last_by_sem = {}

    def op(self, eng, fn, reads=(), writes=(), dma=False, sem_key=None):
        o = _Op(eng, fn)
        o.is_dma = dma
        deps = []
        for r in reads:
            w = self.last_w.get(r, self.epoch)
            if w is not None:
                deps.append((w, True))
        for r in writes:
            w = self.last_w.get(r, self.epoch)
            if w is not None:
                deps.append((w, False))
            for rd in self.readers.get(r, {}).values():
                deps.append((rd, False))
        seen = set()
        for d, raw in deps:
            if id(d) in seen:
                continue
            same = (not d.is_dma) and (not dma) and d.eng == eng
            if same and eng == "tensor":
                continue
            seen.add(id(d))
            o.waits.append(d)
            d.signal = True
        if dma:
            o.sem = ("dma", sem_key if sem_key is not None else (writes[0] if writes else reads[0]))
            o.inc = 16
            o.signal = True
        else:
            o.sem = ("eng", eng)
        for r in reads:
            self.readers.setdefault(r, {})[o.sem] = o
        for r in writes:
            self.last_w[r] = o
            self.readers[r] = {}
        self.ops[eng].append(o)
        self.all_ops.append(o)
        self.last_by_sem[o.sem] = o
        return o

    def barrier(self, fn):
        o = _Op("gpsimd", fn)
        o.sem = ("eng", "gpsimd")
        for d in self.last_by_sem.values():
            if d.eng == "gpsimd" and not d.is_dma:
                continue
            o.waits.append(d)
            d.signal = True
        o.signal = True
        self.ops["gpsimd"].append(o)
        self.all_ops.append(o)
        self.last_by_sem[o.sem] = o
        self.last_w = {}
        self.readers = {}
        self.epoch = o

    def emit(self, final_wait_eng="sync"):
        nc = self.nc
        last_dma = {}
        for o in self.all_ops:
            if o.is_dma:
                last_dma[o.sem] = o
        counters = {}
        for o in self.all_ops:
            if o.signal:
                counters[o.sem] = counters.get(o.sem, 0) + o.inc
                o.val = counters[o.sem]
        sem_keys = list(counters.keys())
        self.n_sems = len(sem_keys)
        with ExitStack() as st:
            sems = {}
            for i, k in enumerate(sem_keys):
                sems[k] = st.enter_context(nc.semaphore("s%d" % i))
            block = st.enter_context(nc.Block())

            def make(engname):
                ops = self.ops[engname]

                def body(e):
                    waited = {}
                    for o in ops:
                        for d in o.waits:
                            if waited.get(d.sem, 0) >= d.val:
                                continue
                            e.wait_ge(sems[d.sem], d.val)
                            waited[d.sem] = d.val
                        inst = o.fn(e)
                        if o.signal:
                            inst.then_inc(sems[o.sem], o.inc)
                    if engname == final_wait_eng:
                        for k, o in last_dma.items():
                            if waited.get(k, 0) < o.val:
                                e.wait_ge(sems[k], o.val)
                return body

            for engname in ENGS:
                if not self.ops[engname] and engname != final_wait_eng:
                    continue
                getattr(block, engname)(make(engname))


class Arena:
    def __init__(self, tensor, nbytes):
        self.t = tensor
        self.n = nbytes
        self.off = 0

    def alloc(self, shape_free, dt):
        es = 4 if dt in (F32, U32) else 2
        n = int(np.prod(shape_free)) * es
        n_al = (n + 63) // 64 * 64
        assert self.off + n_al <= self.n, ("arena overflow", self.off, n_al, self.n)
        v = self.t[:, self.off // 2:(self.off + n) // 2]
        self.off += n_al
        if es == 4:
            v = v.bitcast(dt)
        if len(shape_free) == 2:
            v = v.rearrange("p (a b) -> p a b", b=shape_free[1])
        elif len(shape_free) == 3:
            v = v.rearrange("p (a b c) -> p a b c", b=shape_free[1], c=shape_free[2])
        return v

    def mark(self):
        return self.off

    def release(self, m):
        self.off = m


def build_nc(stop_after=None, debug=False):
    nc = bass.Bass("TRN2", target_bir_lowering=False)

    def din(name, shape, dt=F32):
        return nc.dram_tensor(name, list(shape), dt, kind="ExternalInput").ap()

    xk = din("xk", [34, 128, D])
    xo = din("xo", [17, 128, D])
    vecs_d = din("vecs", [128, NV])
    wmod = din("wmod", [D, 6 * D])
    bmodg = din("bmodg", [128, 2048])
    fgrow_d = din("fgrow", [128, D])
    win = din("win", [D, 4512])
    wkr = din("wkr", [D, 192])
    wuq = din("wuq", [256, 1536])
    wuqp = din("wuqp", [256, 1536])
    wukv = din("wukv", [128, 2048])
    womla = din("womla", [D, D])
    wpw = din("wpw", [D, D])
    wout = din("wout", [D, D])
    wpq = din("wpq", [D, 2048])
    subkT_d = din("subkT", [128, 16 * 128])
    ut = din("ut", [128, 128, 1024])
    vt = din("vt", [128, 128, 1024])
    ropek = din("ropek", [2, 128, SEQ])
    ropeq = din("ropeq", [2, 128, NOWN])
    out_d = nc.dram_tensor("out", [16, 128, D], F32, kind="ExternalOutput").ap()
    utb = nc.dram_tensor("utb", [64, 128, 2048], BF16, kind="Internal").ap()
    vtb = nc.dram_tensor("vtb", [64, 128, 2048], BF16, kind="Internal").ap()
    x1s = nc.dram_tensor("x1s", [16, 128, D], F32, kind="Internal").ap()
    hts = nc.dram_tensor("hts", [128, 8 * NOWN], BF16, kind="Internal").ap()
    zts = nc.dram_tensor("zts", [128, 8 * NOWN], BF16, kind="Internal").ap()
    dbg = {}
    if debug:
        for nm, shp in debug.items():
            dt_ = F32
            if isinstance(shp, tuple) and len(shp) == 2 and shp[1] == "bf16":
                shp, dt_ = shp[0], BF16
            dbg[nm] = nc.dram_tensor("dbg_" + nm, list(shp), dt_, kind="ExternalOutput").ap()

    st = ExitStack()
    with st:
        def sbuf(name, shape, dt):
            return st.enter_context(nc.sbuf_tensor("sb_" + name, list(shape), dt))

        ARENA_BYTES = 172 * 1024
        arena_t = sbuf("arena", [128, ARENA_BYTES // 2], BF16)
        ar = Arena(arena_t, ARENA_BYTES)
        identb = sbuf("identb", [128, 128], BF16)
        identf = sbuf("identf", [128, 128], F32)
        onesb = sbuf("onesb", [128, 128], BF16)
        onesf = sbuf("onesf", [128, 128], F32)
        vecs = sbuf("vecs", [128, NV], F32)
        scT = sbuf("scT", [128, 8, 2], F32)
        screp = sbuf("screp", [128, 8, 128], F32)
        modT = sbuf("modT", [128, 48, 2], F32)
        A1 = sbuf("A1", [128, 8, 2], F32)
        A2 = sbuf("A2", [128, 8], F32)
        g1row = sbuf("g1row", [128, D], F32)
        g2row = sbuf("g2row", [128, D], F32)
        fgrow = sbuf("fgrow", [128, D], F32)
        stage = sbuf("stage", [128, 2, 1024], F32)
        junk = sbuf("junk", [128, 8], F32)
        ps = [st.enter_context(nc.psum_tensor("ps%d" % i, [128, 512], F32)) for i in range(8)]

        P = Prog(nc)

        def MM(out, lhsT, rhs, start=True, stop=True, r=(), w=()):
            P.op("tensor", lambda e: e.matmul(out, lhsT=lhsT, rhs=rhs, start=start, stop=stop), reads=r, writes=w)

        def TR(out, in_, ident, r=(), w=()):
            P.op("tensor", lambda e: e.transpose(out=out, in_=in_, identity=ident), reads=r, writes=w)

        def ACT(out, in_, func, r=(), w=(), scale=None, bias=None, accum=None):
            kw = {}
            if scale is not None:
                kw["scale"] = scale
            if bias is not None:
                kw["bias"] = bias
            if accum is not None:
                kw["accum_out"] = accum
            P.op("scalar", lambda e: e.activation(out=out, in_=in_, func=func, **kw), reads=r, writes=w)

        def TT(eng, out, in0, in1, op, r=(), w=()):
            P.op(eng, lambda e: e.tensor_tensor(out=out, in0=in0, in1=in1, op=op), reads=r, writes=w)

        def TS(eng, out, in0, s1, s2, op0, op1=None, r=(), w=()):
            if op1 is None:
                P.op(eng, lambda e: e.tensor_scalar(out=out, in0=in0, scalar1=s1, scalar2=None, op0=op0), reads=r, writes=w)
            else:
                P.op(eng, lambda e: e.tensor_scalar(out=out, in0=in0, scalar1=s1, scalar2=s2, op0=op0, op1=op1), reads=r, writes=w)

        def STT(out, in0, scalar, in1, op0, op1, r=(), w=()):
            P.op("vector", lambda e: e.scalar_tensor_tensor(out=out, in0=in0, scalar=scalar, in1=in1, op0=op0, op1=op1), reads=r, writes=w)

        def CP(eng, out, in_, r=(), w=()):
            if eng == "scalar":
                P.op(eng, lambda e: e.copy(out=out, in_=in_), reads=r, writes=w)
            else:
                P.op(eng, lambda e: e.tensor_copy(out=out, in_=in_), reads=r, writes=w)

        def RECIP(out, in_, r=(), w=()):
            P.op("vector", lambda e: e.reciprocal(out=out, in_=in_), reads=r, writes=w)

        def MEMSET(eng, ap, val, w=()):
            P.op(eng, lambda e: e.memset(ap, val), writes=w)

        dmaq = ["sync", "scalar"]
        dma_i = [0]

        def DMA(out, in_, r=(), w=(), q=None, sem_key=None):
            if q is None:
                q = "sync"
            P.op(q, lambda e: e.dma_start(out=out, in_=in_), reads=r, writes=w, dma=True, sem_key=sem_key)

        def BARRIER():
            P.barrier(lambda e: e.memset(junk[:, 0:1], 0.0))
            ar.off = RC

        RA, RB, RC = 0, 34 * 1024, 67 * 1024
        stage_i = [0]
        cv_i = [0]

        def load_w(dst, src, ncols, wkey, conv_eng=None):
            assert ncols <= 1024
            s = stage_i[0] % 2
            stage_i[0] += 1
            sv = stage[:, s, 0:ncols]
            if len(src.shape) == 3:
                sv = sv.rearrange("p (a b) -> p a b", b=src.shape[2])
            DMA(sv, src, w=[("stage", s)])
            if conv_eng is None:
                conv_eng = ("gpsimd", "vector")[cv_i[0] % 2]
                cv_i[0] += 1
            CP(conv_eng, dst, sv, r=[("stage", s)], w=[wkey])

        def dump(name, src, r):
            if debug and name in dbg:
                DMA(dbg[name], src, r=r, w=[("dbg", name)])

        MEMSET("gpsimd", identf[:], 0.0, w=["identf"])
        P.op("gpsimd", lambda e: e.affine_select(out=identf[:], in_=identf[:], pattern=[[-1, 128]], base=0,
                                                 channel_multiplier=1, compare_op=ALU.not_equal, fill=1.0),
             reads=["identf"], writes=["identf"])
        CP("vector", identb[:], identf[:], r=["identf"], w=["identb"])
        MEMSET("vector", onesb[:], 1.0, w=["onesb"])
        MEMSET("vector", onesf[:], 1.0, w=["onesf"])
        DMA(vecs[:], vecs_d, w=["vecs"])
        DMA(fgrow[:], fgrow_d, w=["fgrow"])
        epsb = sbuf("epsb", [128, 1], F32)
        MEMSET("vector", epsb[:], EPS, w=["epsb"])
        MEMSET("vector", modT[:], 0.0, w=["modT"])
        V_N1, V_N2, V_QG, V_KVG, V_CB, V_LNG, V_LNB, V_CW, V_BM, V_CT, V_HM = 0, 8, 16, 18, 19, 27, 35, 43, 291, 339, 355

        cT = vecs[:, V_CT:V_CT + 16].rearrange("p (k j) -> p k j", j=2)
        ACT(scT[:], cT, AF.Silu, r=["vecs"], w=["scT"])
        CP("vector", screp[:], scT[:, :, 0:1].to_broadcast([128, 8, 128]), r=["scT"], w=["screp"])
        ar.off = RC
        wmb = [ar.alloc([8, 512], F32) for _ in range(2)]
        bg = ar.alloc([2048], F32)
        DMA(bg, bmodg, w=["bg"])
        wmod_v = wmod.rearrange("(k p) f -> p k f", p=128)
        for blk in range(12):
            s = blk % 2
            DMA(wmb[s], wmod_v[:, :, blk * 512:(blk + 1) * 512], w=[("wmb", s)], q=dmaq[blk % 2])
            if blk in (4, 5, 10, 11):
                row = g1row if blk < 6 else g2row
                half = blk % 2
                for k in range(8):
                    MM(ps[0][:], screp[:, k, :], wmb[s][:, k, :], start=(k == 0), stop=(k == 7),
                       r=["screp", ("wmb", s)], w=[("ps", 0)])
                goff = (0 if blk < 6 else 1024) + half * 512
                TT("vector", row[:, half * 512:(half + 1) * 512], ps[0][:], bg[:, goff:goff + 512], ALU.add,
                   r=[("ps", 0), "bg"], w=[("row", blk)])
            else:
                for fc in range(4):
                    for k in range(8):
                        MM(ps[1][:, fc * 2:fc * 2 + 2], wmb[s][:, k, fc * 128:(fc + 1) * 128], scT[:, k, :],
                           start=(k == 0), stop=(k == 7), r=["scT", ("wmb", s)], w=[("ps", 1)])
                for fc in range(4):
                    f = blk * 4 + fc
                    TS("vector", modT[:, f, :], ps[1][:, fc * 2:fc * 2 + 2], vecs[:, V_BM + f:V_BM + f + 1], None, ALU.add,
                       r=[("ps", 1), "vecs"], w=["modT"])
        for j in range(2):
            STT(A1[:, :, j], modT[:, 8:16, j], 1.0, vecs[:, V_N1:V_N1 + 8], ALU.add, ALU.mult, r=["modT", "vecs"], w=["A1"])
        STT(A2[:], modT[:, 32:40, 0], 1.0, vecs[:, V_N2:V_N2 + 8], ALU.add, ALU.mult, r=["modT", "vecs"], w=["A2"])
        dump("modT", modT[:].rearrange("p a b -> p (a b)"), ["modT"])
        dump("g1row", g1row[:], [("row", 4), ("row", 5)])
        if stop_after == "mod":
            P.emit()
            return nc
        BARRIER()

        ar.off = RA
        ckvnT = ar.alloc([NKEY], BF16)
        KT = [ar.alloc([NKEY], BF16) for _ in range(2)]
        cqnT = ar.alloc([2, NOWN], BF16)
        assert ar.off <= RB
        ar.off = RC

        def norm_tile(src_tile, dst, j, Asc, Bsh, xt, sq, xn, ss, rs, psb, tag, dkey):
            if src_tile is not None:
                DMA(xt, src_tile, w=[(tag, "xt")], q=dmaq[j % 2])
            if CUT == 5:
                return
            ACT(sq, xt, AF.Square, r=[(tag, "xt")], w=[(tag, "sq"), (tag, "ss")], accum=ss)
            if CUT == 6:
                return
            ACT(rs, ss, AF.Sqrt, r=[(tag, "ss")], w=[(tag, "rs")], scale=1.0 / D, bias=epsb[:])
            if CUT == 7:
                return
            RECIP(rs, rs, r=[(tag, "rs")], w=[(tag, "rs")])
            TS("vector", xn, xt, rs, None, ALU.mult, r=[(tag, "xt"), (tag, "rs")], w=[(tag, "xn")])
            if CUT == 8:
                return
            pb = ps[psb][:].bitcast(BF16)
            for k in range(8):
                TR(pb[:, k * 128:(k + 1) * 128], xn[:, k * 128:(k + 1) * 128], identb[:], r=[(tag, "xn"), "identb"], w=[("ps", psb)])
            if CUT == 9:
                return
            pb3 = pb.rearrange("p (k t) -> p k t", k=8)
            tmpn = tmpns[j % 2]
            TT("vector", tmpn[:], pb3, Asc.unsqueeze(2).to_broadcast([128, 8, 128]), ALU.mult,
               r=[("ps", psb), "A1", "A2", "modT"], w=[("tmpn", j % 2)])
            TT("gpsimd", dst, tmpn[:], Bsh.unsqueeze(2).to_broadcast([128, 8, 128]), ALU.add,
               r=[("tmpn", j % 2), "A1", "A2", "modT"], w=[(dkey, 0)])

        tmpns = [sbuf("tmpn%d" % i, [128, 8, 128], F32) for i in range(2)]

        xts = [ar.alloc([D], F32) for _ in range(2)]
        sqj = ar.alloc([D], F32)
        xns = [ar.alloc([D], BF16) for _ in range(2)]
        sss = [ar.alloc([1], F32) for _ in range(2)]
        rss = [ar.alloc([1], F32) for _ in range(2)]
        hTb = [ar.alloc([8, 512], BF16) for _ in range(2)]
        wkvb = ar.alloc([8, 320], BF16)
        sqb = ar.alloc([512], BF16)
        rstd = ar.alloc([512], F32)
        rk = [ar.alloc([2, 512], F32) for _ in range(2)]
        t1 = ar.alloc([512], F32)
        t2 = ar.alloc([512], F32)
        win_v = win.rearrange("(k p) f -> p k f", p=128)
        wkr_v = wkr.rearrange("(k p) f -> p k f", p=128)
        load_w(wkvb[:, :, 0:128], win_v[:, :, OFF_KV:OFF_KV + 128], 8 * 128, ("wkvb", 0))
        load_w(wkvb[:, 0:4, 128:320], wkr_v[:, 0:4, :], 4 * 192, ("wkvb", 1))
        load_w(wkvb[:, 4:8, 128:320], wkr_v[:, 4:8, :], 4 * 192, ("wkvb", 2))
        ti = 0
        if CUT == 1:
            P.emit()
            return nc
        for blk in range(9):
            ntile = 4 if blk < 8 else 2
            n = ntile * 128
            hb = hTb[blk % 2]
            jm = 0 if blk < 8 else 1
            for tl in range(ntile):
                t = blk * 4 + tl
                s = ti % 2
                norm_tile(xk[t], hb[:, :, tl * 128:(tl + 1) * 128], ti, A1[:, :, jm], modT[:, 0:8, jm],
                          xts[s], sqj, xns[s], sss[s], rss[s], 2 + s, ("nk", s), ("hTb", blk % 2, s))
                ti += 1
            hr = [(("hTb", blk % 2, s_), 0) for s_ in range(2)]
            if CUT == 2 or CUT >= 5:
                P.emit()
                return nc
            for k in range(8):
                MM(ps[4][:, 0:n], wkvb[:, k, 0:128], hb[:, k, 0:n], start=(k == 0), stop=(k == 7),
                   r=hr + [("wkvb", 0)], w=[("ps", 4)])
            for k in range(8):
                MM(ps[5][0:96, 0:n], wkvb[:, k, 128:224], hb[:, k, 0:n], start=(k == 0), stop=(k == 7),
                   r=hr + [("wkvb", 1), ("wkvb", 2)], w=[("ps", 5)])
            if blk < 8:
                for k in range(8):
                    MM(ps[6][0:96, 0:n], wkvb[:, k, 224:320], hb[:, k, 0:n], start=(k == 0), stop=(k == 7),
                       r=hr + [("wkvb", 1), ("wkvb", 2)], w=[("ps", 6)])
            if CUT == 3:
                P.emit()
                return nc
            ACT(sqb[:, 0:n], ps[4][:, 0:n], AF.Square, r=[("ps", 4)], w=["sqb"])
            MM(ps[7][:, 0:n], onesb[:], sqb[:, 0:n], r=["onesb", "sqb"], w=[("ps", 7)])
            ACT(rstd[:, 0:n], ps[7][:, 0:n], AF.Sqrt, r=[("ps", 7)], w=["rstd"], scale=1.0 / 128, bias=epsb[:])
            RECIP(rstd[:, 0:n], rstd[:, 0:n], r=["rstd"], w=["rstd"])
            STT(ckvnT[:, blk * 512:blk * 512 + n], ps[4][:, 0:n], vecs[:, V_KVG:V_KVG + 1], rstd[:, 0:n], ALU.mult, ALU.mult,
                r=[("ps", 4), "rstd", "vecs"], w=[("ckvnT", blk)])
            if CUT == 4:
                P.emit()
                return nc
            if blk < 8:
                rkb = rk[blk % 2]
                DMA(rkb[64:96, 0, :], ropek[0, 64:96, blk * 512:(blk + 1) * 512], w=[("rk", blk % 2, 0)], q="scalar")
                DMA(rkb[64:96, 1, :], ropek[1, 64:96, blk * 512:(blk + 1) * 512], w=[("rk", blk % 2, 1)], q="scalar")
                TT("vector", t1[64:96, :], ps[5][64:96, :], rkb[64:96, 0, :], ALU.mult, r=[("ps", 5), ("rk", blk % 2, 0)], w=["t1"])
                TT("vector", t2[64:96, :], ps[6][64:96, :], rkb[64:96, 1, :], ALU.mult, r=[("ps", 6), ("rk", blk % 2, 1)], w=["t2"])
                TT("vector", KT[0][64:96, blk * 512:(blk + 1) * 512], t1[64:96, :], t2[64:96, :], ALU.add, r=["t1", "t2"], w=[("KTr", 0, blk)])
                CP("gpsimd", KT[1][64:96, blk * 512:(blk + 1) * 512], KT[0][64:96, blk * 512:(blk + 1) * 512], r=[("KTr", 0, blk)], w=[("KTr", 1, blk)])
            else:
                CP("vector", KT[0][64:96, blk * 512:blk * 512 + n], ps[5][64:96, 0:n], r=[("ps", 5)], w=[("KTr", 0, blk)])
                CP("gpsimd", KT[1][64:96, blk * 512:blk * 512 + n], KT[0][64:96, blk * 512:blk * 512 + n], r=[("KTr", 0, blk)], w=[("KTr", 1, blk)])
        if debug:
            dk = ar.alloc([NKEY], F32)
            CP("vector", dk[:], ckvnT[:], r=[("ckvnT", b) for b in range(9)], w=["dk"])
            dump("ckvnT", dk[:], ["dk"])
            dk2 = ar.alloc([NKEY], F32)
            CP("vector", dk2[64:96, :], KT[1][64:96, :], r=[("KTr", 1, b) for b in range(9)], w=["dk2"])
            dump("krot", dk2[64:96, :], ["dk2"])
        if stop_after == "K":
            P.emit()
            return nc
        BARRIER()

        ar.off = RB
        ypad = ar.alloc([8, NOWN + 30], BF16)
        assert ar.off <= RC
        ar.off = RC
        xts = [ar.alloc([D], F32) for _ in range(2)]
        sqj = ar.alloc([D], F32)
        xns = [ar.alloc([D], BF16) for _ in range(2)]
        sss = [ar.alloc([1], F32) for _ in range(2)]
        rss = [ar.alloc([1], F32) for _ in range(2)]
        hTo = ar.alloc([8, 17 * 128], BF16)
        wch = [ar.alloc([8, 128], BF16) for _ in range(4)]
        sg = [ar.alloc([512], F32) for _ in range(2)]
        yh = ar.alloc([8, 128], BF16)
        sqb2 = ar.alloc([2, 512], BF16)
        rstd = ar.alloc([512], F32)
        for t in range(17):
            s = t % 2
            norm_tile(xo[t], hTo[:, :, t * 128:(t + 1) * 128], t, A1[:, :, 0], modT[:, 0:8, 0],
                      xts[s], sqj, xns[s], sss[s], rss[s], 2 + s, ("no", s), ("hTo", t))
        DMA(hts.rearrange("p (k t) -> p k t", k=8), hTo[:, :, 0:NOWN],
            r=[(("hTo", t), 0) for t in range(16)], w=["hts"])
        blocks = [(tb * 512, 512, [4 * tb + i for i in range(4)]) for tb in range(4)] + [(2048, 128, [16])]
        load_w(wch[0], win_v[:, :, 0:128], 1024, ("wch", 0))
        load_w(wch[1], win_v[:, :, 128:256], 1024, ("wch", 1))
        for bi in range(4):
            t0, n, tiles = blocks[bi]
            hr = [(("hTo", t), 0) for t in tiles]
            for cc in range(2):
                for k in range(8):
                    MM(ps[4 + cc][:], wch[cc][:, k, :], hTo[:, k, t0:t0 + n], start=(k == 0), stop=(k == 7),
                       r=hr + [("wch", cc)], w=[("ps", 4 + cc)])
                ACT(sqb2[:, cc, :], ps[4 + cc][:], AF.Square, r=[("ps", 4 + cc)], w=[("sqb2", cc)])
            MM(ps[6][:], onesb[:], sqb2[:, 0, :], start=True, stop=False, r=[("sqb2", 0)], w=[("ps", 6)])
            MM(ps[6][:], onesb[:], sqb2[:, 1, :], start=False, stop=True, r=[("sqb2", 1)], w=[("ps", 6)])
            ACT(rstd[:], ps[6][:], AF.Sqrt, r=[("ps", 6)], w=["rstd"], scale=1.0 / 256, bias=epsb[:])
            RECIP(rstd[:], rstd[:], r=["rstd"], w=["rstd"])
            for cc in range(2):
                STT(cqnT[:, cc, t0:t0 + n], ps[4 + cc][:], vecs[:, V_QG + cc:V_QG + cc + 1], rstd[:], ALU.mult, ALU.mult,
                    r=[("ps", 4 + cc), "rstd"], w=[("cqnT", cc, bi)])
        for c in range(8):
            wa, wg = (2 * c) % 4, (2 * c + 1) % 4
            load_w(wch[wa], win_v[:, :, OFF_CONV + c * 128:OFF_CONV + (c + 1) * 128], 1024, ("wch", wa))
            load_w(wch[wg], win_v[:, :, OFF_CONV + 1024 + c * 128:OFF_CONV + 1024 + (c + 1) * 128], 1024, ("wch", wg))
            for bi, (t0, n, tiles) in enumerate(blocks):
                hr = [(("hTo", t), 0) for t in tiles]
                ba, bg_ = (4, 5) if bi % 2 == 0 else (6, 7)
                for k in range(8):
                    MM(ps[ba][:, 0:n], wch[wa][:, k, :], hTo[:, k, t0:t0 + n], start=(k == 0), stop=(k == 7),
                       r=hr + [("wch", wa)], w=[("ps", ba)])
                for k in range(8):
                    MM(ps[bg_][:, 0:n], wch[wg][:, k, :], hTo[:, k, t0:t0 + n], start=(k == 0), stop=(k == 7),
                       r=hr + [("wch", wg)], w=[("ps", bg_)])
                sgb = sg[bi % 2]
                ACT(sgb[:, 0:n], ps[bg_][:, 0:n], AF.Sigmoid, r=[("ps", bg_)], w=[("sg", bi % 2)])
                dst = ypad[:, c, 15 + t0:15 + t0 + n] if bi < 4 else yh[:, c, :]
                TT("vector", dst, ps[ba][:, 0:n], sgb[:, 0:n], ALU.mult, r=[("ps", ba), ("sg", bi % 2)],
                   w=[("ypad", c, bi)])
            TS("vector", ypad[:, c, 0:15], yh[:, c, 0:15], vecs[:, V_HM:V_HM + 1], None, ALU.mult,
               r=[("ypad", c, 4), "vecs"], w=[("ypad", c, 5)])
            TS("vector", ypad[:, c, NOWN + 15:NOWN + 30], yh[:, c, 15:30], vecs[:, V_HM + 1:V_HM + 2], None, ALU.mult,
               r=[("ypad", c, 4), "vecs"], w=[("ypad", c, 6)])
        if debug:
            dump("cqnT", cqnT[:].rearrange("p a b -> p (a b)"), [("cqnT", cc, bi) for cc in range(2) for bi in range(4)])
            dump("ypad", ypad[:].rearrange("p a b -> p (a b)"), [("ypad", c, i) for c in range(8) for i in range(7)])
        if stop_after == "O":
            P.emit()
            return nc
        BARRIER()

        dg = [ar.alloc([31, 128], BF16) for _ in range(2)]
        vT = ar.alloc([8, NOWN], BF16)
        sqv = [ar.alloc([512], BF16) for _ in range(2)]
        mean = ar.alloc([512], F32)
        msq = ar.alloc([512], F32)
        rstdc = ar.alloc([512], F32)
        tmpc = [ar.alloc([512], F32) for _ in range(2)]
        for c in range(8):
            d_ = dg[c % 2]
            TT("vector", d_[:], identb[:].unsqueeze(1).to_broadcast([128, 31, 128]),
               vecs[:, V_CW + c * 31:V_CW + (c + 1) * 31].unsqueeze(2).to_broadcast([128, 31, 128]), ALU.mult,
               r=[], w=[("dg", c % 2)])
            for tb in range(4):
                bank = 4 + (tb % 2)
                for k in range(31):
                    MM(ps[bank][:], d_[:, k, :], ypad[:, c, tb * 512 + k:tb * 512 + k + 512], start=(k == 0), stop=(k == 30),
                       r=[("dg", c % 2)], w=[("ps", bank)])
                ACT(vT[:, c, tb * 512:(tb + 1) * 512], ps[bank][:], AF.Identity, r=[("ps", bank)], w=[("vT", c, tb)],
                    bias=vecs[:, V_CB + c:V_CB + c + 1])
        for tb in range(4):
            tsl = slice(tb * 512, (tb + 1) * 512)
            for c in range(8):
                sv = sqv[c % 2]
                ACT(sv[:], vT[:, c, tsl], AF.Square, r=[("vT", c, tb)], w=[("sqv", c % 2)])
                MM(ps[6][:], onesb[:], vT[:, c, tsl], start=(c == 0), stop=(c == 7), r=[("vT", c, tb)], w=[("ps", 6)])
                MM(ps[7][:], onesb[:], sv[:], start=(c == 0), stop=(c == 7), r=[("sqv", c % 2)], w=[("ps", 7)])
            ACT(mean[:], ps[6][:], AF.Identity, r=[("ps", 6)], w=["mean"], scale=1.0 / D)
            ACT(msq[:], ps[7][:], AF.Identity, r=[("ps", 7)], w=["msq"], scale=1.0 / D)
            TT("vector", rstdc[:], mean[:], mean[:], ALU.mult, r=["mean"], w=["rstdc"])
            TT("vector", rstdc[:], msq[:], rstdc[:], ALU.subtract, r=["msq", "rstdc"], w=["rstdc"])
            ACT(rstdc[:], rstdc[:], AF.Sqrt, r=["rstdc"], w=["rstdc"], bias=epsb[:])
            RECIP(rstdc[:], rstdc[:], r=["rstdc"], w=["rstdc"])
            for c in range(8):
                tm = tmpc[c % 2]
                TT("vector", tm[:], vT[:, c, tsl], mean[:], ALU.subtract, r=[("vT", c, tb), "mean"], w=[("tmpc", c % 2)])
                TT("vector", tm[:], tm[:], rstdc[:], ALU.mult, r=[("tmpc", c % 2), "rstdc"], w=[("tmpc", c % 2)])
                ACT(vT[:, c, tsl], tm[:], AF.Silu, r=[("tmpc", c % 2)], w=[("vT", c, tb)],
                    scale=vecs[:, V_LNG + c:V_LNG + c + 1], bias=vecs[:, V_LNB + c:V_LNB + c + 1])
        zkeys = [("vT", c, tb) for c in range(8) for tb in range(4)]
        DMA(zts.rearrange("p (k t) -> p k t", k=8), vT[:], r=zkeys, w=["zts"])
        if debug:
            dump("zT", vT[:].rearrange("p a b -> p (a b)"), zkeys)
        if stop_after == "C":
            P.emit()
            return nc
        BARRIER()

        ar.off = RB
        OT = ar.alloc([8, NOWN], BF16)
        assert ar.off <= RC
        ar.off = RC
        VH = [ar.alloc([34, 128], BF16) for _ in range(2)]
        QT = [ar.alloc([NOWN], BF16) for _ in range(2)]
        PT = [ar.alloc([512], BF16) for _ in range(4)]
        wuqb = ar.alloc([2, 1536], BF16)
        wuqpb = ar.alloc([2, 1536], BF16)
        wukvb = ar.alloc([2048], BF16)
        rq = ar.alloc([2, NOWN], F32)
        qt1 = ar.alloc([512], F32)
        qt2 = ar.alloc([512], F32)
        rden = ar.alloc([512], F32)
        rdenB = ar.alloc([512], F32)
        wuq_v = wuq.rearrange("(k p) f -> p k f", p=128)
        wuqp_v = wuqp.rearrange("(k p) f -> p k f", p=128)
        for k in range(2):
            for hh in range(2):
                load_w(wuqb[:, k, hh * 768:(hh + 1) * 768], wuq_v[:, k, hh * 768:(hh + 1) * 768], 768, ("wuqb", k, hh))
                load_w(wuqpb[:, k, hh * 768:(hh + 1) * 768], wuqp_v[:, k, hh * 768:(hh + 1) * 768], 768, ("wuqpb", k, hh))
        for hh in range(2):
            load_w(wukvb[:, hh * 1024:(hh + 1) * 1024], wukv[:, hh * 1024:(hh + 1) * 1024], 1024, ("wukvb", hh))
        wq_keys = [("wuqb", k, hh) for k in range(2) for hh in range(2)]
        wqp_keys = [("wuqpb", k, hh) for k in range(2) for hh in range(2)]
        wkv_keys = [("wukvb", 0), ("wukvb", 1)]
        DMA(rq[0:96, 0, :], ropeq[0, 0:96, :], w=[("rq", 0)])
        DMA(rq[0:96, 1, :], ropeq[1, 0:96, :], w=[("rq", 1)], q="scalar")
        for hb in range(2):
            MEMSET("gpsimd", VH[hb][:], 0.0, w=[("VH", hb)])
        MEMSET("gpsimd", VH[0][:, :, 64:65], 1.0, w=[("VH", 0)])
        MEMSET("gpsimd", VH[1][:, :, 0:1], 1.0, w=[("VH", 1)])
        ckeys = [("ckvnT", b_) for b_ in range(9)]
        cvf = [ar.alloc([2, 1024], F32) for _ in range(2)]
        cvb = [ar.alloc([2, 1024], BF16) for _ in range(2)]
        cv_steps = [(tab, tabb, nm, g) for (tab, tabb, nm) in ((ut, utb, "u"), (vt, vtb, "v")) for g in range(64)]
        cv_pos = [0]

        def conv_step():
            if cv_pos[0] >= len(cv_steps):
                return
            tab, tabb, nm, g = cv_steps[cv_pos[0]]
            s = cv_pos[0] % 2
            DMA(cvf[s][:], tab[g * 2:(g + 1) * 2].rearrange("i p f -> p i f"), w=[("cvf", s)], q="sync")
            CP(("gpsimd", "vector")[cv_pos[0] % 2], cvb[s][:], cvf[s][:], r=[("cvf", s)], w=[("cvb", s)])
            DMA(tabb[g], cvb[s][:].rearrange("p a b -> p (a b)"), r=[("cvb", s)], w=[("tabb", nm, g)], q="sync",
                sem_key=("tabb", s))
            cv_pos[0] += 1

        def prep_steps(h):
            hb = h % 2
            voff = 0 if hb == 0 else 64
            steps = []
            for blk in range(9):
                def st_k(blk=blk):
                    n = 512 if blk < 8 else 256
                    bank = 5 + (blk % 2)
                    MM(ps[bank][0:64, 0:n], wukvb[:, h * 128:h * 128 + 64], ckvnT[:, blk * 512:blk * 512 + n],
                       r=wkv_keys + ckeys, w=[("ps", bank)])
                    CP("vector", KT[hb][0:64, blk * 512:blk * 512 + n], ps[bank][0:64, 0:n], r=[("ps", bank)], w=[("KTn", hb)])
                steps.append(st_k)
            for g in range(5):
                def st_v(g=g):
                    cnt = 8 if g < 4 else 2
                    bank = 5 + (g % 2)
                    for j in range(cnt):
                        kt = g * 8 + j
                        MM(ps[bank][:, j * 64:(j + 1) * 64], ckvnT[:, kt * 128:(kt + 1) * 128], wukvb[:, h * 128 + 64:h * 128 + 128],
                           r=wkv_keys + ckeys, w=[("ps", bank)])
                    CP("vector", VH[hb][:, g * 8:g * 8 + cnt, voff:voff + 64],
                       ps[bank][:, 0:cnt * 64].rearrange("p (a b) -> p a b", b=64), r=[("ps", bank)], w=[("VH", hb)])
                steps.append(st_v)
            for qb in range(4):
                def st_q(qb=qb):
                    qs = slice(qb * 512, (qb + 1) * 512)
                    for k in range(2):
                        MM(ps[5][0:96, :], wuqb[:, k, h * 96:(h + 1) * 96], cqnT[:, k, qs], start=(k == 0), stop=(k == 1),
                           r=wq_keys + [("cqnT", k, qb)], w=[("ps", 5)])
                    for k in range(2):
                        MM(ps[6][0:96, :], wuqpb[:, k, h * 96:(h + 1) * 96], cqnT[:, k, qs], start=(k == 0), stop=(k == 1),
                           r=wqp_keys + [("cqnT", k, qb)], w=[("ps", 6)])
                    TT("vector", qt1[0:96, :], ps[5][0:96, :], rq[0:96, 0, qs], ALU.mult, r=[("ps", 5), ("rq", 0)], w=["qt1"])
                    TT("vector", qt2[0:96, :], ps[6][0:96, :], rq[0:96, 1, qs], ALU.mult, r=[("ps", 6), ("rq", 1)], w=["qt2"])
                    TT("vector", QT[hb][0:96, qs], qt1[0:96, :], qt2[0:96, :], ALU.add, r=["qt1", "qt2"], w=[("QT", hb)])
                steps.append(st_q)
            return steps

        def prep(h):
            for st_ in prep_steps(h):
                st_()

        prep(0)
        gi = [0]
        for h in range(NH):
            hb = h % 2
            ktr = [("KTn", hb)] + [("KTr", hb, b_) for b_ in range(9)]
            items = [(qb, kt) for qb in range(4) for kt in range(34)]
            base = gi[0]

            def S(idx):
                qb, kt = items[idx]
                sb_ = (base + idx) % 3
                MM(ps[sb_][:], KT[hb][0:96, kt * 128:(kt + 1) * 128], QT[hb][0:96, qb * 512:(qb + 1) * 512],
                   r=ktr + [("QT", hb)], w=[("ps", sb_)])

            pending = []
            nsteps = prep_steps(h + 1) if h + 1 < NH else []
            S(0)
            S(1)
            for idx, (qb, kt) in enumerate(items):
                if idx + 2 < len(items):
                    S(idx + 2)
                sb_ = (base + idx) % 3
                pb_ = (base + idx) % 4
                ob = 3 + (qb % 2)
                qs = slice(qb * 512, (qb + 1) * 512)
                ACT(PT[pb_][:], ps[sb_][:], AF.Exp, r=[("ps", sb_)], w=[("PT", pb_)], scale=ATTN_SCALE)
                MM(ps[ob][:], VH[hb][:, kt, :], PT[pb_][:], start=(kt == 0), stop=(kt == 33),
                   r=[("VH", hb), ("PT", pb_)], w=[("ps", ob)])
                if pending and pending[0][0] <= idx:
                    _, (r0, prow, ob2, qs2) = pending.pop(0)
                    MM(ps[7][:], onesf[r0:r0 + 1, :], rden[r0:r0 + 1, :], r=["rden"], w=[("ps", 7)])
                    CP("vector", rdenB[:], ps[7][:], r=[("ps", 7)], w=["rdenB"])
                    TT("vector", OT[prow, h // 2, qs2], ps[ob2][prow, :], rdenB[prow, :], ALU.mult, r=[("ps", ob2), "rdenB"],
                       w=[("OT", h, qs2.start // 512)])
                if kt == 33:
                    r0 = 64 if hb == 0 else 0
                    prow = slice(0, 64) if hb == 0 else slice(64, 128)
                    RECIP(rden[r0:r0 + 1, :], ps[ob][r0:r0 + 1, :], r=[("ps", ob)], w=["rden"])
                    pending.append((idx + 6 if qb < 3 else idx, (r0, prow, ob, qs)))
                if idx % 17 == 8:
                    conv_step()
                if idx >= 10 and idx % 5 == 0 and nsteps:
                    nsteps.pop(0)()
            while pending:
                _, (r0, prow, ob2, qs2) = pending.pop(0)
                MM(ps[7][:], onesf[r0:r0 + 1, :], rden[r0:r0 + 1, :], r=["rden"], w=[("ps", 7)])
                CP("vector", rdenB[:], ps[7][:], r=[("ps", 7)], w=["rdenB"])
                TT("vector", OT[prow, h // 2, qs2], ps[ob2][prow, :], rdenB[prow, :], ALU.mult, r=[("ps", ob2), "rdenB"],
                   w=[("OT", h, qs2.start // 512)])
            while nsteps:
                nsteps.pop(0)()
            gi[0] += len(items)
        while cv_pos[0] < len(cv_steps):
            conv_step()
        okeys = [("OT", h, qb) for h in range(NH) for qb in range(4)]
        if debug:
            dump("OT", OT[:].rearrange("p a b -> p (a b)"), okeys)
        if stop_after == "ATT":
            P.emit()
            return nc
        BARRIER()

        ar.off = RA
        mT = ar.alloc([8, NOWN], BF16)
        assert ar.off <= RB
        ar.off = RC
        hTo2 = ar.alloc([8, NOWN], BF16)
        zT2 = ar.alloc([8, NOWN], BF16)
        DMA(hTo2[:], hts.rearrange("p (k t) -> p k t", k=8), w=["hTo2"])
        DMA(zT2[:], zts.rearrange("p (k t) -> p k t", k=8), w=["zT2"], q="scalar")
        wm = [[ar.alloc([8, 128], BF16) for _ in range(4)] for _ in range(2)]
        sga = [ar.alloc([512], F32) for _ in range(2)]
        sgc = [ar.alloc([512], F32) for _ in range(2)]
        m1 = [ar.alloc([512], F32) for _ in range(2)]
        womla_v = womla.rearrange("(k p) f -> p k f", p=128)
        wpw_v = wpw.rearrange("(k p) f -> p k f", p=128)
        for c in range(8):
            ws = c % 2
            cs = slice(c * 128, (c + 1) * 128)
            load_w(wm[ws][0], womla_v[:, :, cs], 1024, ("wm", ws, 0))
            load_w(wm[ws][1], wpw_v[:, :, cs], 1024, ("wm", ws, 1))
            load_w(wm[ws][2], win_v[:, :, OFF_GATE + c * 128:OFF_GATE + (c + 1) * 128], 1024, ("wm", ws, 2))
            load_w(wm[ws][3], win_v[:, :, OFF_GATE + 1024 + c * 128:OFF_GATE + 1024 + (c + 1) * 128], 1024, ("wm", ws, 3))
            for tb in range(4):
                tsl = slice(tb * 512, (tb + 1) * 512)
                banks = (0, 1, 2, 3) if tb % 2 == 0 else (4, 5, 6, 7)
                srcs = [(OT, "OT"), (zT2, "zT2"), (hTo2, "hTo2"), (hTo2, "hTo2")]
                for i in range(4):
                    for k in range(8):
                        MM(ps[banks[i]][:], wm[ws][i][:, k, :], srcs[i][0][:, k, tsl], start=(k == 0), stop=(k == 7),
                           r=[("wm", ws, i), srcs[i][1]], w=[("ps", banks[i])])
                p2 = tb % 2
                ACT(sga[p2][:], ps[banks[2]][:], AF.Sigmoid, r=[("ps", banks[2])], w=[("sga", p2)])
                ACT(sgc[p2][:], ps[banks[3]][:], AF.Sigmoid, r=[("ps", banks[3])], w=[("sgc", p2)])
                TT("vector", m1[p2][:], ps[banks[0]][:], sga[p2][:], ALU.mult, r=[("ps", banks[0]), ("sga", p2)], w=[("m1", p2)])
                TT("vector", sgc[p2][:], ps[banks[1]][:], sgc[p2][:], ALU.mult, r=[("ps", banks[1]), ("sgc", p2)], w=[("sgc", p2)])
                TT("vector", mT[:, c, tsl], m1[p2][:], sgc[p2][:], ALU.add, r=[("m1", p2), ("sgc", p2)], w=[("mT", c, tb)])
        if stop_after == "M":
            P.emit()
            return nc
        BARRIER()

        ar.off = RB
        h2T = ar.alloc([8, NOWN], BF16)
        assert ar.off <= RC
        ar.off = RC
        woutb = ar.alloc([8, 1024], BF16)
        wout_v = wout.rearrange("(k p) f -> p k f", p=128)
        for k in range(8):
            load_w(woutb[:, k, :], wout_v[:, k, :], 1024, ("woutb", k))
        wok = [("woutb", k) for k in range(8)]
        xts = [ar.alloc([D], F32) for _ in range(2)]
        x1t = [ar.alloc([D], F32) for _ in range(2)]
        sqj = ar.alloc([D], F32)
        xns = [ar.alloc([D], BF16) for _ in range(2)]
        sss = [ar.alloc([1], F32) for _ in range(2)]
        rss = [ar.alloc([1], F32) for _ in range(2)]
        for tt in range(16):
            s = tt % 2
            DMA(xts[s][:], xo[tt], w=[("xo2", s)], q=dmaq[tt % 2])
            for half in range(2):
                bank = (4 + half) if s == 0 else (6 + half)
                hs = slice(half * 512, (half + 1) * 512)
                for k in range(8):
                    MM(ps[bank][:], mT[:, k, tt * 128:(tt + 1) * 128], woutb[:, k, hs], start=(k == 0), stop=(k == 7),
                       r=wok + ["mT"], w=[("ps", bank)])
                TT("vector", x1t[s][:, hs], ps[bank][:], g1row[:, hs], ALU.mult, r=[("ps", bank)], w=[("x1t", s, half)])
                TT("gpsimd", x1t[s][:, hs], x1t[s][:, hs], xts[s][:, hs], ALU.add, r=[("x1t", s, half), ("xo2", s)], w=[("x1t", s, half)])
            DMA(x1s[tt], x1t[s][:], r=[("x1t", s, 0), ("x1t", s, 1)], w=[("x1s", tt)])
            P.op("gpsimd", lambda e: e.memset(junk[:, 1:2], 0.0), reads=[("x1t", s, 0), ("x1t", s, 1)], writes=[(("n2", s), "xt")])
            norm_tile(None, h2T[:, :, tt * 128:(tt + 1) * 128], tt, A2[:], modT[:, 24:32, 0],
                      x1t[s][:], sqj[:], xns[s][:], sss[s][:], rss[s][:], 2 + s, ("n2", s), ("h2T", tt))
        if stop_after == "M2":
            P.emit()
            return nc
        BARRIER()

        ar.off = RA
        E1T = ar.alloc([NOWN], BF16)
        E2T = ar.alloc([NOWN], BF16)
        WT = ar.alloc([NOWN], F32)
        ar.off = RC
        subkb = ar.alloc([16, 128], BF16)
        wpqc = [ar.alloc([8, 128], BF16) for _ in range(2)]
        qT = ar.alloc([16, 512], BF16)
        scs = [ar.alloc([16, 128], F32) for _ in range(2)]
        sc2 = ar.alloc([16, 128], F32)
        stop_ = ar.alloc([16, 16], F32)
        itopu = ar.alloc([16, 16], U32)
        itopf = ar.alloc([16, 16], F32)
        cand = ar.alloc([8, 256], F32)
        cand2 = ar.alloc([8, 256], F32)
        best = ar.alloc([8, 16], F32)
        ciu = ar.alloc([8, 16], U32)
        cif = ar.alloc([8, 16], F32)
        c1f = ar.alloc([8, 16], F32)
        c2f = ar.alloc([8, 16], F32)
        eq = ar.alloc([8, 16, 16], F32)
        e1f = ar.alloc([8, 16], F32)
        e2f = ar.alloc([8, 16], F32)
        ex = ar.alloc([8, 16], F32)
        se = ar.alloc([8], F32)
        wg = ar.alloc([8, 16], F32)
        iota16 = ar.alloc([16], F32)
        thr16 = ar.alloc([16], F32)
        P.op("gpsimd", lambda e: e.iota(iota16[:], pattern=[[1, 16]], base=0, channel_multiplier=0,
                                        allow_small_or_imprecise_dtypes=True), writes=["iota16"])
        P.op("gpsimd", lambda e: e.iota(thr16[:], pattern=[[16, 16]], base=16, channel_multiplier=0,
                                        allow_small_or_imprecise_dtypes=True), writes=["thr16"])
        MEMSET("gpsimd", thr16[:, 15:16], 1.0e9, w=["thr16"])
        load_w(subkb[:, 0:8, :], subkT_d[:, 0:1024].rearrange("p (a b) -> p a b", b=128), 1024, ("subkb", 0))
        load_w(subkb[:, 8:16, :], subkT_d[:, 1024:2048].rearrange("p (a b) -> p a b", b=128), 1024, ("subkb", 1))
        wpq_v = wpq.rearrange("(k p) f -> p k f", p=128)
        itop_v = itopf[:].rearrange("p (h two) m -> p h two m", two=2)
        stop_v = stop_[:].rearrange("p (h two) m -> p h two m", two=2)
        B4 = [128, 8, 16, 16]
        for tb in range(4):
            for j in range(16):
                ws = j % 2
                load_w(wpqc[ws], wpq_v[:, :, j * 128:(j + 1) * 128], 1024, ("wpqc", ws))
                for k in range(8):
                    MM(ps[4 + ws][:], wpqc[ws][:, k, :], h2T[:, k, tb * 512:(tb + 1) * 512], start=(k == 0), stop=(k == 7),
                       r=[("wpqc", ws), "h2T"], w=[("ps", 4 + ws)])
                CP("vector" if j % 2 else "scalar", qT[:, j, :], ps[4 + ws][:], r=[("ps", 4 + ws)], w=[("qT", j)])
            def score(tt_):
                tl_ = tt_ % 4
                sc_ = scs[tt_ % 2]
                for j in range(16):
                    MM(ps[j // 4][:, (j % 4) * 128:(j % 4 + 1) * 128], qT[:, j, tl_ * 128:(tl_ + 1) * 128], subkb[:, j, :],
                       r=[("qT", j), ("subkb", 0), ("subkb", 1)], w=[("ps", j // 4)])
                for g in range(4):
                    CP("scalar", sc_[:, g * 4:(g + 1) * 4, :], ps[g][:].rearrange("p (a b) -> p a b", b=128), r=[("ps", g)],
                       w=[("sc", tt_ % 2, g)])

            score(tb * 4)
            for tl in range(4):
                tt = tb * 4 + tl
                if tl < 3:
                    score(tt + 1)
                sc = scs[tt % 2]
                def sk_(j):
                    return ("sc", tt % 2, j // 4)
                for j in range(16):
                    P.op("vector", lambda e, j=j, sc=sc: e.max(out=stop_[:, j, 0:8], in_=sc[:, j, :]), reads=[sk_(j)], writes=[("stop", j)])
                for j in range(16):
                    P.op("vector", lambda e, j=j, sc=sc: e.max_index(out=itopu[:, j, 0:8], in_max=stop_[:, j, 0:8], in_values=sc[:, j, :]),
                         reads=[sk_(j), ("stop", j)], writes=[("itopu", j)])
                for j in range(16):
                    P.op("vector", lambda e, j=j, sc=sc: e.match_replace(out=sc2[:, j, :], in_to_replace=stop_[:, j, 0:8], in_values=sc[:, j, :], imm_value=-1.0e30),
                         reads=[sk_(j), ("stop", j)], writes=[("sc2", j)])
                for j in range(16):
                    P.op("vector", lambda e, j=j: e.max(out=stop_[:, j, 8:16], in_=sc2[:, j, :]), reads=[("sc2", j)], writes=[("stop", j)])
                for j in range(16):
                    P.op("vector", lambda e, j=j: e.max_index(out=itopu[:, j, 8:16], in_max=stop_[:, j, 8:16], in_values=sc2[:, j, :]),
                         reads=[("sc2", j), ("stop", j)], writes=[("itopu", j)])
                sk = [("stop", j) for j in range(16)]
                ik = [("itopu", j) for j in range(16)]
                CP("vector", itopf[:], itopu[:], r=ik, w=["itopf"])
                TT("vector", cand[:].rearrange("p h (a b) -> p h a b", b=16), stop_v[:, :, 0, :].unsqueeze(3).to_broadcast(B4),
                   stop_v[:, :, 1, :].unsqueeze(2).to_broadcast(B4), ALU.add, r=sk, w=["cand"])
                for h in range(8):
                    P.op("vector", lambda e, h=h: e.max(out=best[:, h, 0:8], in_=cand[:, h, :]), reads=["cand"], writes=[("best", h)])
                for h in range(8):
                    P.op("vector", lambda e, h=h: e.max_index(out=ciu[:, h, 0:8], in_max=best[:, h, 0:8], in_values=cand[:, h, :]),
                         reads=["cand", ("best", h)], writes=[("ciu", h)])
                for h in range(8):
                    P.op("vector", lambda e, h=h: e.match_replace(out=cand2[:, h, :], in_to_replace=best[:, h, 0:8], in_values=cand[:, h, :], imm_value=-1.0e30),
                         reads=["cand", ("best", h)], writes=[("cand2", h)])
                for h in range(8):
                    P.op("vector", lambda e, h=h: e.max(out=best[:, h, 8:16], in_=cand2[:, h, :]), reads=[("cand2", h)], writes=[("best", h)])
                for h in range(8):
                    P.op("vector", lambda e, h=h: e.max_index(out=ciu[:, h, 8:16], in_max=best[:, h, 8:16], in_values=cand2[:, h, :]),
                         reads=[("cand2", h), ("best", h)], writes=[("ciu", h)])
                bk = [("best", h) for h in range(8)]
                ck = [("ciu", h) for h in range(8)]
                CP("vector", cif[:], ciu[:], r=ck, w=["cif"])
                TT("vector", eq[:], cif[:].unsqueeze(3).to_broadcast(B4), thr16[:].unsqueeze(1).unsqueeze(1).to_broadcast(B4), ALU.is_ge,
                   r=["cif", "thr16"], w=["eq"])
                P.op("vector", lambda e: e.tensor_reduce(out=c1f[:], in_=eq[:], axis=AX.X, op=ALU.add), reads=["eq"], writes=["c1f"])
                STT(c2f[:], c1f[:], -16.0, cif[:], ALU.mult, ALU.add, r=["c1f", "cif"], w=["c2f"])
                for (cf_, two, ef_) in ((c1f, 0, e1f), (c2f, 1, e2f)):
                    TT("vector", eq[:], cf_[:].unsqueeze(3).to_broadcast(B4), iota16[:].unsqueeze(1).unsqueeze(1).to_broadcast(B4), ALU.is_equal,
                       r=["c1f", "c2f", "iota16"], w=["eq"])
                    TT("vector", eq[:], eq[:], itop_v[:, :, two, :].unsqueeze(2).to_broadcast(B4), ALU.mult, r=["eq", "itopf"], w=["eq"])
                    P.op("vector", lambda e, ef_=ef_: e.tensor_reduce(out=ef_[:], in_=eq[:], axis=AX.X, op=ALU.add), reads=["eq"], writes=[("ef", two)])
                TT("vector", ex[:], best[:], best[:, :, 0:1].to_broadcast([128, 8, 16]), ALU.subtract, r=bk, w=["ex"])
                ACT(ex[:], ex[:], AF.Exp, r=["ex"], w=["ex"])
                P.op("vector", lambda e: e.tensor_reduce(out=se[:], in_=ex[:], axis=AX.X, op=ALU.add), reads=["ex"], writes=["se"])
                RECIP(se[:], se[:], r=["se"], w=["se"])
                TT("vector", wg[:], ex[:], se[:].unsqueeze(2).to_broadcast([128, 8, 16]), ALU.mult, r=["ex", "se"], w=["wg"])
                tsl = slice(tt * 128, (tt + 1) * 128)
                TR(ps[6][:, 0:128], e1f[:].rearrange("p a b -> p (a b)"), identf[:], r=[("ef", 0)], w=[("ps", 6)])
                TR(ps[6][:, 128:256], e2f[:].rearrange("p a b -> p (a b)"), identf[:], r=[("ef", 1)], w=[("ps", 6)])
                TR(ps[6][:, 256:384], wg[:].rearrange("p a b -> p (a b)"), identf[:], r=["wg"], w=[("ps", 6)])
                CP("scalar", E1T[:, tsl], ps[6][:, 0:128], r=[("ps", 6)], w=[("E1T", tt)])
                CP("scalar", E2T[:, tsl], ps[6][:, 128:256], r=[("ps", 6)], w=[("E2T", tt)])
                CP("scalar", WT[:, tsl], ps[6][:, 256:384], r=[("ps", 6)], w=[("WT", tt)])
        if stop_after == "P0":
            P.emit()
            return nc
        BARRIER()

        ar.off = RA + 16 * 1024
        E1Tm = ar.alloc([NOWN], BF16)
        iota3 = ar.alloc([8, 128], BF16)
        iotaH = ar.alloc([8, 64], BF16)
        xfin = ar.alloc([D], F32)
        x1r = ar.alloc([D], F32)
        assert ar.off <= RB
        ar.off = RC
        Wh = [ar.alloc([256, 64], BF16) for _ in range(2)]
        NSLOT = 3
        UTb = [ar.alloc([2, 1024], BF16) for _ in range(NSLOT)]
        Vb = [ar.alloc([2, 1024], BF16) for _ in range(NSLOT)]
        Gs = [ar.alloc([256], BF16) for _ in range(3)]
        GWs = [ar.alloc([256], BF16) for _ in range(3)]
        ssf = ar.alloc([1], F32)
        E1p = [ar.alloc([8, 64], BF16) for _ in range(3)]
        E2p = [ar.alloc([8, 128], BF16) for _ in range(3)]
        xsq = x1r.bitcast(BF16)[:, 0:D]
        P.op("gpsimd", lambda e: e.iota(iota3[:], pattern=[[0, 8], [1, 128]], base=0, channel_multiplier=0,
                                        allow_small_or_imprecise_dtypes=True), writes=["iota3"])
        P.op("gpsimd", lambda e: e.iota(iotaH[:], pattern=[[0, 8], [1, 64]], base=0, channel_multiplier=0,
                                        allow_small_or_imprecise_dtypes=True), writes=["iotaH"])
        TS("vector", E1Tm[:], E1T[:], -64.0, None, ALU.add, r=[], w=["E1Tm"])

        def b_eq(tb_, hf_, m):
            t0 = tb_ * 256 + m * 8
            bi = m % 3
            e1src = E1T if hf_ == 0 else E1Tm
            TT("vector", E1p[bi][:], iotaH[:], e1src[:, t0:t0 + 8].unsqueeze(2).to_broadcast([128, 8, 64]), ALU.is_equal,
               r=["iotaH", "E1Tm"], w=[("E1p", bi)])
            TT("vector", E2p[bi][:], iota3[:], E2T[:, t0:t0 + 8].unsqueeze(2).to_broadcast([128, 8, 128]), ALU.is_equal,
               r=["iota3"], w=[("E2p", bi)])

        def b_mul(tb_, hf_, m):
            t0 = tb_ * 256 + m * 8
            bi = m % 3
            TT("vector", E1p[bi][:], E1p[bi][:], WT[:, t0:t0 + 8].unsqueeze(2).to_broadcast([128, 8, 64]), ALU.mult,
               r=[("E1p", bi)], w=[("E1p", bi)])

        def b_mm_pe(tb_, hf_, m):
            bi = m % 3
            for tq in range(8):
                MM(ps[6][:, tq * 64:(tq + 1) * 64], E2p[bi][:, tq, :], E1p[bi][:, tq, :], r=[("E1p", bi), ("E2p", bi)], w=[("ps", 6)])

        def b_mm_act(tb_, hf_, m):
            CP("scalar", Wh[hf_][:, m * 8:m * 8 + 8, :], ps[6][:].rearrange("p (a b) -> p a b", b=64), r=[("ps", 6)], w=[("Wh", hf_)])

        def b_mm(tb_, hf_, m):
            b_mm_pe(tb_, hf_, m)
            b_mm_act(tb_, hf_, m)

        def loadg(cg):
            s = cg % NSLOT
            DMA(UTb[s][:].rearrange("p a b -> p (a b)"), utb[cg], w=[("UTb", s)], q="sync")
            DMA(Vb[s][:].rearrange("p a b -> p (a b)"), vtb[cg], w=[("Vb", s)], q="sync")

        for m in range(32):
            b_eq(0, 0, m)
            b_mul(0, 0, m)
            b_mm(0, 0, m)
        for tb in range(8):
            T0 = tb * 256
            ABANK = (4, 5, 7)

            def Amm(i):
                s = (i // 2) % NSLOT
                ab = i % 3
                for k in range(8):
                    MM(ps[ABANK[ab]][:, 0:256], UTb[s][:, i % 2, k * 128:(k + 1) * 128], h2T[:, k, T0:T0 + 256], start=(k == 0), stop=(k == 7),
                       r=[("UTb", s), "h2T"], w=[("ps", ABANK[ab])])

            for c0 in range(NSLOT):
                loadg(c0)
            Amm(0)
            Amm(1)
            for i in range(128):
                hf2 = i // 64
                if i + 2 < 128:
                    Amm(i + 2)
                cg = i // 2
                s = cg % NSLOT
                ab = i % 3
                nxt = (tb, 1) if hf2 == 0 else ((tb + 1, 0) if tb + 1 < 8 else None)
                m = (i % 64) // 2
                do_b = nxt is not None and i % 2 == 0 and m >= 2
                if do_b:
                    b_mm_pe(nxt[0], nxt[1], m - 2)
                ACT(Gs[ab][:], ps[ABANK[ab]][:, 0:256], AF.Gelu, r=[("ps", ABANK[ab])], w=[("Gs", ab)])
                if do_b:
                    b_mm_act(nxt[0], nxt[1], m - 2)
                TT("vector", GWs[ab][:], Gs[ab][:], Wh[hf2][:, :, i - 64 * hf2], ALU.mult, r=[("Gs", ab), ("Wh", hf2)], w=[("GWs", ab)])
                for tl in range(2):
                    for half in range(2):
                        MM(ps[tl * 2 + half][:], GWs[ab][:, tl * 128:(tl + 1) * 128], Vb[s][:, i % 2, half * 512:(half + 1) * 512],
                           start=(i == 0), stop=(i == 127), r=[("GWs", ab), ("Vb", s)], w=[("ps", tl * 2 + half)])
                if i % 2 == 1 and cg + NSLOT < 64:
                    loadg(cg + NSLOT)
                if nxt is not None:
                    if i % 2 == 0:
                        b_eq(nxt[0], nxt[1], m)
                    else:
                        b_mul(nxt[0], nxt[1], m)
                        if m == 31:
                            b_mm(nxt[0], nxt[1], 30)
                            b_mm(nxt[0], nxt[1], 31)
            for tl in range(2):
                tt = tb * 2 + tl
                DMA(x1r[:], x1s[tt], w=["x1r"])
                for half in range(2):
                    hs = slice(half * 512, (half + 1) * 512)
                    TT("vector", xfin[:, hs], ps[tl * 2 + half][:], g2row[:, hs], ALU.mult, r=[("ps", tl * 2 + half)], w=["xfin"])
                TT("vector", xfin[:], xfin[:], x1r[:], ALU.add, r=["xfin", "x1r"], w=["xfin"])
                ACT(xsq, xfin[:], AF.Square, r=["xfin"], w=["x1r", "ssf"], accum=ssf[:])
                ACT(ssf[:], ssf[:], AF.Sqrt, r=["ssf"], w=["ssf"], scale=1.0 / D, bias=epsb[:])
                RECIP(ssf[:], ssf[:], r=["ssf"], w=["ssf"])
                STT(xfin[:], xfin[:], ssf[:, 0:1], fgrow[:], ALU.mult, ALU.mult, r=["xfin", "ssf"], w=["xfin"])
                DMA(out_d[tt], xfin[:], r=["xfin"], w=[("out", tt)], sem_key="out")

        P.emit()
    return nc


def _rope_tabs(pos):
    n_freq = 8
    freq = (np.float32(10000.0) ** (-np.arange(n_freq, dtype=np.float32) / np.float32(n_freq))).astype(np.float32)
    r = (pos // 64).astype(np.float32)
    c = (pos % 64).astype(np.float32)
    ang = np.stack([r[:, None] * freq[None, :], c[:, None] * freq[None, :]], axis=1).astype(np.float32)
    cos = np.cos(ang).astype(np.float32)
    sin = np.sin(ang).astype(np.float32)
    ct = np.zeros((32, pos.shape[0]), np.float32)
    stb = np.zeros((32, pos.shape[0]), np.float32)
    perm = np.zeros(32, np.int64)
    for a in range(2):
        for half in range(2):
            for f in range(8):
                d = a * 16 + half * 8 + f
                perm[d] = a * 16 + (1 - half) * 8 + f
                ct[d] = cos[:, a, f]
                stb[d] = -sin[:, a, f] if half == 0 else sin[:, a, f]
    return ct, stb, perm


def prep_inputs(inp):
    f = lambda a: np.ascontiguousarray(np.asarray(a, dtype=np.float32))
    x, c, ctx, c_ctx = f(inp["x"]), f(inp["c"]), f(inp["ctx"]), f(inp["c_ctx"])
    w_in = f(inp["w_in"])[0]
    _, _, perm = _rope_tabs(np.arange(4))
    shared = {}
    shared["wmod"] = f(inp["w_mod"])[0]
    b_mod = f(inp["b_mod"])[0]
    shared["bmodg"] = np.ascontiguousarray(np.broadcast_to(np.concatenate([b_mod[2048:3072], b_mod[5120:6144]])[None, :], (128, 2048)))
    shared["fgrow"] = np.ascontiguousarray(np.broadcast_to(f(inp["final_g"])[None, :], (128, D)))
    shared["win"] = w_in
    wkr = np.zeros((D, 192), np.float32)
    wkr[:, 64:96] = w_in[:, OFF_KR:OFF_KR + 32]
    wkr[:, 160:192] = w_in[:, OFF_KR + perm]
    shared["wkr"] = wkr
    w_uq = f(inp["w_uq"])[0]
    shared["wuq"] = w_uq
    colp = np.arange(1536).reshape(16, 96).copy()
    for h in range(16):
        colp[h, 64:96] = h * 96 + 64 + perm
    shared["wuqp"] = np.ascontiguousarray(w_uq[:, colp.reshape(-1)])
    shared["wukv"] = f(inp["w_ukv"])[0]
    shared["womla"] = f(inp["w_o_mla"])[0]
    shared["wpw"] = f(inp["w_pw"])[0]
    shared["wout"] = f(inp["w_out"])[0]
    shared["wpq"] = f(inp["w_pq"])[0]
    sk = f(inp["sub_keys"])[0].reshape(16, 128, 128)
    shared["subkT"] = np.ascontiguousarray(sk.transpose(2, 0, 1).reshape(128, 16 * 128))
    u = f(inp["u_experts"])[0]
    shared["ut"] = np.ascontiguousarray(u.reshape(128, 128, 8, 128).transpose(0, 3, 2, 1).reshape(128, 128, 1024))
    shared["vt"] = f(inp["v_experts"])[0].reshape(128, 128, 1024)
    ck, sk_, _ = _rope_tabs(np.arange(SEQ))
    ropek = np.zeros((2, 128, SEQ), np.float32)
    ropek[0, 64:96] = ck
    ropek[1, 64:96] = sk_
    shared["ropek"] = ropek

    def tomaj(v):
        return v.reshape(-1, 128).T

    maps = []
    for core in range(8):
        b, hf = core // 2, core % 2
        m = dict(shared)
        m["xk"] = np.ascontiguousarray(np.concatenate([x[b], ctx[b]], axis=0).reshape(34, 128, D))
        xo = np.zeros((17 * 128, D), np.float32)
        lo = hf * NOWN
        xo[:NOWN] = x[b, lo:lo + NOWN]
        if hf == 1:
            xo[NOWN:NOWN + 15] = x[b, lo - 15:lo]
        else:
            xo[NOWN + 15:NOWN + 30] = x[b, lo + NOWN:lo + NOWN + 15]
        m["xo"] = xo.reshape(17, 128, D)
        vecs = np.zeros((128, NV), np.float32)
        vecs[:, 0:8] = tomaj(f(inp["norm1_g"])[0])
        vecs[:, 8:16] = tomaj(f(inp["norm2_g"])[0])
        vecs[:, 16:18] = tomaj(f(inp["q_norm_g"])[0])
        vecs[:, 18:19] = tomaj(f(inp["kv_norm_g"])[0])
        vecs[:, 19:27] = tomaj(f(inp["conv_b"])[0])
        vecs[:, 27:35] = tomaj(f(inp["conv_ln_g"])[0])
        vecs[:, 35:43] = tomaj(f(inp["conv_ln_b"])[0])
        cw = f(inp["conv_w"])[0]
        vecs[:, 43:291] = cw.reshape(31, 8, 128).transpose(2, 1, 0).reshape(128, 248)
        vecs[:, 291:339] = tomaj(b_mod)
        ct2 = np.stack([tomaj(c[b]), tomaj(c_ctx)], axis=2)
        vecs[:, 339:355] = ct2.reshape(128, 16)
        vecs[:, 355] = 1.0 if hf == 1 else 0.0
        vecs[:, 356] = 1.0 if hf == 0 else 0.0
        m["vecs"] = vecs
        cq_, sq_, _ = _rope_tabs(np.arange(lo, lo + NOWN))
        ropeq = np.zeros((2, 128, NOWN), np.float32)
        ropeq[0, 0:64] = 1.0
        ropeq[0, 64:96] = cq_
        ropeq[1, 64:96] = sq_
        m["ropeq"] = ropeq
        maps.append(m)
    return maps


_NC_CACHE = {}


def kernel(**inputs):
    maps = prep_inputs(inputs)
    if "nc" not in _NC_CACHE:
        _NC_CACHE["nc"] = build_nc()
    nc = _NC_CACHE["nc"]
    res = run_bass_kernel_spmd(nc, maps, core_ids=list(range(8)))
    out = np.zeros((4, SEQ, D), np.float32)
    for core in range(8):
        b, hf = core // 2, core % 2
        out[b, hf * NOWN:(hf + 1) * NOWN] = np.asarray(res.results[core]["out"]).reshape(NOWN, D)
    return out
```
